# Optimizing a Trainium2 kernel written in Bass

```python
import math
import jax
import jax.numpy as jnp
from jax import lax
import numpy as np


D_MODEL = 1024
BATCH = 8
SEQ = 8192
DEPTH = 2

CTX_LEN = 256
GRID_W = 64
RMS_EPS = 1e-6
ATT_HEADS = 4
ATT_DH = 64
ATT_DV = 2 * ATT_DH
ATT_QK_W = ATT_HEADS * 2 * ATT_DH
ATT_V_W = ATT_HEADS * ATT_DV
Q_BLOCK = 128
ROPE_BASE = 10000.0
S5_W = 256
S5_GROUP = 16
S5_GROUPS = S5_W // S5_GROUP
S5_STATE = 64
S5_DT_MIN = 1e-3
S5_DT_MAX = 1e-1
HG_W = 256
HG_HEADS = 4
HG_DH = HG_W // HG_HEADS
HG_CHUNK = 64
HG_EXP_CLIP = 60.0
N_BRANCH = 3
SECTION_WIDTHS = (ATT_QK_W, ATT_QK_W, ATT_V_W, S5_W, HG_W, HG_W, HG_W, HG_W, HG_W, N_BRANCH * D_MODEL)
IN_W = 2 * ATT_QK_W + ATT_V_W + S5_W + 5 * HG_W + N_BRANCH * D_MODEL
N_GROUPS = 4
EXP_PER_GROUP = 8
N_EXPERTS = N_GROUPS * EXP_PER_GROUP
TOP_K = 2
D_EXPERT = 512
MOE_BLOCK = 128

kernel_name = 'hybrid_diffusion_trunk'


def rms_norm(x, g):
    xf = x.astype(jnp.float32)
    y = xf * lax.rsqrt(jnp.mean(xf * xf, axis=-1, keepdims=True) + RMS_EPS)
    return (y * g.astype(jnp.float32)).astype(x.dtype)


def project(h, w, keep):
    outs, off = [], 0
    for width, k in zip(SECTION_WIDTHS, keep):
        outs.append(h @ w[:, off:off + width] if k else None)
        off += width
    return outs


def axial_rope(n_tok):
    rows = n_tok // GRID_W
    r, cl = jnp.meshgrid(jnp.arange(rows, dtype=jnp.float32), jnp.arange(GRID_W, dtype=jnp.float32), indexing='ij')
    n_freq = ATT_DH // 4
    inv = ROPE_BASE ** (-jnp.arange(n_freq, dtype=jnp.float32) / n_freq)
    ang = jnp.concatenate([r.reshape(-1, 1) * inv, cl.reshape(-1, 1) * inv], axis=-1)
    return jnp.cos(ang), jnp.sin(ang)


def apply_rope(t, cos, sin):
    half = ATT_DH // 2
    cs = cos[None, :, None, None, :].astype(t.dtype)
    sn = sin[None, :, None, None, :].astype(t.dtype)
    t1, t2 = t[..., :half], t[..., half:]
    return jnp.concatenate([t1 * cs - t2 * sn, t1 * sn + t2 * cs], axis=-1)


def qk_heads(t, g, rope):
    bsz, n = t.shape[:2]
    t = rms_norm(t.reshape(bsz, n, ATT_HEADS, 2, ATT_DH), g)
    return t if rope is None else apply_rope(t, *rope)


def diff_softmax_attend(q, k, v, lam):
    s = jnp.einsum('bqhmd,bkhmd->bhmqk', q, k, preferred_element_type=jnp.float32) * (ATT_DH ** -0.5)
    p = jax.nn.softmax(s, axis=-1)
    a = p[:, :, 0] - lam * p[:, :, 1]
    return jnp.einsum('bhqk,bkhv->bqhv', a.astype(v.dtype), v)


def diff_attention(q_lat, k_all, v_all, lam):
    bsz, n = q_lat.shape[:2]
    nb = n // Q_BLOCK
    qb = jnp.swapaxes(q_lat.reshape(bsz, nb, Q_BLOCK, ATT_HEADS, 2, ATT_DH), 0, 1)
    ob = lax.map(lambda qq: diff_softmax_attend(qq, k_all, v_all, lam), qb)
    return jnp.swapaxes(ob, 0, 1).reshape(bsz, n, ATT_HEADS, ATT_DV)


def diff_head_out(o, g, lam_init):
    bsz, n = o.shape[:2]
    return (rms_norm(o, g) * (1.0 - lam_init)).reshape(bsz, n, ATT_V_W)


def s5_discretise(a_re, a_im, log_dt, b_re, b_im):
    lam = lax.complex(a_re.astype(jnp.float32), a_im.astype(jnp.float32))
    dt = jnp.exp(log_dt.astype(jnp.float32))[:, None]
    lam_bar = jnp.exp(lam * dt)
    bmat = lax.complex(b_re.astype(jnp.float32), b_im.astype(jnp.float32))
    b_bar = ((lam_bar - 1.0) / lam)[:, :, None] * bmat
    return lam_bar, b_bar


def s5_drive(u, b_bar):
    bsz, n = u.shape[:2]
    ug = u.astype(jnp.float32).reshape(bsz, n, S5_GROUPS, S5_GROUP)
    return lax.complex(jnp.einsum('bngh,gph->bngp', ug, jnp.real(b_bar)),
                       jnp.einsum('bngh,gph->bngp', ug, jnp.imag(b_bar)))


def diag_scan(lam_bar, bu, h0, reverse):
    if h0 is not None:
        edge = bu.shape[1] - 1 if reverse else 0
        bu = bu.at[:, edge].add(lam_bar * h0)
    a = jnp.broadcast_to(lam_bar, (1, bu.shape[1]) + lam_bar.shape)

    def combine(e1, e2):
        a1, b1 = e1
        a2, b2 = e2
        return a1 * a2, a2 * b1 + b2

    _, h = lax.associative_scan(combine, (a, bu), reverse=reverse, axis=1)
    return h


def s5_readout(h, u, c_re, c_im, d, glu_w, glu_b):
    bsz, n = u.shape[:2]
    y = (jnp.einsum('bngp,ghp->bngh', jnp.real(h), c_re.astype(jnp.float32))
         - jnp.einsum('bngp,ghp->bngh', jnp.imag(h), c_im.astype(jnp.float32)))
    y = y.reshape(bsz, n, S5_W) + d.astype(jnp.float32) * u.astype(jnp.float32)
    z = jax.nn.gelu(y)
    z = z * jax.nn.sigmoid(z @ glu_w.astype(jnp.float32) + glu_b.astype(jnp.float32))
    return z.astype(u.dtype)


def hgrn_heads(t):
    return t.astype(jnp.float32).reshape(t.shape[0], t.shape[1], HG_HEADS, HG_DH)


def hgrn_forget(fz, lb):
    fz = hgrn_heads(fz)
    lb = lb.reshape(HG_HEADS, HG_DH)
    log_f = jax.nn.log_sigmoid(fz) + jnp.log1p(lb * jnp.exp(jnp.minimum(-fz, HG_EXP_CLIP)))
    k = (1.0 - lb) * jax.nn.sigmoid(-fz)
    return k, log_f


def hgrn_chunk_scan(q, k, v, log_f, s0):
    bsz, n = q.shape[:2]
    n_chunk = n // HG_CHUNK

    def chunks(t):
        return t.reshape(bsz, n_chunk, HG_CHUNK, HG_HEADS, t.shape[-1]).transpose(1, 0, 3, 2, 4)

    lower_tri = jnp.tril(jnp.ones((HG_CHUNK, HG_CHUNK), dtype=bool))[:, :, None]

    def step(state, inp):
        qc, kc, vc, gc = inp
        b = jnp.cumsum(gc, axis=2)
        o_inter = jnp.einsum('bhtk,bhkv->bhtv', qc * jnp.exp(b), state)
        diff = b[:, :, :, None, :] - b[:, :, None, :, :]
        decay = jnp.where(lower_tri, jnp.exp(jnp.where(lower_tri, diff, 0.0)), 0.0)
        att = jnp.einsum('bhtk,bhsk,bhtsk->bhts', qc, kc, decay)
        o = o_inter + jnp.einsum('bhts,bhsv->bhtv', att, vc)
        b_last = b[:, :, -1:, :]
        new_state = (jnp.exp(b_last[:, :, 0, :, None]) * state
                     + jnp.einsum('bhsk,bhsv->bhkv', kc * jnp.exp(b_last - b), vc))
        return new_state, o

    s_fin, o = lax.scan(step, s0, (chunks(q), chunks(k), chunks(v), chunks(log_f)))
    return o.transpose(1, 0, 3, 2, 4).reshape(bsz, n, HG_HEADS, v.shape[-1]), s_fin


def hgrn_bidir(q, v, kf, gf, kb, gb, s0f, s0b):
    flip = lambda t: t[:, ::-1]
    of, sf = hgrn_chunk_scan(q, kf, v, gf, s0f)
    ob, sb = hgrn_chunk_scan(flip(q), flip(kb), flip(v), flip(gb), s0b)
    return of + flip(ob), sf, sb


def hgrn_readout(o, g, norm_g):
    bsz, n = o.shape[:2]
    y = rms_norm(o, norm_g) * jax.nn.silu(hgrn_heads(g))
    return y.reshape(bsz, n, HG_W).astype(g.dtype)


def merge_branches(y_att, y_s5, y_hg, gate_logits, w_o_att, w_o_s5, w_o_hg, w_out):
    bsz, n = y_att.shape[:2]
    g = jax.nn.sigmoid(gate_logits.reshape(bsz, n, N_BRANCH, D_MODEL))
    m = g[:, :, 0] * (y_att @ w_o_att) + g[:, :, 1] * (y_s5 @ w_o_s5) + g[:, :, 2] * (y_hg @ w_o_hg)
    return m @ w_out


def hier_moe(h, w_group, b_group, w_expert, b_expert, w_gate, w_up, w_down):
    n_tok, dm = h.shape
    lg = (h @ w_group).astype(jnp.float32) + b_group.astype(jnp.float32)
    g_sel = jnp.argmax(lg, axis=-1)
    p_g = jnp.max(jax.nn.softmax(lg, axis=-1), axis=-1)
    le = ((h @ w_expert).astype(jnp.float32) + b_expert.astype(jnp.float32)).reshape(n_tok, N_GROUPS, EXP_PER_GROUP)
    le_sel = jnp.take_along_axis(le, g_sel[:, None, None], axis=1)[:, 0]
    top_p, top_i = lax.top_k(jax.nn.softmax(le_sel, axis=-1), TOP_K)
    wts = p_g[:, None] * top_p / jnp.sum(top_p, axis=-1, keepdims=True)
    eid = (g_sel[:, None] * EXP_PER_GROUP + top_i).astype(jnp.int32)
    n_asg = n_tok * TOP_K
    n_blk = -(-n_asg // MOE_BLOCK) + N_EXPERTS
    cap = n_blk * MOE_BLOCK
    flat_e = eid.reshape(-1)
    flat_w = wts.reshape(-1)
    flat_t = jnp.repeat(jnp.arange(n_tok, dtype=jnp.int32), TOP_K)
    counts = jax.ops.segment_sum(jnp.ones((n_asg,), jnp.int32), flat_e, num_segments=N_EXPERTS)
    padded = (counts + MOE_BLOCK - 1) // MOE_BLOCK * MOE_BLOCK
    pad_end = jnp.cumsum(padded)
    pad_start = pad_end - padded
    cnt_start = jnp.cumsum(counts) - counts
    order = jnp.argsort(flat_e)
    se = flat_e[order]
    dest = pad_start[se] + jnp.arange(n_asg, dtype=jnp.int32) - cnt_start[se]
    buf_tok = jnp.full((cap,), n_tok, jnp.int32).at[dest].set(flat_t[order])
    buf_w = jnp.zeros((cap,), jnp.float32).at[dest].set(flat_w[order])
    blk_e = jnp.minimum(jnp.searchsorted(pad_end, jnp.arange(n_blk, dtype=jnp.int32) * MOE_BLOCK, side='right'),
                        N_EXPERTS - 1)
    hp = jnp.concatenate([h, jnp.zeros((1, dm), h.dtype)], axis=0)
    xb = hp[buf_tok].reshape(n_blk, MOE_BLOCK, dm)

    def expert_block(args):
        xblk, e = args
        return (jax.nn.silu(xblk @ w_gate[e]) * (xblk @ w_up[e])) @ w_down[e]

    yb = lax.map(expert_block, (xb, blk_e)).reshape(cap, dm)
    out = jnp.zeros((n_tok + 1, dm), h.dtype).at[buf_tok].add(yb * buf_w[:, None].astype(h.dtype))
    return out[:n_tok]


def setup_inputs(seed: int = 0) -> dict:
    key = jax.random.key(seed)
    keys = jax.random.split(key, 48)
    counter = [0]

    def nxt():
        k = keys[counter[0]]
        counter[0] += 1
        return k

    def nrm(shape, scale):
        return jax.random.normal(nxt(), shape, jnp.float32) * scale

    def gain(shape):
        return 1.0 + nrm(shape, 0.02)

    L, D = DEPTH, D_MODEL
    G, P, H = S5_GROUPS, S5_STATE, S5_GROUP
    x = nrm((BATCH, SEQ, D), 1.0)
    c = nrm((BATCH, D), 1.0)
    ctx = nrm((BATCH, CTX_LEN, D), 1.0)
    c_ctx = nrm((D,), 1.0)
    ada_w = nrm((L, D, 6 * D), 0.5 * D ** -0.5)
    ada_b = nrm((L, 6 * D), 0.02)
    norm_mix_g = gain((L, D))
    norm_ffn_g = gain((L, D))
    w_in = nrm((L, D, IN_W), D ** -0.5)
    q_norm_g = gain((L, ATT_DH))
    k_norm_g = gain((L, ATT_DH))
    diff_lambda = nrm((L, 4, ATT_DH), 0.1)
    subln_g = gain((L, ATT_DV))
    s5_a_re = -0.5 + nrm((L, 2, G, P), 1e-3)
    s5_a_im = np.pi * jnp.arange(P, dtype=jnp.float32) + nrm((L, 2, G, P), 1e-3)
    s5_log_dt = math.log(S5_DT_MIN) + jax.random.uniform(nxt(), (L, 2, G), jnp.float32) * (
        math.log(S5_DT_MAX) - math.log(S5_DT_MIN))
    s5_b_re = nrm((L, G, P, H), (2.0 * H) ** -0.5)
    s5_b_im = nrm((L, G, P, H), (2.0 * H) ** -0.5)
    s5_c_re = nrm((L, G, H, P), (2.0 * P) ** -0.5)
    s5_c_im = nrm((L, G, H, P), (2.0 * P) ** -0.5)
    s5_d = nrm((L, S5_W), 0.5)
    s5_glu_w = nrm((L, S5_W, S5_W), S5_W ** -0.5)
    s5_glu_b = nrm((L, S5_W), 0.02)
    hg_lb_logits = nrm((L, 2, HG_W), 0.5)
    hg_norm_g = gain((L, HG_DH))
    w_o_att = nrm((L, ATT_V_W, D), ATT_V_W ** -0.5)
    w_o_s5 = nrm((L, S5_W, D), S5_W ** -0.5)
    w_o_hg = nrm((L, HG_W, D), HG_W ** -0.5)
    w_out = nrm((L, D, D), D ** -0.5)
    moe_w_group = nrm((L, D, N_GROUPS), D ** -0.5)
    moe_b_group = nrm((L, N_GROUPS), 0.01)
    moe_w_expert = nrm((L, D, N_EXPERTS), D ** -0.5)
    moe_b_expert = nrm((L, N_EXPERTS), 0.01)
    moe_w_gate = nrm((L, N_EXPERTS, D, D_EXPERT), D ** -0.5)
    moe_w_up = nrm((L, N_EXPERTS, D, D_EXPERT), D ** -0.5)
    moe_w_down = nrm((L, N_EXPERTS, D_EXPERT, D), D_EXPERT ** -0.5)
    return {'x': x, 'c': c, 'ctx': ctx, 'c_ctx': c_ctx, 'ada_w': ada_w, 'ada_b': ada_b,
            'norm_mix_g': norm_mix_g, 'norm_ffn_g': norm_ffn_g, 'w_in': w_in,
            'q_norm_g': q_norm_g, 'k_norm_g': k_norm_g, 'diff_lambda': diff_lambda, 'subln_g': subln_g,
            's5_a_re': s5_a_re, 's5_a_im': s5_a_im, 's5_log_dt': s5_log_dt,
            's5_b_re': s5_b_re, 's5_b_im': s5_b_im, 's5_c_re': s5_c_re, 's5_c_im': s5_c_im,
            's5_d': s5_d, 's5_glu_w': s5_glu_w, 's5_glu_b': s5_glu_b,
            'hg_lb_logits': hg_lb_logits, 'hg_norm_g': hg_norm_g,
            'w_o_att': w_o_att, 'w_o_s5': w_o_s5, 'w_o_hg': w_o_hg, 'w_out': w_out,
            'moe_w_group': moe_w_group, 'moe_b_group': moe_b_group,
            'moe_w_expert': moe_w_expert, 'moe_b_expert': moe_b_expert,
            'moe_w_gate': moe_w_gate, 'moe_w_up': moe_w_up, 'moe_w_down': moe_w_down}


def reference(x, c, ctx, c_ctx, ada_w, ada_b, norm_mix_g, norm_ffn_g, w_in,
              q_norm_g, k_norm_g, diff_lambda, subln_g,
              s5_a_re, s5_a_im, s5_log_dt, s5_b_re, s5_b_im, s5_c_re, s5_c_im,
              s5_d, s5_glu_w, s5_glu_b, hg_lb_logits, hg_norm_g,
              w_o_att, w_o_s5, w_o_hg, w_out,
              moe_w_group, moe_b_group, moe_w_expert, moe_b_expert,
              moe_w_gate, moe_w_up, moe_w_down):
    bsz, n_lat, dm = x.shape
    rope = axial_rope(n_lat)
    p_lb = jax.nn.softmax(hg_lb_logits.astype(jnp.float32), axis=0)
    lower = jnp.cumsum(p_lb, axis=0) - p_lb[:1]
    c_act = jax.nn.silu(c)
    cc_act = jax.nn.silu(c_ctx)
    keep_all = (True,) * len(SECTION_WIDTHS)
    keep_ctx_last = (False, True, True, True, True, True, True, True, False, False)
    for l in range(DEPTH):
        need_ctx = l < DEPTH - 1
        mod = (c_act @ ada_w[l] + ada_b[l]).reshape(bsz, 1, 6, dm)
        mod_c = (cc_act @ ada_w[l] + ada_b[l]).reshape(1, 1, 6, dm)
        sh1, sc1, g1, sh2, sc2, g2 = (mod[:, :, i] for i in range(6))
        sh1c, sc1c, g1c, sh2c, sc2c, g2c = (mod_c[:, :, i] for i in range(6))

        hx = rms_norm(x, norm_mix_g[l]) * (1.0 + sc1) + sh1
        hc = rms_norm(ctx, norm_mix_g[l]) * (1.0 + sc1c) + sh1c
        px = project(hx, w_in[l], keep_all)
        pc = project(hc, w_in[l], keep_all if need_ctx else keep_ctx_last)

        lam_init = 0.8 - 0.6 * math.exp(-0.3 * l)
        lv = diff_lambda[l].astype(jnp.float32)
        lam = jnp.exp(jnp.sum(lv[0] * lv[1])) - jnp.exp(jnp.sum(lv[2] * lv[3])) + lam_init
        k_ctx = qk_heads(pc[1], k_norm_g[l], None)
        v_ctx = pc[2].reshape(bsz, -1, ATT_HEADS, ATT_DV)
        k_all = jnp.concatenate([k_ctx, qk_heads(px[1], k_norm_g[l], rope)], axis=1)
        v_all = jnp.concatenate([v_ctx, px[2].reshape(bsz, n_lat, ATT_HEADS, ATT_DV)], axis=1)
        q_lat = qk_heads(px[0], q_norm_g[l], rope)
        ya = diff_head_out(diff_attention(q_lat, k_all, v_all, lam), subln_g[l], lam_init)

        lam_f, b_f = s5_discretise(s5_a_re[l, 0], s5_a_im[l, 0], s5_log_dt[l, 0], s5_b_re[l], s5_b_im[l])
        lam_b, b_b = s5_discretise(s5_a_re[l, 1], s5_a_im[l, 1], s5_log_dt[l, 1], s5_b_re[l], s5_b_im[l])
        hs_cf = diag_scan(lam_f, s5_drive(pc[3], b_f), None, False)
        hs_cb = diag_scan(lam_b, s5_drive(pc[3], b_b), None, True)
        hs = (diag_scan(lam_f, s5_drive(px[3], b_f), hs_cf[:, -1], False)
              + diag_scan(lam_b, s5_drive(px[3], b_b), hs_cb[:, 0], True))
        s5_par = (s5_c_re[l], s5_c_im[l], s5_d[l], s5_glu_w[l], s5_glu_b[l])
        ys = s5_readout(hs, px[3], *s5_par)

        s0 = jnp.zeros((bsz, HG_HEADS, HG_DH, HG_DH), jnp.float32)
        kcf, gcf = hgrn_forget(pc[5], lower[l, 0])
        kcb, gcb = hgrn_forget(pc[6], lower[l, 1])
        oh_c, st_f, st_b = hgrn_bidir(jax.nn.silu(hgrn_heads(pc[4])), hgrn_heads(pc[7]), kcf, gcf, kcb, gcb, s0, s0)
        klf, glf = hgrn_forget(px[5], lower[l, 0])
        klb, glb = hgrn_forget(px[6], lower[l, 1])
        oh, _, _ = hgrn_bidir(jax.nn.silu(hgrn_heads(px[4])), hgrn_heads(px[7]), klf, glf, klb, glb, st_f, st_b)
        yh = hgrn_readout(oh, px[8], hg_norm_g[l])

        br = (w_o_att[l], w_o_s5[l], w_o_hg[l], w_out[l])
        x = x + g1 * merge_branches(ya, ys, yh, px[9], *br)
        moe_par = (moe_w_group[l], moe_b_group[l], moe_w_expert[l], moe_b_expert[l],
                   moe_w_gate[l], moe_w_up[l], moe_w_down[l])
        if need_ctx:
            q_c = qk_heads(pc[0], q_norm_g[l], None)
            ya_c = diff_head_out(diff_softmax_attend(q_c, k_ctx, v_ctx, lam), subln_g[l], lam_init)
            ys_c = s5_readout(hs_cf + hs_cb, pc[3], *s5_par)
            yh_c = hgrn_readout(oh_c, pc[8], hg_norm_g[l])
            ctx_mid = ctx + g1c * merge_branches(ya_c, ys_c, yh_c, pc[9], *br)
            hcf = rms_norm(ctx_mid, norm_ffn_g[l]) * (1.0 + sc2c) + sh2c
            ctx = ctx_mid + g2c * hier_moe(hcf.reshape(-1, dm), *moe_par).reshape(ctx_mid.shape)

        hxf = rms_norm(x, norm_ffn_g[l]) * (1.0 + sc2) + sh2
        x = x + g2 * hier_moe(hxf.reshape(-1, dm), *moe_par).reshape(x.shape)
    return x
```

```python
import math
import numpy as np
import concourse.bass as bass
import concourse.mybir as mybir
from concourse.bass_utils import run_bass_kernel_spmd

F32 = mybir.dt.float32
BF16 = mybir.dt.bfloat16
I32 = mybir.dt.int32
AF = mybir.ActivationFunctionType
ALU = mybir.AluOpType
AX = mybir.AxisListType


class Buf:
    def __init__(self, name, t):
        self.name = name
        self.t = t
        self.writers = {}
        self.reads = {}

    def ap(self):
        return self.t[:]

    def __getitem__(self, idx):
        return self.t[idx]


class _Eng:
    def __init__(self, key, eng, sem):
        self.key = key
        self.eng = eng
        self.sem = sem
        self.count = 0
        self.waited = {}


class FW:
    NDQ = 16

    def __init__(self, nc):
        self.nc = nc
        self._ctx = []
        self.E = {}
        for key, eng in (('pe', nc.tensor), ('dve', nc.vector), ('act', nc.scalar),
                         ('pool', nc.gpsimd), ('sp', nc.sync)):
            self.E[key] = _Eng(key, eng, self._sem("s_" + key))
        self.dq = [_Eng("dq%d" % i, None, self._sem("s_dq%d" % i)) for i in range(self.NDQ)]
        for d in self.dq:
            self.E[d.key] = d
        self.dq_next = 0
        self.final_dmas = []
        self.n_inst = 0

    def _enter(self, cm):
        v = cm.__enter__()
        self._ctx.append(cm)
        return v

    def _sem(self, name):
        return self._enter(self.nc.semaphore(name))

    def _uniq(self, name):
        self._uid = getattr(self, '_uid', 0) + 1
        return "%s_%d" % (name, self._uid)

    def sb(self, name, shape, dt):
        return Buf(name, self._enter(self.nc.sbuf_tensor(self._uniq(name), shape, dt)))

    def ps(self, name, shape, dt=F32):
        return Buf(name, self._enter(self.nc.psum_tensor(self._uniq(name), shape, dt)))

    def dram(self, name, shape, dt):
        return Buf(name, self.nc.dram_tensor(name, shape, dt, kind="Internal"))

    def wrap(self, name, t):
        return Buf(name, t)

    def _scale(self, key):
        return 16 if key.startswith("dq") else 1

    def _need(self, X, key, cnt):
        if X.waited.get(key, 0) >= cnt or cnt == 0:
            return
        if X.key == 'pe' and key == 'pe':
            return
        X.eng.wait_ge(self.E[key].sem, cnt * self._scale(key))
        X.waited[key] = cnt

    def _deps(self, X, reads, writes, part=False):
        for b in reads:
            for k, c in b.writers.items():
                self._need(X, k, c)
        for b in writes:
            if not part:
                for k, c in b.writers.items():
                    self._need(X, k, c)
            for k, c in b.reads.items():
                self._need(X, k, c)

    def _mark(self, key, cnt, reads, writes):
        for b in reads:
            if b.reads.get(key, 0) < cnt:
                b.reads[key] = cnt
        for b in writes:
            b.writers[key] = cnt

    def push(self):
        self._marks = getattr(self, '_marks', [])
        self._marks.append(len(self._ctx))

    def pop(self):
        self.barrier()
        n = self._marks.pop()
        while len(self._ctx) > n:
            self._ctx.pop().__exit__(None, None, None)

    def barrier(self):
        for xk in ('pe', 'dve', 'act', 'pool', 'sp'):
            X = self.E[xk]
            for yk, Y in self.E.items():
                if yk != xk:
                    self._need(X, yk, Y.count)

    def op(self, ek, fn, reads=(), writes=(), part=False):
        X = self.E[ek]
        self._deps(X, reads, writes, part)
        ins = fn(X.eng)
        X.count += 1
        ins.then_inc(X.sem, 1)
        self._mark(ek, X.count, reads, writes)
        self.n_inst += 1
        return ins

    def dma(self, qk, out, in_, reads=(), writes=(), final=False, part=True, **kw):
        Q = self.E[qk]
        D = self._pick(qk == 'pool')
        self._deps(Q, reads, writes, part)
        self._need(Q, D.key, D.count)
        ins = Q.eng.dma_start(out=out, in_=in_, **kw)
        D.count += 1
        ins.then_inc(D.sem, 16)
        self._mark(D.key, D.count, reads, writes)
        if final:
            self.final_dmas.append((D.key, D.count))
        self.n_inst += 1
        return ins

    def _pick(self, sw):
        h = self.NDQ // 2
        if sw:
            self._sw_next = (getattr(self, '_sw_next', -1) + 1) % (self.NDQ - h)
            return self.dq[h + self._sw_next]
        self._hw_next = (getattr(self, '_hw_next', -1) + 1) % h
        return self.dq[self._hw_next]

    def idma(self, fn, reads=(), writes=(), part=True):
        Q = self.E['pool']
        D = self._pick(True)
        self._deps(Q, reads, writes, part)
        self._need(Q, D.key, D.count)
        ins = fn(Q.eng)
        D.count += 1
        ins.then_inc(D.sem, 16)
        self._mark(D.key, D.count, reads, writes)
        self.n_inst += 1
        return ins

    def finish(self):
        S = self.E['sp']
        for D in self.dq:
            if D.count:
                self._need(S, D.key, D.count)
        for k in ('pe', 'dve', 'act', 'pool'):
            if self.E[k].count:
                self._need(S, k, self.E[k].count)
        while self._ctx:
            self._ctx.pop().__exit__(None, None, None)


SPARSE = True
D = 1024
IN_W = 6144
C_LEN = 256
EPS = 1e-6
TWO_PI = 2.0 * math.pi


class Prog:
    def __init__(self, nc, T, NE, debug=False, L5=256, stop=None):
        self.nc, self.T, self.NE, self.debug, self.L5, self.stop = nc, T, NE, debug, L5, stop
        self.NT = T // 128
        self.NCt = C_LEN // 128
        self.NTA = self.NT + self.NCt
        self.NTOK = T + C_LEN
        self.fw = FW(nc)
        self.din = {}
        self.scr = {}

    def inp(self, name, shape, dt=F32):
        t = self.nc.dram_tensor(name, list(shape), dt, kind="ExternalInput")
        self.din[name] = Buf(name, t)
        return self.din[name]

    def scratch(self, name, shape, dt):
        kind = "ExternalOutput" if self.debug else "Internal"
        t = self.nc.dram_tensor(name, list(shape), dt, kind=kind)
        self.scr[name] = Buf(name, t)
        return self.scr[name]

    def declare(self):
        T, NE, NT, NTOK = self.T, self.NE, self.NT, self.NTOK
        i = self.inp
        i("x", [T, D]); i("ctx", [C_LEN, D]); i("cvecT", [128, 8, 2])
        i("ada_w", [2, D, IN_W]); i("ada_b", [2, IN_W]); i("norm_mix_g", [2, D]); i("norm_ffn_g", [2, D])
        i("w_in", [2, D, IN_W]); i("q_norm_g", [2, 64]); i("k_norm_g", [2, 64]); i("diff_lambda", [2, 256])
        i("subln_g", [2, 128]); i("s5_a_re", [2, 2, 16, 64]); i("s5_a_im", [2, 2, 16, 64]); i("s5_log_dt", [2, 32])
        i("s5_b_re", [2, 16, 64, 16]); i("s5_b_im", [2, 16, 64, 16]); i("s5_c_re", [2, 256, 64]); i("s5_c_im", [2, 256, 64])
        i("s5_d", [2, 256]); i("s5_glu_w", [2, 256, 256]); i("s5_glu_b", [2, 256])
        i("hg_lb_logits", [2, 2, 256]); i("hg_norm_g", [2, 64])
        i("w_o_att", [2, 512, D]); i("w_o_s5", [2, 256, D]); i("w_o_hg", [2, 256, D]); i("w_out", [2, D, D])
        i("moe_w_group", [2, D, 4]); i("moe_b_group", [2, 4]); i("moe_w_expert", [2, D, NE]); i("moe_b_expert", [2, NE])
        i("moe_w_gate", [2, NE, D, 512]); i("moe_w_up", [2, NE, D, 512]); i("moe_w_down", [2, NE, 512, D])
        i("k_ident", [128, 128]); i("k_ropeC", [128, NT, 32]); i("k_ropeS", [128, NT, 32])
        i("k_tri", [128, 2, 64]); i("k_su", [128, 2, 64]); i("k_tribd", [128, 2, 128]); i("k_subd", [128, 2, 128]); i("k_rowmask", [128, 8]); i("k_colmask", [128, 8, 128])
        i("k_sgn", [128, 2]); i("k_p2", [128, 128]); i("k_iota", [128, 512])
        self.NBLK = (2 * NTOK) // 128 + NE
        i("k_tokid", [128, self.NTA], I32); i("k_bstart", [128, self.NBLK]); i("k_pidx", [128, 1]); i("k_slt", [128, 128])
        self.out = Buf("y", self.nc.dram_tensor("y", [T, D], F32, kind="ExternalOutput"))
        s = self.scratch
        s("modd", [2, IN_W], F32)
        s("xa", [NTOK, D], F32); s("xb", [NTOK, D], F32)
        s("QT", [4, 128, NTOK], BF16); s("KT", [4, 128, NTOK], BF16); s("V", [NTOK, 512], BF16)
        s("UT", [256, NTOK], BF16)
        s("HGtok", [NTOK, 7, 256], F32)
        s("HGfeat", [3, 256, NTOK], F32)
        s("G", [NTOK, 3 * D], BF16)
        s("YAT", [512, NTOK], BF16); s("YST", [256, NTOK], BF16); s("YHT", [256, NTOK], BF16)
        s("HXFT", [128, 8, NTOK], BF16); s("WV", [NTOK, NE], F32)
        s("S5st", [128, 64], F32)
        s("E12", [NTOK, 2 * NE], BF16); s("W12", [NTOK, 2], F32); s("HXFtok", [NTOK, D], BF16)
        s("BUFTOK", [self.NBLK * 128, 1], I32); s("YBUF", [self.NBLK * 128, D], BF16)
        if self.debug:
            s("dDST", [128, self.NTA, 2], I32); s("dIDXG", [128, 1, self.NBLK], I32); s("dBLKE", [128, self.NBLK], F32)
            s("dPEND", [128, NE], F32); s("dCNT", [128, 2 * NE], F32)

    def mm(self, out, lhsT, rhs, start, stop, R, W):
        return self.fw.op('pe', lambda e: e.matmul(out, lhsT, rhs, start=start, stop=stop), reads=R, writes=W)

    def tr(self, out, in_, ident, R, W):
        return self.fw.op('pe', lambda e: e.transpose(out, in_, ident), reads=R, writes=W)

    def act(self, out, in_, func, R, W, eng='act', **kw):
        return self.fw.op('act', lambda e: e.activation(out=out, in_=in_, func=func, **kw), reads=R, writes=W)

    def tt(self, out, in0, in1, op, R, W, eng='dve'):
        return self.fw.op(eng, lambda e: e.tensor_tensor(out=out, in0=in0, in1=in1, op=op), reads=R, writes=W)

    def ts(self, out, in0, s1, s2, op0, op1, R, W, eng='dve'):
        if op1 is None:
            return self.fw.op(eng, lambda e: e.tensor_scalar(out=out, in0=in0, scalar1=s1, scalar2=None, op0=op0), reads=R, writes=W)
        return self.fw.op(eng, lambda e: e.tensor_scalar(out=out, in0=in0, scalar1=s1, scalar2=s2, op0=op0, op1=op1), reads=R, writes=W)

    def stt(self, out, in0, sc, in1, op0, op1, R, W, eng='dve'):
        return self.fw.op(eng, lambda e: e.scalar_tensor_tensor(out=out, in0=in0, scalar=sc, in1=in1, op0=op0, op1=op1), reads=R, writes=W)

    def cp(self, out, in_, R, W, eng='dve'):
        if eng == 'act':
            return self.fw.op('act', lambda e: e.activation(out=out, in_=in_, func=AF.Copy), reads=R, writes=W)
        return self.fw.op(eng, lambda e: e.tensor_copy(out=out, in_=in_), reads=R, writes=W)

    def rstd(self, buf, n, scratch=None):
        self.ts(buf.ap(), buf.ap(), 1.0 / n, EPS, ALU.mult, ALU.add, [buf], [buf])
        self.act(buf.ap(), buf.ap(), AF.Ln, [buf], [buf])
        self.act(buf.ap(), buf.ap(), AF.Exp, [buf], [buf], scale=-0.5)

    def tok_src(self, which, l):
        raise NotImplementedError

    def load_consts(self):
        fw, din = self.fw, self.din
        self.bcreg = self.nc.gpsimd.to_reg(2 * self.NE * 128 - 1)
        self.identf = fw.sb("identf", [128, 128], F32)
        self.identb = fw.sb("identb", [128, 128], BF16)
        fw.dma('sp', self.identf.ap(), din["k_ident"].ap(), writes=[self.identf])
        fw.dma('pool', self.identb.ap(), din["k_ident"].ap(), writes=[self.identb])
        self.onesb = fw.sb("onesb", [128, 128], BF16)
        fw.op('dve', lambda e: e.memset(self.onesb.ap(), 1.0), writes=[self.onesb])
        self.cact = fw.sb("cact", [128, 8, 2], F32)
        fw.dma('sp', self.cact.ap(), din["cvecT"].ap(), writes=[self.cact])
        self.act(self.cact.ap(), self.cact.ap(), AF.Silu, [self.cact], [self.cact])

    def prologue(self, l):
        fw, din = self.fw, self.din
        fw.push()
        modd = self.scr["modd"]
        msb = fw.sb("msb", [2, IN_W], F32)
        bsb = fw.sb("bsb", [2, IN_W], F32)
        fw.dma('sp', bsb.ap(), din["ada_b"][l:l + 1, :].partition_broadcast(2) if False else din["ada_b"][l:l + 1, :].to_broadcast([2, IN_W]), writes=[bsb])
        wst = [fw.sb("adaw%d" % i, [128, 8, 512], F32) for i in range(2)]
        pp = [fw.ps("pmod%d" % i, [128, 512]) for i in range(2)]
        for nb in range(IN_W // 512):
            w = wst[nb % 2]
            p = pp[nb % 2]
            fw.dma('sp' if nb % 2 == 0 else 'act', w.ap(),
                   din["ada_w"][l, :, nb * 512:(nb + 1) * 512].rearrange("(kc p) n -> p kc n", p=128), writes=[w], part=False)
            for kc in range(8):
                self.mm(p[0:2, :], self.cact[:, kc, :], w[:, kc, :], kc == 0, kc == 7, [self.cact, w], [p])
            self.tt(msb[:, nb * 512:(nb + 1) * 512], p[0:2, :], bsb[:, nb * 512:(nb + 1) * 512], ALU.add, [p, bsb], [msb])
        fw.dma('sp', modd.ap(), msb.ap(), reads=[msb], writes=[modd], part=False)
        fw.pop()

    def load_mod(self, l, row, sect, name):
        fw = self.fw
        t = fw.sb(name, [128, D], F32)
        fw.dma('sp', t.ap(), self.scr["modd"][row:row + 1, sect * D:(sect + 1) * D].to_broadcast([128, D]),
               reads=[self.scr["modd"]], writes=[t])
        return t

    def bcast_row(self, name, ap_row, n, dt=F32, q='sp'):
        t = self.fw.sb(name, [128, n], dt)
        self.fw.dma(q, t.ap(), ap_row.to_broadcast([128, n]), writes=[t])
        return t


def _phaseA(self, l):
    fw, din, scr = self.fw, self.din, self.scr
    NCt, NTA = self.NCt, self.NTA
    fw.push()
    win = fw.sb("win", [128, 8, IN_W], BF16)
    for kc in range(8):
        fw.dma('pool', win[:, kc, :], din["w_in"][l, kc * 128:(kc + 1) * 128, :], writes=[win])
    gmix = self.bcast_row("gmix", din["norm_mix_g"][l:l + 1, :], D)
    A1, SH1 = [], []
    for row in range(2):
        sc = self.load_mod(l, row, 1, "sc1_%d" % row)
        self.stt(sc.ap(), sc.ap(), 1.0, gmix.ap(), ALU.add, ALU.mult, [sc, gmix], [sc])
        A1.append(sc)
        SH1.append(self.load_mod(l, row, 0, "sh1_%d" % row))
    gq = self.bcast_row("gq", din["q_norm_g"][l:l + 1, :], 64)
    gk = self.bcast_row("gk", din["k_norm_g"][l:l + 1, :], 64)
    ropeC = fw.sb("ropeC", [128, self.NT, 32], F32)
    ropeS = fw.sb("ropeS", [128, self.NT, 32], F32)
    fw.dma('sp', ropeC.ap(), din["k_ropeC"].ap(), writes=[ropeC])
    fw.dma('sp', ropeS.ap(), din["k_ropeS"].ap(), writes=[ropeS])
    LB = fw.sb("LB", [128, 512], F32)
    OML = fw.sb("OML", [128, 512], F32)
    LBT = fw.sb("LBT", [128, 2, 2], F32)
    OMLT = fw.sb("OMLT", [128, 2, 2], F32)
    if l == 0:
        fw.op('dve', lambda e: e.memset(LB.ap(), 0.0), writes=[LB])
        fw.op('dve', lambda e: e.memset(LBT.ap(), 0.0), writes=[LBT])
    else:
        l0 = self.bcast_row("lbl0", din["hg_lb_logits"][0:1].rearrange("o d c -> o (d c)"), 512)
        fw.dma('sp', LB.ap(), din["hg_lb_logits"][1:2].rearrange("o d c -> o (d c)").to_broadcast([128, 512]), writes=[LB])
        self.tt(LB.ap(), LB.ap(), l0.ap(), ALU.subtract, [LB, l0], [LB])
        self.act(LB.ap(), LB.ap(), AF.Sigmoid, [LB], [LB])
        l0t = fw.sb("l0t", [128, 2, 2], F32)
        fw.dma('sp', l0t.ap(), din["hg_lb_logits"][0].rearrange("d (ct p) -> p d ct", p=128), writes=[l0t], allow_slow_non_contiguous=True)
        fw.dma('sp', LBT.ap(), din["hg_lb_logits"][1].rearrange("d (ct p) -> p d ct", p=128), writes=[LBT], allow_slow_non_contiguous=True)
        self.tt(LBT.ap(), LBT.ap(), l0t.ap(), ALU.subtract, [LBT, l0t], [LBT])
        self.act(LBT.ap(), LBT.ap(), AF.Sigmoid, [LBT], [LBT])
    self.ts(OML.ap(), LB.ap(), -1.0, 1.0, ALU.mult, ALU.add, [LB], [OML])
    self.ts(OMLT.ap(), LBT.ap(), -1.0, 1.0, ALU.mult, ALU.add, [LBT], [OMLT])

    NB = 2
    xt = [fw.sb("xt%d" % i, [128, D], F32) for i in range(NB)]
    junk = fw.sb("junk", [128, D], F32)
    ssx = [fw.sb("ssx%d" % i, [128, 1], F32) for i in range(NB)]
    hx = [fw.sb("hx%d" % i, [128, D], BF16) for i in range(NB)]
    hxT = [fw.sb("hxT%d" % i, [128, 8, 128], BF16) for i in range(NB)]
    pT = fw.ps("pT", [128, 8, 128], BF16)
    pM = [fw.ps("pM%d" % i, [128, 512]) for i in range(3)]
    pF = fw.ps("pF", [128, 8, 128])
    pQ = fw.ps("pQ", [128, 4, 128], BF16)
    sq = fw.sb("sq", [128, 512], F32)
    ss8 = fw.sb("ss8", [128, 8], F32)
    qn = fw.sb("qn", [128, 512], F32)
    r1 = fw.sb("r1", [128, 8, 32], F32)
    r2 = fw.sb("r2", [128, 8, 32], F32)
    qr = [fw.sb("qr%d" % i, [128, 512], BF16) for i in range(2)]
    qTs = [fw.sb("qTs%d" % i, [128, 4, 128], BF16) for i in range(2)]
    vsb = [fw.sb("vsb%d" % i, [128, 512], BF16) for i in range(2)]
    usb = [fw.sb("usb%d" % i, [128, 2, 128], BF16) for i in range(2)]
    hgt = [fw.sb("hgt%d" % i, [128, 7, 256], F32) for i in range(2)]
    hgf = [fw.sb("hgf%d" % i, [128, 3, 2, 128], F32) for i in range(2)]
    gsb = [fw.sb("gsb%d" % i, [128, 3 * D], BF16) for i in range(2)]
    mi = 0
    for i in range(NTA):
        b = i % NB
        row = 1 if i < NCt else 0
        tok0 = i * 128
        if l == 0:
            src = din["ctx"] if i < NCt else din["x"]
            sap = src[tok0:tok0 + 128, :] if i < NCt else src[tok0 - C_LEN:tok0 - C_LEN + 128, :]
        else:
            src = scr["xb"]
            sap = src[tok0:tok0 + 128, :]
        fw.dma('sp', xt[b].ap(), sap, reads=[src], writes=[xt[b]], part=False)
        self.act(junk.ap(), xt[b].ap(), AF.Square, [xt[b]], [junk, ssx[b]], accum_out=ssx[b].ap())
        self.rstd(ssx[b], D)
        self.stt(junk.ap(), xt[b].ap(), ssx[b].ap(), A1[row].ap(), ALU.mult, ALU.mult, [xt[b], ssx[b], A1[row]], [junk])
        self.tt(hx[b].ap(), junk.ap(), SH1[row].ap(), ALU.add, [junk, SH1[row]], [hx[b]])
        for kc in range(8):
            self.tr(pT[:, kc, :], hx[b][:, kc * 128:(kc + 1) * 128], self.identb.ap(), [hx[b], self.identb], [pT])
        self.cp(hxT[b].ap(), pT.ap(), [pT], [hxT[b]], eng='act')
        H = hxT[b]

        def tokmm(c0, n):
            nonlocal mi
            p = pM[mi % 3]
            mi += 1
            for kc in range(8):
                self.mm(p[:, 0:n], H[:, kc, :], win[:, kc, c0:c0 + n], kc == 0, kc == 7, [H, win], [p])
            return p

        for sec, gbc, dst in ((0, gq, scr["QT"]), (1, gk, scr["KT"])):
            p = tokmm(sec * 512, 512)
            self.act(sq.ap(), p.ap(), AF.Square, [p], [sq])
            fw.op('dve', lambda e: e.tensor_reduce(out=ss8.ap(), in_=sq.ap().rearrange("p (a d) -> p a d", d=64),
                                                   axis=AX.X, op=ALU.add), reads=[sq], writes=[ss8])
            self.rstd(ss8, 64)
            qn3 = qn.ap().rearrange("p (a d) -> p a d", d=64)
            self.tt(qn3, p.ap().rearrange("p (a d) -> p a d", d=64),
                    ss8.ap().rearrange("p (a o) -> p a o", o=1).to_broadcast([128, 8, 64]), ALU.mult, [p, ss8], [qn])
            Q = qr[sec]
            if i < NCt:
                self.tt(Q.ap().rearrange("p (a d) -> p a d", d=64), qn3,
                        gbc.ap().rearrange("p (o d) -> p o d", o=1).to_broadcast([128, 8, 64]), ALU.mult, [qn, gbc], [Q])
            else:
                self.tt(qn3, qn3, gbc.ap().rearrange("p (o d) -> p o d", o=1).to_broadcast([128, 8, 64]), ALU.mult, [qn, gbc], [qn])
                q4 = qn.ap().rearrange("p (a m d) -> p a m d", a=8, m=2)
                Q4 = Q.ap().rearrange("p (a m d) -> p a m d", a=8, m=2)
                t1, t2 = q4[:, :, 0, :], q4[:, :, 1, :]
                ti = i - NCt
                cs = ropeC[:, ti, :].rearrange("p (o d) -> p o d", o=1).to_broadcast([128, 8, 32])
                sn = ropeS[:, ti, :].rearrange("p (o d) -> p o d", o=1).to_broadcast([128, 8, 32])
                self.tt(r1.ap(), t1, cs, ALU.mult, [qn, ropeC], [r1])
                self.tt(r2.ap(), t2, sn, ALU.mult, [qn, ropeS], [r2], eng='pool')
                self.tt(Q4[:, :, 0, :], r1.ap(), r2.ap(), ALU.subtract, [r1, r2], [Q])
                self.tt(r1.ap(), t1, sn, ALU.mult, [qn, ropeS], [r1])
                self.tt(r2.ap(), t2, cs, ALU.mult, [qn, ropeC], [r2], eng='pool')
                self.tt(Q4[:, :, 1, :], r1.ap(), r2.ap(), ALU.add, [r1, r2], [Q])
            for a in range(4):
                self.tr(pQ[:, a, :], Q[:, a * 128:(a + 1) * 128], self.identb.ap(), [Q, self.identb], [pQ])
            self.cp(qTs[sec].ap(), pQ.ap(), [pQ], [qTs[sec]], eng='act')
            fw.dma('sp', dst[:, :, tok0:tok0 + 128].rearrange("a p t -> p a t"), qTs[sec].ap(), reads=[qTs[sec]], writes=[dst])
        p = tokmm(1024, 512)
        self.cp(vsb[i % 2].ap(), p.ap(), [p], [vsb[i % 2]], eng='act')
        fw.dma('act', scr["V"][tok0:tok0 + 128, :], vsb[i % 2].ap(), reads=[vsb[i % 2]], writes=[scr["V"]])
        for j in range(8):
            c0 = 1536 + j * 128
            for kc in range(8):
                self.mm(pF[:, j, :], win[:, kc, c0:c0 + 128], H[:, kc, :], kc == 0, kc == 7, [H, win], [pF])
        U = usb[i % 2]
        self.cp(U.ap(), pF[:, 0:2, :], [pF], [U], eng='act')
        fw.dma('act', scr["UT"][:, tok0:tok0 + 128].rearrange("(ct p) t -> p ct t", p=128), U.ap(), reads=[U], writes=[scr["UT"]])
        HF = hgf[i % 2]
        self.act(HF[:, 0, :, :], pF[:, 2:4, :], AF.Silu, [pF], [HF])
        for d_ in range(2):
            for ct in range(2):
                self.act(HF[:, 1 + d_, ct, :], pF[:, 4 + 2 * d_ + ct, :], AF.Sigmoid, [pF], [HF], scale=-1.0)
                self.ts(HF[:, 1 + d_, ct, :], HF[:, 1 + d_, ct, :], OMLT[:, d_, ct:ct + 1], None, ALU.mult, None, [HF, OMLT], [HF])
        fw.dma('sp', scr["HGfeat"][:, :, tok0:tok0 + 128].rearrange("s (ct p) t -> p s ct t", p=128), HF.ap(), reads=[HF], writes=[scr["HGfeat"]])
        HT = hgt[i % 2]
        p = tokmm(1792, 256)
        self.act(HT[:, 0, :], p[:, 0:256], AF.Silu, [p], [HT])
        p = tokmm(2048, 512)
        self.act(sq.ap(), p.ap(), AF.Sigmoid, [p], [sq])
        self.tt(sq.ap(), sq.ap(), OML.ap(), ALU.mult, [sq, OML], [sq])
        self.tt(sq.ap(), sq.ap(), LB.ap(), ALU.add, [sq, LB], [sq])
        self.ts(HT[:, 1:3, :], sq.ap().rearrange("p (a d) -> p a d", d=256), -1.0, 1.0, ALU.mult, ALU.add, [sq], [HT])
        self.act(HT[:, 3:5, :], sq.ap().rearrange("p (a d) -> p a d", d=256), AF.Ln, [sq], [HT])
        p = tokmm(2560, 512)
        self.cp(HT[:, 5, :], p[:, 0:256], [p], [HT])
        self.act(HT[:, 6, :], p[:, 256:512], AF.Silu, [p], [HT])
        fw.dma('sp', scr["HGtok"][tok0:tok0 + 128, :, :], HT.ap(), reads=[HT], writes=[scr["HGtok"]])
        Gs = gsb[i % 2]
        for j in range(6):
            p = tokmm(3072 + j * 512, 512)
            self.act(Gs[:, j * 512:(j + 1) * 512], p.ap(), AF.Sigmoid, [p], [Gs])
        fw.dma('act', scr["G"][tok0:tok0 + 128, :], Gs.ap(), reads=[Gs], writes=[scr["G"]])
    fw.pop()


Prog.phaseA = _phaseA


def _phaseB(self, l):
    fw, din, scr = self.fw, self.din, self.scr
    NCt, NTA, NTOK = self.NCt, self.NTA, self.NTOK
    lam_init = 0.8 - 0.6 * math.exp(-0.3 * l)
    fw.push()
    KTr = fw.sb("KTr", [128, 4, NTOK], BF16)
    Vr = fw.sb("Vr", [128, NTA, 512], BF16)
    for a in range(4):
        fw.dma('sp' if a % 2 == 0 else 'act', KTr[:, a, :], scr["KT"][a, :, :], reads=[scr["KT"]], writes=[KTr])
    step = 8
    for k0 in range(0, NTA, step):
        k1 = min(NTA, k0 + step)
        fw.dma('sp' if (k0 // step) % 2 == 0 else 'act', Vr[:, k0:k1, :],
               scr["V"][k0 * 128:k1 * 128, :].rearrange("(kt p) c -> p kt c", p=128), reads=[scr["V"]], writes=[Vr])
    dl = self.bcast_row("dl", din["diff_lambda"][l:l + 1, :], 256)
    pr = fw.sb("pr", [128, 2, 64], F32)
    dl4 = dl.ap().rearrange("p (a b d) -> p a b d", a=2, b=2)
    self.tt(pr.ap(), dl4[:, :, 0, :], dl4[:, :, 1, :], ALU.mult, [dl], [pr])
    e2 = fw.sb("e2", [128, 2], F32)
    fw.op('dve', lambda e: e.tensor_reduce(out=e2.ap(), in_=pr.ap(), axis=AX.X, op=ALU.add), reads=[pr], writes=[e2])
    self.act(e2.ap(), e2.ap(), AF.Exp, [e2], [e2])
    nlam = fw.sb("nlam", [128, 1], F32)
    self.tt(nlam.ap(), e2[:, 1:2], e2[:, 0:1], ALU.subtract, [e2], [nlam])
    self.ts(nlam.ap(), nlam.ap(), -lam_init, None, ALU.add, None, [nlam], [nlam])
    sgs = fw.sb("sgs", [128, 1], F32)
    fw.dma('sp', sgs.ap(), din["subln_g"][l].rearrange("(p o) -> p o", o=1), writes=[sgs])
    self.ts(sgs.ap(), sgs.ap(), 1.0 - lam_init, None, ALU.mult, None, [sgs], [sgs])

    ps_s = [[fw.ps("ps_s%d%d" % (i, m), [128, 512]) for m in range(2)] for i in range(2)]
    ps_o = [fw.ps("ps_o%d" % m, [128, 512]) for m in range(2)]
    ps_z = fw.ps("ps_z0", [128, 512])
    ps_n = fw.ps("ps_n", [128, 512])
    NPB = 4
    pbuf = [[fw.sb("pb%d%d" % (i, m), [128, 512], BF16) for m in range(2)] for i in range(NPB)]
    QTb = [fw.sb("QTb%d" % i, [128, 4, 512], BF16) for i in range(2)]
    zacc = fw.sb("zacc", [128, 512], F32)
    onesf = fw.sb("onesf", [128, 128], F32)
    fw.op('pool', lambda e: e.memset(onesf.ap(), 1.0), writes=[onesf])
    rr = [fw.sb("rr%d" % m, [128, 512], F32) for m in range(2)]
    aa = [fw.sb("aa%d" % m, [128, 512], F32) for m in range(2)]
    asq = fw.sb("asq", [128, 512], BF16)
    rs = fw.sb("rs", [128, 512], F32)
    yab = [fw.sb("yab%d" % i, [128, 512], BF16) for i in range(2)]
    blocks = []
    if l == 0:
        blocks.append((0, C_LEN, 0, NCt))
    for j in range(self.T // 512):
        blocks.append((C_LEN + j * 512, 512, 0, NTA))
    cnt = 0
    ei = 0
    for bi, (q0, nq, kt0, kt1) in enumerate(blocks):
        Qb = QTb[bi % 2]
        fw.dma('sp', Qb[:, :, 0:nq], scr["QT"][:, :, q0:q0 + nq].rearrange("a p t -> p a t"), reads=[scr["QT"]], writes=[Qb], part=False)
        for h in range(4):
            def score(kt, c):
                for m in range(2):
                    base = m * 64
                    S = ps_s[c % 2][m]
                    self.mm(S[:, 0:nq], KTr[base:base + 64, h, kt * 128:(kt + 1) * 128], Qb[base:base + 64, h, 0:nq], True, True, [KTr, Qb], [S])
            score(kt0, cnt)
            for kt in range(kt0, kt1):
                if kt + 1 < kt1:
                    score(kt + 1, cnt + 1)
                for m in range(2):
                    self.act(pbuf[cnt % NPB][m][:, 0:nq], ps_s[cnt % 2][m][:, 0:nq], AF.Exp, [ps_s[cnt % 2][m]], [pbuf[cnt % NPB][m]], scale=0.125)
                for m in range(2):
                    pT = pbuf[cnt % NPB][m]
                    self.mm(ps_o[m][:, 0:nq], Vr[:, kt, h * 128:(h + 1) * 128], pT[:, 0:nq], kt == kt0, kt == kt1 - 1, [Vr, pT], [ps_o[m]])
                pT0, pT1 = pbuf[cnt % NPB]
                self.mm(ps_z[:, 0:nq], self.onesb.ap(), pT0[:, 0:nq], kt == kt0, kt == kt1 - 1, [self.onesb, pT0], [ps_z])
                if kt == kt0:
                    self.cp(zacc[:, 0:nq], pT1[:, 0:nq], [pT1], [zacc])
                else:
                    self.tt(zacc[:, 0:nq], zacc[:, 0:nq], pT1[:, 0:nq], ALU.add, [zacc, pT1], [zacc])
                cnt += 1
            self.mm(ps_n[:, 0:nq], onesf.ap(), zacc[:, 0:nq], True, True, [onesf, zacc], [ps_n])
            fw.op('dve', lambda e: e.reciprocal(out=rr[0][:, 0:nq], in_=ps_z[:, 0:nq]), reads=[ps_z], writes=[rr[0]])
            fw.op('dve', lambda e: e.reciprocal(out=rr[1][:, 0:nq], in_=ps_n[:, 0:nq]), reads=[ps_n], writes=[rr[1]])
            for m in range(2):
                self.tt(aa[m][:, 0:nq], ps_o[m][:, 0:nq], rr[m][:, 0:nq], ALU.mult, [ps_o[m], rr[m]], [aa[m]])
            self.stt(aa[0][:, 0:nq], aa[1][:, 0:nq], nlam.ap(), aa[0][:, 0:nq], ALU.mult, ALU.add, [aa[0], aa[1], nlam], [aa[0]])
            self.tt(asq[:, 0:nq], aa[0][:, 0:nq], aa[0][:, 0:nq], ALU.mult, [aa[0]], [asq], eng='pool')
            self.mm(ps_n[:, 0:nq], self.onesb.ap(), asq[:, 0:nq], True, True, [self.onesb, asq], [ps_n])
            self.ts(rs[:, 0:nq], ps_n[:, 0:nq], 1.0 / 128, EPS, ALU.mult, ALU.add, [ps_n], [rs])
            self.act(rs[:, 0:nq], rs[:, 0:nq], AF.Ln, [rs], [rs])
            self.act(rs[:, 0:nq], rs[:, 0:nq], AF.Exp, [rs], [rs], scale=-0.5)
            Y = yab[ei % 2]
            ei += 1
            self.stt(Y[:, 0:nq], aa[0][:, 0:nq], sgs.ap(), rs[:, 0:nq], ALU.mult, ALU.mult, [aa[0], sgs, rs], [Y])
            fw.dma('act', scr["YAT"][h * 128:(h + 1) * 128, q0:q0 + nq], Y[:, 0:nq], reads=[Y], writes=[scr["YAT"]])
    fw.pop()


Prog.phaseB = _phaseB


def _rev(ap):
    a = [list(x) for x in ap.ap]
    st, n = a[-1]
    a[-1] = [-st, n]
    return bass.AP(ap.tensor, ap.offset + st * (n - 1), a)


def _sincos(self, dst, ang, shape, tmpf, tmpi, want_cos):
    P = [ang, tmpf, tmpi]
    if want_cos:
        self.ts(ang.ap(), ang.ap(), math.pi / 2, None, ALU.add, None, [ang], [ang])
    self.ts(tmpf.ap(), ang.ap(), 1.0 / TWO_PI, None, ALU.mult, None, [ang], [tmpf])
    self.cp(tmpi.ap(), tmpf.ap(), [tmpf], [tmpi])
    self.cp(tmpf.ap(), tmpi.ap(), [tmpi], [tmpf])
    self.stt(ang.ap(), tmpf.ap(), -TWO_PI, ang.ap(), ALU.mult, ALU.add, [tmpf, ang], [ang])
    self.ts(tmpf.ap(), ang.ap(), math.pi, -TWO_PI, ALU.is_gt, ALU.mult, [ang], [tmpf])
    self.tt(ang.ap(), ang.ap(), tmpf.ap(), ALU.add, [ang, tmpf], [ang])
    self.ts(tmpf.ap(), ang.ap(), -math.pi, TWO_PI, ALU.is_lt, ALU.mult, [ang], [tmpf])
    self.tt(ang.ap(), ang.ap(), tmpf.ap(), ALU.add, [ang, tmpf], [ang])
    self.ts(ang.ap(), ang.ap(), math.pi, -math.pi, ALU.min, ALU.max, [ang], [ang])
    self.act(dst, ang.ap(), AF.Sin, [ang], [])


def _phaseC(self, l):
    fw, din, scr = self.fw, self.din, self.scr
    L, NTOK = self.L5, self.NTOK
    NCH = NTOK // L
    NCc = C_LEN // L
    fw.push()
    are = fw.sb("are", [128, 32], F32); aim = fw.sb("aim", [128, 32], F32); dt = fw.sb("dt", [128, 32], F32)
    for hf in range(2):
        fw.dma('sp', are[hf * 64:(hf + 1) * 64, :], din["s5_a_re"][l].rearrange("d g p -> p (d g)"), writes=[are], allow_slow_non_contiguous=True)
        fw.dma('act', aim[hf * 64:(hf + 1) * 64, :], din["s5_a_im"][l].rearrange("d g p -> p (d g)"), writes=[aim], allow_slow_non_contiguous=True)
    fw.dma('sp', dt.ap(), din["s5_log_dt"][l:l + 1, :].to_broadcast([128, 32]), writes=[dt])
    self.act(dt.ap(), dt.ap(), AF.Exp, [dt], [dt])
    th = fw.sb("th", [128, 32], F32); R = fw.sb("R", [128, 32], F32)
    self.tt(th.ap(), aim.ap(), dt.ap(), ALU.mult, [aim, dt], [th])
    self.tt(R.ap(), are.ap(), dt.ap(), ALU.mult, [are, dt], [R])
    self.act(R.ap(), R.ap(), AF.Exp, [R], [R])
    a32 = fw.sb("a32", [128, 32], F32); f32 = fw.sb("f32t", [128, 32], F32); i32 = fw.sb("i32t", [128, 32], I32)
    sn1 = fw.sb("sn1", [128, 32], F32); cs1 = fw.sb("cs1", [128, 32], F32)
    snL = fw.sb("snL", [128, 32], F32); csL = fw.sb("csL", [128, 32], F32)
    for dst, mul, wc in ((sn1, 1.0, False), (cs1, 1.0, True), (snL, float(L), False), (csL, float(L), True)):
        self.ts(a32.ap(), th.ap(), mul, None, ALU.mult, None, [th], [a32])
        _sincos(self, dst.ap(), a32, None, f32, i32, wc)
        dst.writers['act'] = fw.E['act'].count
    nre = fw.sb("nre", [128, 32], F32); nim = fw.sb("nim", [128, 32], F32); den = fw.sb("den", [128, 32], F32)
    kre = fw.sb("kre", [128, 32], F32); kim = fw.sb("kim", [128, 32], F32); tmp = fw.sb("tmp32", [128, 32], F32)
    self.tt(nre.ap(), R.ap(), cs1.ap(), ALU.mult, [R, cs1], [nre])
    self.ts(nre.ap(), nre.ap(), -1.0, None, ALU.add, None, [nre], [nre])
    self.tt(nim.ap(), R.ap(), sn1.ap(), ALU.mult, [R, sn1], [nim])
    self.tt(den.ap(), are.ap(), are.ap(), ALU.mult, [are], [den])
    self.tt(tmp.ap(), aim.ap(), aim.ap(), ALU.mult, [aim], [tmp])
    self.tt(den.ap(), den.ap(), tmp.ap(), ALU.add, [den, tmp], [den])
    fw.op('dve', lambda e: e.reciprocal(out=den.ap(), in_=den.ap()), reads=[den], writes=[den])
    self.tt(kre.ap(), nre.ap(), are.ap(), ALU.mult, [nre, are], [kre])
    self.tt(tmp.ap(), nim.ap(), aim.ap(), ALU.mult, [nim, aim], [tmp])
    self.tt(kre.ap(), kre.ap(), tmp.ap(), ALU.add, [kre, tmp], [kre])
    self.tt(kre.ap(), kre.ap(), den.ap(), ALU.mult, [kre, den], [kre])
    self.tt(kim.ap(), nim.ap(), are.ap(), ALU.mult, [nim, are], [kim])
    self.tt(tmp.ap(), nre.ap(), aim.ap(), ALU.mult, [nre, aim], [tmp])
    self.tt(kim.ap(), kim.ap(), tmp.ap(), ALU.subtract, [kim, tmp], [kim])
    self.tt(kim.ap(), kim.ap(), den.ap(), ALU.mult, [kim, den], [kim])
    Bexp = fw.sb("Bexp", [128, 2, 16, 2, 128], BF16)
    W1e = fw.sb("W1e", [128, 16, 128], BF16); W2e = fw.sb("W2e", [128, 16, 128], BF16)
    rowm = fw.sb("rowm", [128, 8], F32); colm = fw.sb("colm", [128, 8, 128], F32); sgn = fw.sb("sgn", [128, 2], F32)
    P2 = fw.sb("P2", [128, 128], F32)
    fw.dma('sp', rowm.ap(), din["k_rowmask"].ap(), writes=[rowm]); fw.dma('sp', colm.ap(), din["k_colmask"].ap(), writes=[colm])
    fw.dma('sp', sgn.ap(), din["k_sgn"].ap(), writes=[sgn]); fw.dma('sp', P2.ap(), din["k_p2"].ap(), writes=[P2])
    fw.push()
    Bre = fw.sb("Bre", [64, 16, 16], F32); Bim = fw.sb("Bim", [64, 16, 16], F32)
    fw.dma('sp', Bre.ap(), din["s5_b_re"][l].rearrange("g p h -> p g h"), writes=[Bre])
    fw.dma('act', Bim.ap(), din["s5_b_im"][l].rearrange("g p h -> p g h"), writes=[Bim])
    bb = fw.sb("bb", [64, 2, 2, 256], F32)
    t16 = fw.sb("t16", [64, 256], F32)
    pX = fw.ps("pX", [128, 8, 64])
    TT = fw.sb("TT", [128, 2, 2, 128], F32); TS = fw.sb("TS", [128, 2, 2, 128], F32)
    for d_ in range(2):
        kr = kre[0:64, d_ * 16:(d_ + 1) * 16].rearrange("p (g o) -> p g o", o=1).to_broadcast([64, 16, 16])
        ki = kim[0:64, d_ * 16:(d_ + 1) * 16].rearrange("p (g o) -> p g o", o=1).to_broadcast([64, 16, 16])
        v3 = lambda ap: ap.rearrange("p (g h) -> p g h", h=16)
        self.tt(v3(bb[:, d_, 0, :]), Bre.ap(), kr, ALU.mult, [Bre, kre], [bb])
        self.tt(v3(t16.ap()), Bim.ap(), ki, ALU.mult, [Bim, kim], [t16])
        self.tt(bb[:, d_, 0, :], bb[:, d_, 0, :], t16.ap(), ALU.subtract, [bb, t16], [bb])
        self.tt(v3(bb[:, d_, 1, :]), Bim.ap(), kr, ALU.mult, [Bim, kre], [bb])
        self.tt(v3(t16.ap()), Bre.ap(), ki, ALU.mult, [Bre, kim], [t16])
        self.tt(bb[:, d_, 1, :], bb[:, d_, 1, :], t16.ap(), ALU.add, [bb, t16], [bb])
        for ct in range(2):
            for c in range(2):
                self.tr(pX[:, ct * 2 + c, :], bb[:, d_, c, ct * 128:(ct + 1) * 128], self.identf[0:64, 0:64], [bb, self.identf], [pX])
            self.cp(TT[:, d_, ct, 0:64], pX[:, ct * 2 + 0, :], [pX], [TT])
            self.cp(TT[:, d_, ct, 64:128], pX[:, ct * 2 + 1, :], [pX], [TT])
            self.cp(TS[:, d_, ct, 0:64], pX[:, ct * 2 + 1, :], [pX], [TS])
            self.ts(TS[:, d_, ct, 64:128], pX[:, ct * 2 + 0, :], -1.0, None, ALU.mult, None, [pX], [TS])
        for g in range(16):
            self.ts(Bexp[:, d_, g, 0, :], TT[:, d_, g // 8, :], rowm[:, g % 8:g % 8 + 1], None, ALU.mult, None, [TT, rowm], [Bexp])
            self.ts(Bexp[:, d_, g, 1, :], TS[:, d_, g // 8, :], rowm[:, g % 8:g % 8 + 1], None, ALU.mult, None, [TS, rowm], [Bexp])
    CC = fw.sb("CC", [128, 2, 2, 128], F32)
    for ct in range(2):
        fw.dma('sp', CC[:, ct, 0, 0:64], din["s5_c_re"][l, ct * 128:(ct + 1) * 128, :], writes=[CC])
        fw.dma('act', CC[:, ct, 0, 64:128], din["s5_c_im"][l, ct * 128:(ct + 1) * 128, :], writes=[CC])
        fw.dma('sp', CC[:, ct, 1, 0:64], din["s5_c_im"][l, ct * 128:(ct + 1) * 128, :], writes=[CC])
        fw.dma('act', CC[:, ct, 1, 64:128], din["s5_c_re"][l, ct * 128:(ct + 1) * 128, :], writes=[CC])
    pW = fw.ps("pW", [128, 4, 128])
    WA = fw.sb("WA", [128, 2, 2, 128], F32)
    for ct in range(2):
        for vv in range(2):
            self.tr(pW[:, ct * 2 + vv, :], CC[:, ct, vv, :], self.identf.ap(), [CC, self.identf], [pW])
            self.ts(WA[:, ct, vv, :], pW[:, ct * 2 + vv, :], sgn[:, vv:vv + 1], None, ALU.mult, None, [pW, sgn], [WA])
    for g in range(16):
        self.tt(W1e[:, g, :], WA[:, g // 8, 0, :], colm[:, g % 8, :], ALU.mult, [WA, colm], [W1e])
        self.tt(W2e[:, g, :], WA[:, g // 8, 1, :], colm[:, g % 8, :], ALU.mult, [WA, colm], [W2e])
    fw.pop()
    Ctab = fw.sb("Ctab", [128, 32, L], F32); Stab = fw.sb("Stab", [128, 32, L], F32)
    iot = fw.sb("iot", [128, L], F32)
    fw.dma('sp', iot.ap(), din["k_iota"][:, 0:L], writes=[iot])
    angL = fw.sb("angL", [128, L], F32); fL = fw.sb("fL", [128, L], F32); iL = fw.sb("iL", [128, L], I32)
    for dg in range(32):
        for dst, wc in ((Stab, False), (Ctab, True)):
            self.ts(angL.ap(), iot.ap(), th[:, dg:dg + 1], None, ALU.mult, None, [iot, th], [angL])
            _sincos(self, dst[:, dg, :], angL, None, fL, iL, wc)
            dst.writers['act'] = fw.E['act'].count
    dvec = fw.sb("dvec", [128, 2], F32); glub = fw.sb("glub", [128, 2], F32)
    fw.dma('sp', dvec.ap(), din["s5_d"][l].rearrange("(ct p) -> p ct", p=128), writes=[dvec], allow_slow_non_contiguous=True)
    fw.dma('sp', glub.ap(), din["s5_glu_b"][l].rearrange("(ct p) -> p ct", p=128), writes=[glub], allow_slow_non_contiguous=True)
    gluw = fw.sb("gluw", [128, 2, 256], BF16)
    fw.dma('pool', gluw.ap(), din["s5_glu_w"][l].rearrange("(kt p) n -> p kt n", p=128), writes=[gluw])
    ginit = fw.sb("ginit", [128, 32], F32)
    fw.op('dve', lambda e: e.memset(ginit.ap(), 0.0), writes=[ginit])
    yacc = fw.sb("yacc", [128, 2, NTOK], F32)
    pBU = [fw.ps("pBU%d" % i, [128, 512]) for i in range(2)]
    pBS = [fw.ps("pBS%d" % i, [128, 512]) for i in range(2)]
    pY = [fw.ps("pY%d" % i, [128, 512]) for i in range(2)]
    pG = fw.ps("pG", [128, 512]); pZ = fw.ps("pZ", [128, 512])
    Ub = [fw.sb("Ub%d" % i, [128, 2, L], BF16) for i in range(2)]
    t1 = [fw.sb("t1_%d" % i, [128, L], F32) for i in range(2)]
    t2 = [fw.sb("t2_%d" % i, [128, L], F32) for i in range(2)]
    bp = [fw.sb("bp%d" % i, [128, L], F32) for i in range(2)]
    Gs = [fw.sb("Gs%d" % i, [128, L], F32) for i in range(2)]
    G1 = [fw.sb("G1_%d" % i, [128, L], BF16) for i in range(2)]
    G2 = [fw.sb("G2_%d" % i, [128, L], BF16) for i in range(2)]
    gtb = [fw.sb("gt%d" % i, [128, 1], F32) for i in range(2)]
    ginit_b = [Buf("ginit%d" % i, ginit.t) for i in range(32)]
    for gb_ in ginit_b:
        gb_.writers = dict(ginit.writers)
    pGb = [Buf("pGv%d" % i, pG.t) for i in range(2)]
    yy = fw.sb("yy", [128, 2, L], F32); y2 = fw.sb("y2", [128, 2, L], F32); zz = fw.sb("zz", [128, 2, L], F32)
    zb = fw.sb("zb", [128, 2, L], BF16); gg = fw.sb("gg", [128, L], F32)
    yo = [fw.sb("yo%d" % i, [128, L], BF16) for i in range(2)]
    it = 0
    for d_ in range(2):
        order = list(range(NCH)) if d_ == 0 else (list(range(NCc - 1, -1, -1)) + list(range(NCH - 1, NCc - 1, -1)))
        for ci, c in enumerate(order):
            U = Ub[ci % 2]
            fw.dma('sp', U.ap(), scr["UT"][:, c * L:(c + 1) * L].rearrange("(ct p) t -> p ct t", p=128), reads=[scr["UT"]], writes=[U], part=False)
            for ct in range(2):
                for g8 in range(8):
                    g = ct * 8 + g8
                    dg = d_ * 16 + g
                    k = it % 2
                    it += 1
                    self.mm(pBU[k][:, 0:L], Bexp[:, d_, g, 0, :], U[:, ct, :], True, True, [Bexp, U], [pBU[k]])
                    self.mm(pBS[k][:, 0:L], Bexp[:, d_, g, 1, :], U[:, ct, :], True, True, [Bexp, U], [pBS[k]])
                    i0 = pBU[k][:, 0:L] if d_ == 0 else _rev(pBU[k][:, 0:L])
                    i1 = pBS[k][:, 0:L] if d_ == 0 else _rev(pBS[k][:, 0:L])
                    self.tt(t1[k].ap(), i0, Ctab[:, dg, :], ALU.mult, [pBU[k], Ctab], [t1[k]])
                    self.tt(t2[k].ap(), i1, Stab[:, dg, :], ALU.mult, [pBS[k], Stab], [t2[k]])
                    self.tt(bp[k].ap(), t1[k].ap(), t2[k].ap(), ALU.add, [t1[k], t2[k]], [bp[k]])
                    gi = ginit_b[dg]
                    fw.op('dve', lambda e: e.tensor_tensor_scan(out=Gs[k].ap(), data0=R[:, dg:dg + 1].to_broadcast([128, L]), data1=bp[k].ap(),
                                                                 initial=ginit[:, dg:dg + 1], op0=ALU.mult, op1=ALU.add),
                          reads=[R, bp[k], gi], writes=[Gs[k]])
                    self.mm(pG[:, 2 * k:2 * k + 2], P2.ap(), Gs[k][:, L - 2:L], True, True, [P2, Gs[k]], [pGb[k]])
                    self.tt(G1[k].ap(), Gs[k].ap(), Ctab[:, dg, :], ALU.mult, [Gs[k], Ctab], [G1[k]])
                    self.tt(G2[k].ap(), Gs[k].ap(), Stab[:, dg, :], ALU.mult, [Gs[k], Stab], [G2[k]], eng='pool')
                    self.ts(gtb[k].ap(), Gs[k][:, L - 1:L], csL[:, dg:dg + 1], None, ALU.mult, None, [Gs[k], csL], [gtb[k]])
                    self.stt(ginit[:, dg:dg + 1], pG[:, 2 * k + 1:2 * k + 2], snL[:, dg:dg + 1], gtb[k].ap(), ALU.mult, ALU.add, [pGb[k], snL, gtb[k]], [gi])
                    self.mm(pY[ct][:, 0:L], W1e[:, g, :], G1[k].ap(), g8 == 0, False, [W1e, G1[k]], [pY[ct]])
                    self.mm(pY[ct][:, 0:L], W2e[:, g, :], G2[k].ap(), False, g8 == 7, [W2e, G2[k]], [pY[ct]])
                if d_ == 0:
                    self.cp(yacc[:, ct, c * L:(c + 1) * L], pY[ct][:, 0:L], [pY[ct]], [yacc], eng='act')
                else:
                    self.tt(yacc[:, ct, c * L:(c + 1) * L], yacc[:, ct, c * L:(c + 1) * L], _rev(pY[ct][:, 0:L]), ALU.add, [yacc, pY[ct]], [yacc])
            if d_ == 1:
                for ct in range(2):
                    self.stt(yy[:, ct, :], U[:, ct, :], dvec[:, ct:ct + 1], yacc[:, ct, c * L:(c + 1) * L], ALU.mult, ALU.add, [U, dvec, yacc], [yy])
                self.tt(y2.ap(), yy.ap(), yy.ap(), ALU.mult, [yy], [y2], eng='pool')
                self.ts(y2.ap(), y2.ap(), 0.044715, 1.0, ALU.mult, ALU.add, [y2], [y2])
                self.tt(y2.ap(), y2.ap(), yy.ap(), ALU.mult, [y2, yy], [y2], eng='pool')
                self.act(y2.ap(), y2.ap(), AF.Sigmoid, [y2], [y2], scale=1.5957691216057308)
                self.tt(zz.ap(), yy.ap(), y2.ap(), ALU.mult, [yy, y2], [zz])
                self.cp(zb.ap(), zz.ap(), [zz], [zb], eng='pool')
                for c2 in range(2):
                    for kt in range(2):
                        self.mm(pZ[:, 0:L], gluw[:, kt, c2 * 128:(c2 + 1) * 128], zb[:, kt, :], kt == 0, kt == 1, [gluw, zb], [pZ])
                    self.act(gg.ap(), pZ[:, 0:L], AF.Sigmoid, [pZ, glub], [gg], bias=glub[:, c2:c2 + 1])
                    Y = yo[c2]
                    self.tt(Y.ap(), zz[:, c2, :], gg.ap(), ALU.mult, [zz, gg], [Y])
                    fw.dma('act', scr["YST"][c2 * 128:(c2 + 1) * 128, c * L:(c + 1) * L], Y.ap(), reads=[Y], writes=[scr["YST"]])
    fw.pop()


Prog.phaseC = _phaseC


def _phaseD(self, l):
    fw, din, scr = self.fw, self.din, self.scr
    NCH = self.NTOK // 64
    NCc = C_LEN // 64
    fw.push()
    TRI = fw.sb("TRI", [64, 2, 64], F32); SU = fw.sb("SU", [64, 2, 64], F32)
    fw.dma('sp', TRI.ap(), din["k_tri"][0:64], writes=[TRI]); fw.dma('sp', SU.ap(), din["k_su"][0:64], writes=[SU])
    ghg = self.bcast_row("ghg", din["hg_norm_g"][l:l + 1, :], 64)
    oacc = fw.sb("oacc", [64, NCH, 256], F32)
    S = fw.sb("S", [64, 4, 64], F32); Sb = fw.sb("Sb", [64, 4, 64], BF16)
    HTb = [fw.sb("HTb%d" % i, [64, 7, 256], F32) for i in range(2)]
    HFb = [fw.sb("HFb%d" % i, [64, 3, 4, 64], F32) for i in range(2)]
    Vb = [fw.sb("Vb%d" % i, [64, 256], BF16) for i in range(2)]
    pB = fw.ps("pB", [64, 4, 64]); pE = fw.ps("pE", [64, 256]); pA = fw.ps("pA", [64, 256])
    pO1 = fw.ps("pO1", [64, 256]); pO2 = fw.ps("pO2", [64, 256]); pS = fw.ps("pS", [64, 4, 64])
    bT = fw.sb("bT", [64, 4, 64], F32); nbm = fw.sb("nbm", [64, 4], F32)
    eq = fw.sb("eq", [64, 4, 64], F32); ek = fw.sb("ek", [64, 4, 64], F32); eb = fw.sb("eb", [64, 4, 64], F32)
    qtl = fw.sb("qtl", [64, 4, 64], BF16); ktl = fw.sb("ktl", [64, 4, 64], BF16); qe = fw.sb("qe", [64, 4, 64], BF16)
    ex = fw.sb("ex", [64, 256], F32); khat = fw.sb("khat", [64, 256], BF16); attm = fw.sb("attm", [64, 256], BF16)
    o1 = fw.sb("o1", [64, 256], F32)
    for d_ in range(2):
        fw.op('dve', lambda e: e.memset(S.ap(), 0.0), writes=[S])
        fw.op('dve', lambda e: e.memset(Sb.ap(), 0.0), writes=[Sb])
        if d_ == 0:
            chunks = list(range(NCH))
        else:
            chunks = list(range(NCc - 1, -1, -1)) + list(range(NCH - 1, NCc - 1, -1))
        mid = 31 if d_ == 0 else 32
        last = 63 if d_ == 0 else 0
        for ti, c in enumerate(chunks):
            tok0 = c * 64
            HT = HTb[ti % 2]; HF = HFb[ti % 2]; V = Vb[ti % 2]
            fw.dma('sp', HT.ap(), scr["HGtok"][tok0:tok0 + 64, :, :], reads=[scr["HGtok"]], writes=[HT], part=False)
            fw.dma('act', HF.ap(), scr["HGfeat"][:, :, tok0:tok0 + 64].rearrange("s (h p) t -> p s h t", p=64), reads=[scr["HGfeat"]], writes=[HF], part=False)
            self.cp(V.ap(), HT[:, 5, :], [HT], [V], eng='pool')
            for h in range(4):
                self.mm(pB[:, h, :], HT[:, 3 + d_, h * 64:(h + 1) * 64], TRI[:, d_, :], True, True, [HT, TRI], [pB])
            self.mm(pE.ap(), SU[:, d_, :], HT[:, 3 + d_, :], True, True, [SU, HT], [pE])
            self.cp(bT.ap(), pB.ap(), [pB], [bT], eng='act')
            self.ts(nbm.ap(), bT[:, :, mid], -1.0, None, ALU.mult, None, [bT], [nbm])
            self.act(eb.ap(), bT.ap(), AF.Exp, [bT], [eb])
            for h in range(4):
                self.ts(eq[:, h, :], bT[:, h, :], nbm[:, h:h + 1], 40.0, ALU.add, ALU.min, [bT, nbm], [eq])
            self.ts(eq.ap(), eq.ap(), -40.0, None, ALU.max, None, [eq], [eq])
            self.act(ek.ap(), eq.ap(), AF.Exp, [eq], [ek], scale=-1.0)
            self.act(eq.ap(), eq.ap(), AF.Exp, [eq], [eq])
            self.tt(qtl.ap(), HF[:, 0, :, :], eq.ap(), ALU.mult, [HF, eq], [qtl])
            self.tt(ktl.ap(), HF[:, 1 + d_, :, :], ek.ap(), ALU.mult, [HF, ek], [ktl], eng='pool')
            self.tt(qe.ap(), HF[:, 0, :, :], eb.ap(), ALU.mult, [HF, eb], [qe], eng='pool')
            self.act(ex.ap(), pE.ap(), AF.Exp, [pE], [ex])
            self.tt(khat.ap(), HT[:, 1 + d_, :], ex.ap(), ALU.mult, [HT, ex], [khat])
            for h in range(4):
                self.mm(pA[:, h * 64:(h + 1) * 64], ktl[:, h, :], qtl[:, h, :], True, True, [ktl, qtl], [pA])
            self.tt(attm.ap().rearrange("p (h t) -> p h t", h=4), pA.ap().rearrange("p (h t) -> p h t", h=4),
                    TRI[:, d_, :].rearrange("p (o t) -> p o t", o=1).to_broadcast([64, 4, 64]), ALU.mult, [pA, TRI], [attm])
            for h in range(4):
                self.mm(pO1[:, h * 64:(h + 1) * 64], qe[:, h, :], Sb[:, h, :], True, True, [qe, Sb], [pO1])
            for h in range(4):
                self.mm(pO2[:, h * 64:(h + 1) * 64], attm[:, h * 64:(h + 1) * 64], V[:, h * 64:(h + 1) * 64], True, True, [attm, V], [pO2])
            self.cp(o1.ap(), pO1.ap(), [pO1], [o1], eng='act')
            if d_ == 0:
                self.tt(oacc[:, c, :], o1.ap(), pO2.ap(), ALU.add, [o1, pO2], [oacc])
            else:
                self.tt(o1.ap(), o1.ap(), pO2.ap(), ALU.add, [o1, pO2], [o1])
                self.tt(oacc[:, c, :], oacc[:, c, :], o1.ap(), ALU.add, [oacc, o1], [oacc], eng='pool')
            for h in range(4):
                self.mm(pS[:, h, :], khat[:, h * 64:(h + 1) * 64], V[:, h * 64:(h + 1) * 64], True, True, [khat, V], [pS])
            for h in range(4):
                self.stt(S[:, h, :], S[:, h, :], eb[:, h, last:last + 1], pS[:, h, :], ALU.mult, ALU.add, [S, eb, pS], [S])
            self.cp(Sb.ap(), S.ap(), [S], [Sb], eng='act')
    sqo = fw.sb("sqo", [64, 256], F32); ss4 = fw.sb("ss4", [64, 4], F32); yn = fw.sb("yn", [64, 256], F32)
    gob = [fw.sb("gob%d" % i, [64, 256], F32) for i in range(2)]
    yhb = [fw.sb("yhb%d" % i, [64, 256], BF16) for i in range(2)]
    yhT = [fw.sb("yhT%d" % i, [128, 2, 64], BF16) for i in range(2)]
    pTr = fw.ps("pTr", [128, 2, 64], BF16)
    for c in range(NCH):
        tok0 = c * 64
        go = gob[c % 2]
        fw.dma('sp', go.ap(), scr["HGtok"][tok0:tok0 + 64, 6, :], reads=[scr["HGtok"]], writes=[go], part=False)
        self.tt(sqo.ap(), oacc[:, c, :], oacc[:, c, :], ALU.mult, [oacc], [sqo], eng='pool')
        fw.op('dve', lambda e: e.tensor_reduce(out=ss4.ap(), in_=sqo.ap().rearrange("p (a d) -> p a d", d=64), axis=AX.X, op=ALU.add),
              reads=[sqo], writes=[ss4])
        self.rstd(ss4, 64)
        yn3 = yn.ap().rearrange("p (a d) -> p a d", d=64)
        self.tt(yn3, oacc[:, c, :].rearrange("p (a d) -> p a d", d=64),
                ss4.ap().rearrange("p (a o) -> p a o", o=1).to_broadcast([64, 4, 64]), ALU.mult, [oacc, ss4], [yn])
        self.tt(yn3, yn3, ghg[0:64, :].rearrange("p (o d) -> p o d", o=1).to_broadcast([64, 4, 64]), ALU.mult, [yn, ghg], [yn])
        Y = yhb[c % 2]
        self.tt(Y.ap(), yn.ap(), go.ap(), ALU.mult, [yn, go], [Y])
        for ct in range(2):
            self.tr(pTr[:, ct, :], Y[:, ct * 128:(ct + 1) * 128], self.identb[0:64, 0:64], [Y, self.identb], [pTr])
        YT = yhT[c % 2]
        self.cp(YT.ap(), pTr.ap(), [pTr], [YT], eng='act')
        fw.dma('act', scr["YHT"][:, tok0:tok0 + 64].rearrange("(ct p) t -> p ct t", p=128), YT.ap(), reads=[YT], writes=[scr["YHT"]])
    fw.pop()


Prog.phaseD = _phaseD


def _phaseE(self, l):
    fw, din, scr = self.fw, self.din, self.scr
    NCt, NTA, NE = self.NCt, self.NTA, self.NE
    NR = 4 + NE
    EPG = NE // 4
    fw.push()
    Wa = fw.sb("Wa", [128, 4, D], BF16); Ws = fw.sb("Ws", [128, 2, D], BF16); Wh = fw.sb("Wh", [128, 2, D], BF16)
    Wo = fw.sb("Wo", [128, 8, D], BF16)
    fw.dma('pool', Wa.ap(), din["w_o_att"][l].rearrange("(j p) n -> p j n", p=128), writes=[Wa])
    fw.dma('pool', Ws.ap(), din["w_o_s5"][l].rearrange("(j p) n -> p j n", p=128), writes=[Ws])
    fw.dma('pool', Wh.ap(), din["w_o_hg"][l].rearrange("(j p) n -> p j n", p=128), writes=[Wh])
    fw.dma('pool', Wo.ap(), din["w_out"][l].rearrange("(j p) n -> p j n", p=128), writes=[Wo])
    Wr = fw.sb("Wr", [128, 8, NR], F32)
    fw.dma('sp', Wr[:, :, 0:4], din["moe_w_group"][l].rearrange("(kc p) n -> p kc n", p=128), writes=[Wr])
    fw.dma('sp', Wr[:, :, 4:NR], din["moe_w_expert"][l].rearrange("(kc p) n -> p kc n", p=128), writes=[Wr])
    br = fw.sb("br", [128, NR], F32)
    fw.dma('sp', br[:, 0:4], din["moe_b_group"][l:l + 1, :].to_broadcast([128, 4]), writes=[br])
    fw.dma('sp', br[:, 4:NR], din["moe_b_expert"][l:l + 1, :].to_broadcast([128, NE]), writes=[br])
    gffn = self.bcast_row("gffn", din["norm_ffn_g"][l:l + 1, :], D)
    G1m, A2, SH2 = [], [], []
    for row in range(2 if l == 0 else 1):
        G1m.append(self.load_mod(l, row, 2, "g1m_%d" % row))
        sc = self.load_mod(l, row, 4, "sc2_%d" % row)
        self.stt(sc.ap(), sc.ap(), 1.0, gffn.ap(), ALU.add, ALU.mult, [sc, gffn], [sc])
        A2.append(sc)
        SH2.append(self.load_mod(l, row, 3, "sh2_%d" % row))
    ya = [fw.sb("ya%d" % i, [128, 4, 128], BF16) for i in range(2)]
    ys = [fw.sb("ys%d" % i, [128, 2, 128], BF16) for i in range(2)]
    yh = [fw.sb("yh%d" % i, [128, 2, 128], BF16) for i in range(2)]
    gb = [fw.sb("gb%d" % i, [128, 3 * D], BF16) for i in range(2)]
    xt = [fw.sb("xe%d" % i, [128, D], F32) for i in range(2)]
    x1 = [fw.sb("x1_%d" % i, [128, D], F32) for i in range(2)]
    m1 = fw.sb("m1", [128, 512], F32); m2 = fw.sb("m2", [128, 512], F32); m3 = fw.sb("m3", [128, 512], F32)
    mb = fw.sb("mb", [128, D], BF16); mT = fw.sb("mT", [128, 8, 128], BF16)
    junk = fw.sb("junkE", [128, D], F32); ssx = fw.sb("ssxE", [128, 1], F32)
    hxf = fw.sb("hxf", [128, D], F32); hT = fw.sb("hT", [128, 8, 128], F32)
    hTb = [fw.sb("hTb%d" % i, [128, 8, 128], BF16) for i in range(2)]
    lg = fw.sb("lg", [128, NR], F32); gmx = fw.sb("gmx", [128, 4], F32); ge = fw.sb("ge", [128, 4], F32)
    gmask = fw.sb("gmask", [128, 4], F32); pe = fw.sb("pe", [128, NE], F32); pe2 = fw.sb("pe2", [128, NE], F32)
    e1 = fw.sb("e1m", [128, NE], F32); e2 = fw.sb("e2m", [128, NE], F32)
    sc4 = fw.sb("sc4", [128, 8], F32)
    wvb = [fw.sb("wvb%d" % i, [128, NE], F32) for i in range(2)]
    e12b = [fw.sb("e12b%d" % i, [128, 2 * NE], BF16) for i in range(2)]
    w12b = [fw.sb("w12b%d" % i, [128, 2], F32) for i in range(2)]
    hxfb = [fw.sb("hxfb%d" % i, [128, D], BF16) for i in range(2)]
    pA_ = fw.ps("pEa", [128, 512]); pS_ = fw.ps("pEs", [128, 512]); pH_ = fw.ps("pEh", [128, 512])
    pT = fw.ps("pEt", [128, 8, 128], BF16); pO = [fw.ps("pEo%d" % i, [128, 512]) for i in range(2)]
    pTf = fw.ps("pEtf", [128, 8, 128]);
    tiles = list(range(NTA)) if l == 0 else list(range(NCt, NTA))
    for ti, i in enumerate(tiles):
        b = ti % 2
        row = 1 if i < NCt else 0
        tok0 = i * 128
        fw.dma('sp', ya[b].ap(), scr["YAT"][:, tok0:tok0 + 128].rearrange("(j p) t -> p j t", p=128), reads=[scr["YAT"]], writes=[ya[b]], part=False)
        fw.dma('act', ys[b].ap(), scr["YST"][:, tok0:tok0 + 128].rearrange("(j p) t -> p j t", p=128), reads=[scr["YST"]], writes=[ys[b]], part=False)
        fw.dma('act', yh[b].ap(), scr["YHT"][:, tok0:tok0 + 128].rearrange("(j p) t -> p j t", p=128), reads=[scr["YHT"]], writes=[yh[b]], part=False)
        fw.dma('sp', gb[b].ap(), scr["G"][tok0:tok0 + 128, :], reads=[scr["G"]], writes=[gb[b]], part=False)
        if l == 0:
            src = din["ctx"] if i < NCt else din["x"]
            sap = src[tok0:tok0 + 128, :] if i < NCt else src[tok0 - C_LEN:tok0 - C_LEN + 128, :]
        else:
            src = scr["xb"]; sap = src[tok0:tok0 + 128, :]
        fw.dma('sp', xt[b].ap(), sap, reads=[src], writes=[xt[b]], part=False)
        for half in range(2):
            c0 = half * 512
            for j in range(4):
                self.mm(pA_.ap(), ya[b][:, j, :], Wa[:, j, c0:c0 + 512], j == 0, j == 3, [ya[b], Wa], [pA_])
            for j in range(2):
                self.mm(pS_.ap(), ys[b][:, j, :], Ws[:, j, c0:c0 + 512], j == 0, j == 1, [ys[b], Ws], [pS_])
            for j in range(2):
                self.mm(pH_.ap(), yh[b][:, j, :], Wh[:, j, c0:c0 + 512], j == 0, j == 1, [yh[b], Wh], [pH_])
            self.tt(m1.ap(), pA_.ap(), gb[b][:, c0:c0 + 512], ALU.mult, [pA_, gb[b]], [m1])
            self.tt(m2.ap(), pS_.ap(), gb[b][:, D + c0:D + c0 + 512], ALU.mult, [pS_, gb[b]], [m2])
            self.tt(m3.ap(), pH_.ap(), gb[b][:, 2 * D + c0:2 * D + c0 + 512], ALU.mult, [pH_, gb[b]], [m3])
            self.tt(m1.ap(), m1.ap(), m2.ap(), ALU.add, [m1, m2], [m1])
            self.tt(mb[:, c0:c0 + 512], m1.ap(), m3.ap(), ALU.add, [m1, m3], [mb])
        for kc in range(8):
            self.tr(pT[:, kc, :], mb[:, kc * 128:(kc + 1) * 128], self.identb.ap(), [mb, self.identb], [pT])
        self.cp(mT.ap(), pT.ap(), [pT], [mT], eng='act')
        X1 = x1[b]
        for half in range(2):
            c0 = half * 512
            for kc in range(8):
                self.mm(pO[half].ap(), mT[:, kc, :], Wo[:, kc, c0:c0 + 512], kc == 0, kc == 7, [mT, Wo], [pO[half]])
            self.tt(m1.ap(), pO[half].ap(), G1m[row][:, c0:c0 + 512], ALU.mult, [pO[half], G1m[row]], [m1])
            self.tt(X1[:, c0:c0 + 512], m1.ap(), xt[b][:, c0:c0 + 512], ALU.add, [m1, xt[b]], [X1])
        fw.dma('sp', scr["xa"][tok0:tok0 + 128, :], X1.ap(), reads=[X1], writes=[scr["xa"]])
        self.act(junk.ap(), X1.ap(), AF.Square, [X1], [junk, ssx], accum_out=ssx.ap())
        self.rstd(ssx, D)
        self.stt(junk.ap(), X1.ap(), ssx.ap(), A2[row].ap(), ALU.mult, ALU.mult, [X1, ssx, A2[row]], [junk])
        self.tt(hxf.ap(), junk.ap(), SH2[row].ap(), ALU.add, [junk, SH2[row]], [hxf])
        for kc in range(8):
            self.tr(pTf[:, kc, :], hxf[:, kc * 128:(kc + 1) * 128], self.identf.ap(), [hxf, self.identf], [pTf])
        self.cp(hT.ap(), pTf.ap(), [pTf], [hT], eng='act')
        self.cp(hTb[b].ap(), hT.ap(), [hT], [hTb[b]], eng='act')
        fw.dma('act', scr["HXFT"][:, :, tok0:tok0 + 128], hTb[b].ap(), reads=[hTb[b]], writes=[scr["HXFT"]])
        pR = pA_
        for kc in range(8):
            self.mm(pR[:, 0:NR], hT[:, kc, :], Wr[:, kc, :], kc == 0, kc == 7, [hT, Wr], [pR])
        self.tt(lg.ap(), pR[:, 0:NR], br.ap(), ALU.add, [pR, br], [lg])
        fw.op('dve', lambda e: e.tensor_reduce(out=sc4[:, 0:1], in_=lg[:, 0:4], axis=AX.X, op=ALU.max), reads=[lg], writes=[sc4])
        self.ts(sc4[:, 1:2], sc4[:, 0:1], -1.0, None, ALU.mult, None, [sc4], [sc4])
        self.act(ge.ap(), lg[:, 0:4], AF.Exp, [lg, sc4], [ge], bias=sc4[:, 1:2])
        fw.op('dve', lambda e: e.tensor_reduce(out=sc4[:, 2:3], in_=ge.ap(), axis=AX.X, op=ALU.add), reads=[ge], writes=[sc4])
        fw.op('dve', lambda e: e.reciprocal(out=sc4[:, 2:3], in_=sc4[:, 2:3]), reads=[sc4], writes=[sc4])
        self.ts(gmask.ap(), lg[:, 0:4], sc4[:, 0:1], None, ALU.is_ge, None, [lg, sc4], [gmask])
        fw.op('dve', lambda e: e.tensor_reduce(out=sc4[:, 3:4], in_=lg[:, 4:NR], axis=AX.X, op=ALU.max), reads=[lg], writes=[sc4])
        self.ts(sc4[:, 3:4], sc4[:, 3:4], -1.0, None, ALU.mult, None, [sc4], [sc4])
        self.act(pe.ap(), lg[:, 4:NR], AF.Exp, [lg, sc4], [pe], bias=sc4[:, 3:4])
        self.tt(pe.ap().rearrange("p (g e) -> p g e", g=4), pe.ap().rearrange("p (g e) -> p g e", g=4),
                gmask.ap().rearrange("p (g o) -> p g o", o=1).to_broadcast([128, 4, EPG]), ALU.mult, [pe, gmask], [pe])
        fw.op('dve', lambda e: e.tensor_reduce(out=sc4[:, 4:5], in_=pe.ap(), axis=AX.X, op=ALU.max), reads=[pe], writes=[sc4])
        self.ts(e1.ap(), pe.ap(), sc4[:, 4:5], None, ALU.is_ge, None, [pe, sc4], [e1])
        self.tt(pe2.ap(), e1.ap(), pe.ap(), ALU.mult, [e1, pe], [pe2])
        self.tt(pe2.ap(), pe.ap(), pe2.ap(), ALU.subtract, [pe, pe2], [pe2])
        fw.op('dve', lambda e: e.tensor_reduce(out=sc4[:, 5:6], in_=pe2.ap(), axis=AX.X, op=ALU.max), reads=[pe2], writes=[sc4])
        self.ts(e2.ap(), pe2.ap(), sc4[:, 5:6], None, ALU.is_ge, None, [pe2, sc4], [e2])
        self.tt(sc4[:, 6:7], sc4[:, 4:5], sc4[:, 5:6], ALU.add, [sc4], [sc4])
        fw.op('dve', lambda e: e.reciprocal(out=sc4[:, 6:7], in_=sc4[:, 6:7]), reads=[sc4], writes=[sc4])
        self.tt(sc4[:, 6:7], sc4[:, 6:7], sc4[:, 2:3], ALU.mult, [sc4], [sc4])
        self.tt(sc4[:, 7:8], sc4[:, 5:6], sc4[:, 6:7], ALU.mult, [sc4], [sc4])
        self.tt(sc4[:, 6:7], sc4[:, 4:5], sc4[:, 6:7], ALU.mult, [sc4], [sc4])
        WVt = wvb[b]
        self.ts(WVt.ap(), e1.ap(), sc4[:, 6:7], None, ALU.mult, None, [e1, sc4], [WVt])
        self.stt(WVt.ap(), e2.ap(), sc4[:, 7:8], WVt.ap(), ALU.mult, ALU.add, [e2, sc4, WVt], [WVt])
        fw.dma('sp', scr["WV"][tok0:tok0 + 128, :], WVt.ap(), reads=[WVt], writes=[scr["WV"]])
        E12t = e12b[b]
        self.cp(E12t[:, 0:NE], e1.ap(), [e1], [E12t], eng='pool')
        self.cp(E12t[:, NE:2 * NE], e2.ap(), [e2], [E12t], eng='pool')
        fw.dma('sp', scr["E12"][tok0:tok0 + 128, :], E12t.ap(), reads=[E12t], writes=[scr["E12"]])
        self.cp(w12b[b].ap(), sc4[:, 6:8], [sc4], [w12b[b]], eng='pool')
        fw.dma('sp', scr["W12"][tok0:tok0 + 128, :], w12b[b].ap(), reads=[w12b[b]], writes=[scr["W12"]])
        self.cp(hxfb[b].ap(), hxf.ap(), [hxf], [hxfb[b]], eng='act')
        fw.dma('act', scr["HXFtok"][tok0:tok0 + 128, :], hxfb[b].ap(), reads=[hxfb[b]], writes=[scr["HXFtok"]])
    fw.pop()


def _phaseF(self, l):
    fw, din, scr = self.fw, self.din, self.scr
    NCt, NTA, NE = self.NCt, self.NTA, self.NE
    fw.push()
    G2m = [self.load_mod(l, row, 5, "g2m_%d" % row) for row in range(2 if l == 0 else 1)]
    GT = 8
    groups = []
    if l == 0:
        groups.append(list(range(NCt)))
    for t0 in range(NCt, NTA, GT):
        groups.append(list(range(t0, min(NTA, t0 + GT))))
    hx = fw.sb("hxF", [128, 8, GT * 128], BF16)
    wv = fw.sb("wvF", [128, GT, NE], F32)
    acc = fw.sb("accF", [128, GT, D], F32)
    Wg = [fw.sb("Wg%d" % i, [128, 8, 512], BF16) for i in range(2)]
    Wu = [fw.sb("Wu%d" % i, [128, 8, 512], BF16) for i in range(2)]
    Wd = [fw.sb("Wd%d" % i, [128, 4, D], BF16) for i in range(2)]
    actb = [fw.sb("actb%d" % i, [128, 4, 512], BF16) for i in range(2)]
    sg = [fw.sb("sgF%d" % i, [128, 512], F32) for i in range(2)]
    xr = [fw.sb("xrF%d" % i, [128, D], F32) for i in range(2)]
    pG = [fw.ps("pFg%d" % i, [128, 512]) for i in range(2)]
    pU = [fw.ps("pFu%d" % i, [128, 512]) for i in range(2)]
    pY = [fw.ps("pFy%d" % i, [128, 512]) for i in range(2)]
    wi = 0; ci = 0; yi = 0; ai = 0
    for grp in groups:
        ntl = len(grp); ntok = ntl * 128; tok0 = grp[0] * 128
        fw.dma('sp', hx[:, :, 0:ntok], scr["HXFT"][:, :, tok0:tok0 + ntok], reads=[scr["HXFT"]], writes=[hx], part=False)
        fw.dma('act', wv[:, 0:ntl, :], scr["WV"][tok0:tok0 + ntok, :].rearrange("(t p) e -> p t e", p=128), reads=[scr["WV"]], writes=[wv], part=False)
        fw.op('pool', lambda e: e.memset(acc.ap(), 0.0), writes=[acc])
        for e_ in range(NE):
            k = wi % 2; wi += 1
            fw.dma('pool', Wg[k].ap(), din["moe_w_gate"][l, e_].rearrange("(kc p) n -> p kc n", p=128), writes=[Wg[k]], part=False)
            fw.dma('pool', Wu[k].ap(), din["moe_w_up"][l, e_].rearrange("(kc p) n -> p kc n", p=128), writes=[Wu[k]], part=False)
            fw.dma('pool', Wd[k].ap(), din["moe_w_down"][l, e_].rearrange("(dc p) n -> p dc n", p=128), writes=[Wd[k]], part=False)
            for tb in range((ntok + 511) // 512):
                nt = min(512, ntok - tb * 512)
                AB = actb[ai % 2]; ai += 1
                for dc in range(4):
                    g_ = pG[ci % 2]; u_ = pU[ci % 2]; s_ = sg[ci % 2]; ci += 1
                    for kc in range(8):
                        self.mm(g_[:, 0:nt], Wg[k][:, kc, dc * 128:(dc + 1) * 128], hx[:, kc, tb * 512:tb * 512 + nt], kc == 0, kc == 7, [Wg[k], hx], [g_])
                    for kc in range(8):
                        self.mm(u_[:, 0:nt], Wu[k][:, kc, dc * 128:(dc + 1) * 128], hx[:, kc, tb * 512:tb * 512 + nt], kc == 0, kc == 7, [Wu[k], hx], [u_])
                    self.act(s_[:, 0:nt], g_[:, 0:nt], AF.Silu, [g_], [s_])
                    self.tt(AB[:, dc, 0:nt], s_[:, 0:nt], u_[:, 0:nt], ALU.mult, [s_, u_], [AB])
                for ts_ in range(nt // 128):
                    tl = tb * 4 + ts_
                    for ch in range(2):
                        y_ = pY[yi % 2]; yi += 1
                        for dc in range(4):
                            self.mm(y_.ap(), AB[:, dc, ts_ * 128:(ts_ + 1) * 128], Wd[k][:, dc, ch * 512:(ch + 1) * 512], dc == 0, dc == 3, [AB, Wd[k]], [y_])
                        self.stt(acc[:, tl, ch * 512:(ch + 1) * 512], y_.ap(), wv[:, tl, e_:e_ + 1], acc[:, tl, ch * 512:(ch + 1) * 512],
                                 ALU.mult, ALU.add, [y_, wv, acc], [acc])
        for j, i in enumerate(grp):
            row = 1 if i < NCt else 0
            X = xr[j % 2]
            fw.dma('sp', X.ap(), scr["xa"][i * 128:(i + 1) * 128, :], reads=[scr["xa"]], writes=[X], part=False)
            self.tt(acc[:, j, :], acc[:, j, :], G2m[row].ap(), ALU.mult, [acc, G2m[row]], [acc], eng='pool')
            self.tt(X.ap(), X.ap(), acc[:, j, :], ALU.add, [X, acc], [X])
            if l == 0:
                fw.dma('act', scr["xb"][i * 128:(i + 1) * 128, :], X.ap(), reads=[X], writes=[scr["xb"]])
            else:
                fw.dma('act', self.out[i * 128 - C_LEN:(i + 1) * 128 - C_LEN, :], X.ap(), reads=[X], writes=[self.out], final=True)
    fw.pop()


def _phaseFs(self, l):
    fw, din, scr = self.fw, self.din, self.scr
    NCt, NTA, NE = self.NCt, self.NTA, self.NE
    IO = bass.IndirectOffsetOnAxis
    tiles = list(range(NTA)) if l == 0 else list(range(NCt, NTA))
    nt_ = len(tiles)
    t_lo = tiles[0]
    NBLK = (2 * nt_ * 128) // 128 + NE
    fw.push()
    G2m = [self.load_mod(l, row, 5, "g2m_%d" % row) for row in range(2 if l == 0 else 1)]
    onesb = self.onesb
    slt = fw.sb("slt", [128, 128], BF16)
    fw.dma('pool', slt.ap(), din["k_slt"].ap(), writes=[slt])
    tokid = fw.sb("tokid", [128, NTA], I32); bstart = fw.sb("bstart", [128, NBLK], F32); pidx = fw.sb("pidx", [128, 1], F32)
    fw.dma('sp', tokid.ap(), din["k_tokid"].ap(), writes=[tokid]); fw.dma('sp', bstart.ap(), din["k_bstart"][:, 0:NBLK], writes=[bstart])
    fw.dma('sp', pidx.ap(), din["k_pidx"].ap(), writes=[pidx])
    E12a = fw.sb("E12a", [128, nt_, 2 * NE], BF16); W12a = fw.sb("W12a", [128, nt_, 2], F32)
    fw.dma('sp', E12a.ap(), scr["E12"][t_lo * 128:(t_lo + nt_) * 128, :].rearrange("(t p) c -> p t c", p=128), reads=[scr["E12"]], writes=[E12a])
    fw.dma('sp', W12a.ap(), scr["W12"][t_lo * 128:(t_lo + nt_) * 128, :].rearrange("(t p) c -> p t c", p=128), reads=[scr["W12"]], writes=[W12a])
    zi = fw.sb("zi", [128, NBLK], I32)
    fw.op('dve', lambda e: e.memset(zi.ap(), 0), writes=[zi])
    fw.dma('sp', scr["BUFTOK"][0:NBLK * 128, :].rearrange("(p b) o -> p (b o)", p=128), zi.ap(), reads=[zi], writes=[scr["BUFTOK"]], part=False)
    IDXG = fw.sb("IDXG", [128, 1, NBLK], I32)
    DST = fw.sb("DST", [128, nt_, 2], I32)
    fw.push()
    pC = fw.ps("pC", [128, 2 * NE]); pR = [fw.ps("pR%d" % i, [128, 2 * NE]) for i in range(2)]; pT2 = [fw.ps("pT2%d" % i, [128, 2 * NE]) for i in range(2)]
    for j in range(nt_):
        self.mm(pC.ap(), onesb.ap(), E12a[:, j, :], j == 0, j == nt_ - 1, [onesb, E12a], [pC])
    cnt = fw.sb("cnt", [128, 2 * NE], F32); tot = fw.sb("tot", [128, NE], F32); toti = fw.sb("toti", [128, NE], I32)
    padd = fw.sb("padd", [128, NE], F32); pend = fw.sb("pend", [128, NE], F32); ones32 = fw.sb("ones32", [128, NE], F32)
    carry = fw.sb("carry", [128, 2 * NE], F32)
    self.cp(cnt.ap(), pC.ap(), [pC], [cnt])
    self.tt(tot.ap(), cnt[:, 0:NE], cnt[:, NE:2 * NE], ALU.add, [cnt], [tot])
    qf = fw.sb("qf", [128, NE], F32); qt = fw.sb("qt", [128, NE], F32)
    self.ts(qf.ap(), tot.ap(), 1.0 / 128, None, ALU.mult, None, [tot], [qf])
    self.cp(toti.ap(), qf.ap(), [qf], [toti])
    self.cp(qf.ap(), toti.ap(), [toti], [qf])
    self.stt(qt.ap(), qf.ap(), 128.0, tot.ap(), ALU.mult, ALU.is_lt, [qf, tot], [qt])
    self.tt(qf.ap(), qf.ap(), qt.ap(), ALU.add, [qf, qt], [qf])
    self.ts(qt.ap(), qf.ap(), -1.0, 128.0, ALU.add, ALU.mult, [qf], [qt])
    self.tt(qt.ap(), qt.ap(), tot.ap(), ALU.is_ge, [qt, tot], [qt])
    self.tt(qf.ap(), qf.ap(), qt.ap(), ALU.subtract, [qf, qt], [qf])
    self.ts(padd.ap(), qf.ap(), 128.0, None, ALU.mult, None, [qf], [padd])
    fw.op('dve', lambda e: e.memset(ones32.ap(), 1.0), writes=[ones32])
    fw.op('dve', lambda e: e.tensor_tensor_scan(out=pend.ap(), data0=ones32.ap(), data1=padd.ap(), initial=0.0, op0=ALU.mult, op1=ALU.add),
          reads=[ones32, padd], writes=[pend])
    self.tt(carry[:, 0:NE], pend.ap(), padd.ap(), ALU.subtract, [pend, padd], [carry])
    self.tt(carry[:, NE:2 * NE], carry[:, 0:NE], cnt[:, 0:NE], ALU.add, [carry, cnt], [carry])
    blke = fw.sb("blke", [128, NBLK], F32); tmpb = fw.sb("tmpb", [128, NBLK], F32)
    fw.op('dve', lambda e: e.memset(blke.ap(), 0.0), writes=[blke])
    for e_ in range(NE):
        self.ts(tmpb.ap(), bstart.ap(), pend[:, e_:e_ + 1], None, ALU.is_ge, None, [bstart, pend], [tmpb])
        self.tt(blke.ap(), blke.ap(), tmpb.ap(), ALU.add, [blke, tmpb], [blke])
    self.ts(blke.ap(), blke.ap(), float(NE - 1), None, ALU.min, None, [blke], [blke])
    same2 = fw.sb("same2", [128, NBLK], F32)
    fw.op('dve', lambda e: e.memset(same2.ap(), 0.0), writes=[same2])
    self.tt(same2[:, 1:NBLK], blke[:, 1:NBLK], blke[:, 0:NBLK - 1], ALU.is_equal, [blke], [same2])
    self.ts(tmpb.ap(), blke.ap(), float(l * NE), 128.0, ALU.add, ALU.mult, [blke], [tmpb])
    self.ts(tmpb.ap(), tmpb.ap(), pidx.ap(), None, ALU.add, None, [tmpb, pidx], [tmpb])
    self.stt(tmpb.ap(), same2.ap(), 1.0e6, tmpb.ap(), ALU.mult, ALU.add, [same2, tmpb], [tmpb])
    self.cp(IDXG[:, 0, :], tmpb.ap(), [tmpb], [IDXG])
    rank = fw.sb("rank", [128, 2 * NE], F32); dstf = fw.sb("dstf", [128, 2], F32)
    for j in range(nt_):
        k = j % 2
        self.mm(pR[k].ap(), slt.ap(), E12a[:, j, :], True, True, [slt, E12a], [pR[k]])
        self.mm(pT2[k].ap(), onesb.ap(), E12a[:, j, :], True, True, [onesb, E12a], [pT2[k]])
        self.tt(rank.ap(), pR[k].ap(), carry.ap(), ALU.add, [pR[k], carry], [rank])
        self.tt(rank.ap(), rank.ap(), E12a[:, j, :], ALU.mult, [rank, E12a], [rank])
        fw.op('dve', lambda e: e.tensor_reduce(out=dstf.ap(), in_=rank.ap().rearrange("p (a e) -> p a e", a=2), axis=AX.X, op=ALU.add),
              reads=[rank], writes=[dstf])
        self.cp(DST[:, j, :], dstf.ap(), [dstf], [DST])
        self.tt(carry.ap(), carry.ap(), pT2[k].ap(), ALU.add, [carry, pT2[k]], [carry])
        for a in range(2):
            if self.stop == "F0":
                continue
            fw.idma(lambda e: e.indirect_dma_start(out=scr["BUFTOK"][:, :], out_offset=IO(ap=DST[:, j, a:a + 1], axis=0),
                                                   in_=tokid[:, tiles[j]:tiles[j] + 1], in_offset=None),
                    reads=[DST, tokid], writes=[scr["BUFTOK"]], part=(j > 0 or a > 0))
    if self.stop == "F0":
        fw.dma('sp', scr["dDST"][:, 0:nt_, :], DST.ap(), reads=[DST], writes=[scr["dDST"]])
        fw.dma('sp', scr["dIDXG"][:, :, 0:NBLK], IDXG.ap(), reads=[IDXG], writes=[scr["dIDXG"]])
        fw.dma('sp', scr["dBLKE"][:, 0:NBLK], blke.ap(), reads=[blke], writes=[scr["dBLKE"]])
        fw.dma('sp', scr["dPEND"].ap(), pend.ap(), reads=[pend], writes=[scr["dPEND"]])
        fw.dma('sp', scr["dCNT"].ap(), cnt.ap(), reads=[cnt], writes=[scr["dCNT"]])
        fw.pop(); fw.pop()
        return
    fw.pop()
    if self.stop == "F1":
        fw.pop()
        return
    tki = [fw.sb("tki%d" % i, [128, 1], I32) for i in range(2)]
    xg = [fw.sb("xg%d" % i, [128, D], BF16) for i in range(2)]
    xgT = [fw.sb("xgT%d" % i, [128, 8, 128], BF16) for i in range(2)]
    Wg = [fw.sb("Wg%d" % i, [128, 8, 512], BF16) for i in range(2)]
    Wu = [fw.sb("Wu%d" % i, [128, 8, 512], BF16) for i in range(2)]
    Wd = [fw.sb("Wd%d" % i, [128, 4, D], BF16) for i in range(2)]
    sgt = [fw.sb("sgt%d" % i, [128, 512], F32) for i in range(2)]
    ab = [fw.sb("ab%d" % i, [128, 512], BF16) for i in range(2)]
    aT = [fw.sb("aT%d" % i, [128, 4, 128], BF16) for i in range(2)]
    yb = [fw.sb("ybF%d" % i, [128, D], BF16) for i in range(2)]
    pX = fw.ps("pFx", [128, 8, 128], BF16); pGt = fw.ps("pFg", [128, 512]); pUt = fw.ps("pFu", [128, 512])
    pAT = fw.ps("pFa", [128, 4, 128], BF16); pY = [fw.ps("pFy%d" % i, [128, 512]) for i in range(2)]
    wgate = din["moe_w_gate"].ap().rearrange("l e (p kc) n -> (l e p) (kc n)", kc=8)
    wup = din["moe_w_up"].ap().rearrange("l e (p kc) n -> (l e p) (kc n)", kc=8)
    wdown = din["moe_w_down"].ap().rearrange("l e (p dc) n -> (l e p) (dc n)", dc=4)
    for b in range(NBLK):
        k = b % 2
        fw.dma('sp', tki[k].ap(), scr["BUFTOK"][b * 128:(b + 1) * 128, :], reads=[scr["BUFTOK"]], writes=[tki[k]], part=False)
        fw.idma(lambda e: e.indirect_dma_start(out=xg[k].ap(), out_offset=None, in_=scr["HXFtok"][:, :], in_offset=IO(ap=tki[k].ap(), axis=0)),
                reads=[tki[k], scr["HXFtok"]], writes=[xg[k]], part=False)
        BC = self.bcreg
        ix = IDXG[:, 0, b:b + 1]
        fw.idma(lambda e: e.indirect_dma_start(out=Wg[0].ap().rearrange("p k n -> p (k n)"), out_offset=None, in_=wgate, in_offset=IO(ap=ix, axis=0),
                                               bounds_check=BC, oob_is_err=False), reads=[IDXG], writes=[Wg[0]], part=False)
        fw.idma(lambda e: e.indirect_dma_start(out=Wu[0].ap().rearrange("p k n -> p (k n)"), out_offset=None, in_=wup, in_offset=IO(ap=ix, axis=0),
                                               bounds_check=BC, oob_is_err=False), reads=[IDXG], writes=[Wu[0]], part=False)
        fw.idma(lambda e: e.indirect_dma_start(out=Wd[0].ap().rearrange("p k n -> p (k n)"), out_offset=None, in_=wdown, in_offset=IO(ap=ix, axis=0),
                                               bounds_check=BC, oob_is_err=False), reads=[IDXG], writes=[Wd[0]], part=False)
        for kc in range(8):
            self.tr(pX[:, kc, :], xg[k].ap().rearrange("t (p kc) -> t kc p", kc=8)[:, kc, :], self.identb.ap(), [xg[k], self.identb], [pX])
        self.cp(xgT[k].ap(), pX.ap(), [pX], [xgT[k]], eng='act')
        for kc in range(8):
            self.mm(pGt.ap(), xgT[k][:, kc, :], Wg[0][:, kc, :], kc == 0, kc == 7, [xgT[k], Wg[0]], [pGt])
        for kc in range(8):
            self.mm(pUt.ap(), xgT[k][:, kc, :], Wu[0][:, kc, :], kc == 0, kc == 7, [xgT[k], Wu[0]], [pUt])
        self.act(sgt[k].ap(), pGt.ap(), AF.Silu, [pGt], [sgt[k]])
        self.tt(ab[k].ap(), sgt[k].ap(), pUt.ap(), ALU.mult, [sgt[k], pUt], [ab[k]])
        for dc in range(4):
            self.tr(pAT[:, dc, :], ab[k].ap().rearrange("t (p dc) -> t dc p", dc=4)[:, dc, :], self.identb.ap(), [ab[k], self.identb], [pAT])
        self.cp(aT[k].ap(), pAT.ap(), [pAT], [aT[k]])
        for hh in range(2):
            for dc in range(4):
                self.mm(pY[hh].ap(), aT[k][:, dc, :], Wd[0][:, dc, hh * 512:(hh + 1) * 512], dc == 0, dc == 3, [aT[k], Wd[0]], [pY[hh]])
            self.cp(yb[k][:, hh * 512:(hh + 1) * 512], pY[hh].ap(), [pY[hh]], [yb[k]], eng='act')
        fw.dma('sp', scr["YBUF"][b * 128:(b + 1) * 128, :], yb[k].ap(), reads=[yb[k]], writes=[scr["YBUF"]])
    y1 = [fw.sb("y1c%d" % i, [128, D], BF16) for i in range(2)]
    y2 = [fw.sb("y2c%d" % i, [128, D], BF16) for i in range(2)]
    ycf = [fw.sb("ycf%d" % i, [128, D], F32) for i in range(2)]
    xr = [fw.sb("xrF%d" % i, [128, D], F32) for i in range(2)]
    for j, i in enumerate(tiles):
        k = j % 2
        row = 1 if i < NCt else 0
        fw.idma(lambda e: e.indirect_dma_start(out=y1[k].ap(), out_offset=None, in_=scr["YBUF"][:, :], in_offset=IO(ap=DST[:, j, 0:1], axis=0)),
                reads=[DST, scr["YBUF"]], writes=[y1[k]], part=False)
        fw.idma(lambda e: e.indirect_dma_start(out=y2[k].ap(), out_offset=None, in_=scr["YBUF"][:, :], in_offset=IO(ap=DST[:, j, 1:2], axis=0)),
                reads=[DST, scr["YBUF"]], writes=[y2[k]], part=False)
        X = xr[k]
        fw.dma('sp', X.ap(), scr["xa"][i * 128:(i + 1) * 128, :], reads=[scr["xa"]], writes=[X], part=False)
        Yc = ycf[k]
        self.ts(Yc.ap(), y1[k].ap(), W12a[:, j, 0:1], None, ALU.mult, None, [y1[k], W12a], [Yc])
        self.stt(Yc.ap(), y2[k].ap(), W12a[:, j, 1:2], Yc.ap(), ALU.mult, ALU.add, [y2[k], W12a, Yc], [Yc])
        self.tt(Yc.ap(), Yc.ap(), G2m[row].ap(), ALU.mult, [Yc, G2m[row]], [Yc])
        self.tt(X.ap(), X.ap(), Yc.ap(), ALU.add, [X, Yc], [X])
        if l == 0:
            fw.dma('act', scr["xb"][i * 128:(i + 1) * 128, :], X.ap(), reads=[X], writes=[scr["xb"]])
        else:
            fw.dma('act', self.out[i * 128 - C_LEN:(i + 1) * 128 - C_LEN, :], X.ap(), reads=[X], writes=[self.out], final=True)
    fw.pop()


Prog.phaseFs = _phaseFs
Prog.phaseE = _phaseE
Prog.phaseF = _phaseF

def host_consts(T, NE=32):
    NT = T // 128
    k = {}
    nblk = (2 * (T + C_LEN)) // 128 + NE
    k["k_bstart"] = np.broadcast_to((np.arange(nblk, dtype=np.float32) * 128.0)[None, :], (128, nblk)).copy()
    k["k_ident"] = np.eye(128, dtype=np.float32)
    t = np.arange(T)
    n_freq = 16
    inv = (10000.0 ** (-np.arange(n_freq, dtype=np.float32) / n_freq)).astype(np.float32)
    ang = np.concatenate([(t // 64).astype(np.float32)[:, None] * inv, (t % 64).astype(np.float32)[:, None] * inv], -1)
    k["k_ropeC"] = np.ascontiguousarray(np.cos(ang).astype(np.float32).reshape(NT, 128, 32).transpose(1, 0, 2))
    k["k_ropeS"] = np.ascontiguousarray(np.sin(ang).astype(np.float32).reshape(NT, 128, 32).transpose(1, 0, 2))
    s_ = np.arange(128)[:, None] % 64
    t_ = np.arange(64)[None, :]
    tri = np.stack([(s_ <= t_), (s_ >= t_)], 1).astype(np.float32)
    su = np.stack([(s_ > t_), (s_ < t_)], 1).astype(np.float32)
    k["k_tri"], k["k_su"] = tri, su
    tb = np.zeros((128, 2, 128), np.float32); sb_ = np.zeros((128, 2, 128), np.float32)
    for hf in range(2):
        tb[hf * 64:(hf + 1) * 64, :, hf * 64:(hf + 1) * 64] = tri[hf * 64:(hf + 1) * 64]
        sb_[hf * 64:(hf + 1) * 64, :, hf * 64:(hf + 1) * 64] = su[hf * 64:(hf + 1) * 64]
    k["k_tribd"], k["k_subd"] = tb, sb_
    p = np.arange(128)
    k["k_rowmask"] = (p[:, None] // 16 == np.arange(8)[None, :]).astype(np.float32)
    cm = np.zeros((128, 8, 128), np.float32)
    for g in range(8):
        cm[:, g, g * 16:(g + 1) * 16] = 1.0
    k["k_colmask"] = cm
    sg = np.ones((128, 2), np.float32)
    sg[64:, 0] = -1.0
    sg[:, 1] = -1.0
    k["k_sgn"] = sg
    p2 = np.zeros((128, 128), np.float32)
    for m in range(64):
        p2[m + 64, m] = -1.0
        p2[m, m + 64] = 1.0
    k["k_p2"] = p2
    NTA_ = NT + C_LEN // 128
    k["k_tokid"] = (np.arange(NTA_, dtype=np.int32)[None, :] * 128 + np.arange(128, dtype=np.int32)[:, None]).astype(np.int32)
    k["k_pidx"] = np.arange(128, dtype=np.float32).reshape(128, 1)
    k["k_slt"] = (np.arange(128)[:, None] < np.arange(128)[None, :]).astype(np.float32)
    k["k_iota"] = np.broadcast_to(np.arange(512, dtype=np.float32)[None, :], (128, 512)).copy()
    return k


def core_inputs(inputs, b, T, NE):
    f = lambda a: np.ascontiguousarray(np.asarray(a, dtype=np.float32))
    m = {}
    m["x"] = f(inputs["x"][b]); m["ctx"] = f(inputs["ctx"][b])
    cv = np.stack([np.asarray(inputs["c"][b]), np.asarray(inputs["c_ctx"])], -1)
    m["cvecT"] = f(cv.reshape(8, 128, 2).transpose(1, 0, 2))
    for n in ("ada_w", "ada_b", "norm_mix_g", "norm_ffn_g", "w_in", "q_norm_g", "k_norm_g", "subln_g", "s5_a_re", "s5_a_im",
              "s5_b_re", "s5_b_im", "s5_d", "s5_glu_w", "s5_glu_b", "hg_lb_logits", "hg_norm_g", "w_o_att", "w_o_s5",
              "w_o_hg", "w_out", "moe_w_group", "moe_b_group", "moe_w_expert", "moe_b_expert", "moe_w_gate", "moe_w_up", "moe_w_down"):
        m[n] = f(inputs[n])
    m["diff_lambda"] = f(np.asarray(inputs["diff_lambda"]).reshape(2, 256))
    m["s5_log_dt"] = f(np.asarray(inputs["s5_log_dt"]).reshape(2, 32))
    m["s5_c_re"] = f(np.asarray(inputs["s5_c_re"]).reshape(2, 256, 64))
    m["s5_c_im"] = f(np.asarray(inputs["s5_c_im"]).reshape(2, 256, 64))
    return m


def build_program(T, NE, debug=False, stop=None):
    nc = bass.Bass("TRN2", target_bir_lowering=False)
    P = Prog(nc, T, NE, debug=debug, stop=stop)
    P.declare()
    P.load_consts()
    P.run()
    P.fw.finish()
    return nc, P


def _run(self):
    for l in range(2):
        self.prologue(l)
        self.phaseA(l)
        if self.stop == "A":
            return
        self.phaseB(l)
        if self.stop == "B":
            return
        self.phaseC(l)
        if self.stop == "C":
            return
        self.phaseD(l)
        if self.stop == "D":
            return
        self.phaseE(l)
        if self.stop == "E":
            return
        self.phaseFs(l) if SPARSE else self.phaseF(l)
        if self.stop in ("F", "F0", "F1"):
            return


Prog.run = _run


def run_all(inputs, T, NE, debug=False, stop=None):
    nc, P = build_program(T, NE, debug, stop)
    consts = host_consts(T, NE)
    maps = []
    for b in range(8):
        m = core_inputs(inputs, b, T, NE)
        m.update(consts)
        maps.append({k_: v for k_, v in m.items() if k_ in P.din})
    res = run_bass_kernel_spmd(nc, maps, core_ids=list(range(8)))
    return res.results


def kernel(**inputs):
    T = inputs["x"].shape[1]
    NE = inputs["moe_w_gate"].shape[1]
    res = run_all(inputs, T, NE)
    return np.stack([r["y"] for r in res], 0).astype(np.float32)
```

```python
import math
import numpy as np
import concourse.bass as bass
import concourse.mybir as mybir
from concourse.bass_utils import run_bass_kernel_spmd

F32 = mybir.dt.float32
BF16 = mybir.dt.bfloat16
I32 = mybir.dt.int32
AF = mybir.ActivationFunctionType
ALU = mybir.AluOpType
AX = mybir.AxisListType


class Buf:
    def __init__(self, name, t):
        self.name = name
        self.t = t
        self.writers = {}
        self.reads = {}

    def ap(self):
        return self.t[:]

    def __getitem__(self, idx):
        return self.t[idx]


class _Eng:
    def __init__(self, key, eng, sem):
        self.key = key
        self.eng = eng
        self.sem = sem
        self.count = 0
        self.waited = {}


class FW:
    NDQ = 16

    def __init__(self, nc):
        self.nc = nc
        self._ctx = []
        self.E = {}
        for key, eng in (('pe', nc.tensor), ('dve', nc.vector), ('act', nc.scalar),
                         ('pool', nc.gpsimd), ('sp', nc.sync)):
            self.E[key] = _Eng(key, eng, self._sem("s_" + key))
        self.dq = [_Eng("dq%d" % i, None, self._sem("s_dq%d" % i)) for i in range(self.NDQ)]
        for d in self.dq:
            self.E[d.key] = d
        self.dq_next = 0
        self.final_dmas = []
        self.n_inst = 0

    def _enter(self, cm):
        v = cm.__enter__()
        self._ctx.append(cm)
        return v

    def _sem(self, name):
        return self._enter(self.nc.semaphore(name))

    def _uniq(self, name):
        self._uid = getattr(self, '_uid', 0) + 1
        return "%s_%d" % (name, self._uid)

    def sb(self, name, shape, dt):
        return Buf(name, self._enter(self.nc.sbuf_tensor(self._uniq(name), shape, dt)))

    def ps(self, name, shape, dt=F32):
        return Buf(name, self._enter(self.nc.psum_tensor(self._uniq(name), shape, dt)))

    def dram(self, name, shape, dt):
        return Buf(name, self.nc.dram_tensor(name, shape, dt, kind="Internal"))

    def wrap(self, name, t):
        return Buf(name, t)

    def _scale(self, key):
        return 16 if key.startswith("dq") else 1

    def _need(self, X, key, cnt):
        if X.waited.get(key, 0) >= cnt or cnt == 0:
            return
        if X.key == 'pe' and key == 'pe':
            return
        X.eng.wait_ge(self.E[key].sem, cnt * self._scale(key))
        X.waited[key] = cnt

    def _deps(self, X, reads, writes, part=False):
        for b in reads:
            for k, c in b.writers.items():
                self._need(X, k, c)
        for b in writes:
            if not part:
                for k, c in b.writers.items():
                    self._need(X, k, c)
            for k, c in b.reads.items():
                self._need(X, k, c)

    def _mark(self, key, cnt, reads, writes):
        for b in reads:
            if b.reads.get(key, 0) < cnt:
                b.reads[key] = cnt
        for b in writes:
            b.writers[key] = cnt

    def push(self):
        self._marks = getattr(self, '_marks', [])
        self._marks.append(len(self._ctx))

    def pop(self):
        self.barrier()
        n = self._marks.pop()
        while len(self._ctx) > n:
            self._ctx.pop().__exit__(None, None, None)

    def barrier(self):
        for xk in ('pe', 'dve', 'act', 'pool', 'sp'):
            X = self.E[xk]
            for yk, Y in self.E.items():
                if yk != xk:
                    self._need(X, yk, Y.count)

    def op(self, ek, fn, reads=(), writes=(), part=False):
        X = self.E[ek]
        self._deps(X, reads, writes, part)
        ins = fn(X.eng)
        X.count += 1
        ins.then_inc(X.sem, 1)
        self._mark(ek, X.count, reads, writes)
        self.n_inst += 1
        return ins

    def dma(self, qk, out, in_, reads=(), writes=(), final=False, part=True, **kw):
        Q = self.E[qk]
        D = self._pick(qk == 'pool')
        self._deps(Q, reads, writes, part)
        self._need(Q, D.key, D.count)
        ins = Q.eng.dma_start(out=out, in_=in_, **kw)
        D.count += 1
        ins.then_inc(D.sem, 16)
        self._mark(D.key, D.count, reads, writes)
        if final:
            self.final_dmas.append((D.key, D.count))
        self.n_inst += 1
        return ins

    def _pick(self, sw):
        h = self.NDQ // 2
        if sw:
            self._sw_next = (getattr(self, '_sw_next', -1) + 1) % (self.NDQ - h)
            return self.dq[h + self._sw_next]
        self._hw_next = (getattr(self, '_hw_next', -1) + 1) % h
        return self.dq[self._hw_next]

    def idma(self, fn, reads=(), writes=(), part=True):
        Q = self.E['pool']
        D = self._pick(True)
        self._deps(Q, reads, writes, part)
        self._need(Q, D.key, D.count)
        ins = fn(Q.eng)
        D.count += 1
        ins.then_inc(D.sem, 16)
        self._mark(D.key, D.count, reads, writes)
        self.n_inst += 1
        return ins

    def finish(self):
        S = self.E['sp']
        for D in self.dq:
            if D.count:
                self._need(S, D.key, D.count)
        for k in ('pe', 'dve', 'act', 'pool'):
            if self.E[k].count:
                self._need(S, k, self.E[k].count)
        while self._ctx:
            self._ctx.pop().__exit__(None, None, None)


SPARSE = True
D = 1024
IN_W = 6144
C_LEN = 256
EPS = 1e-6
TWO_PI = 2.0 * math.pi


class Prog:
    def __init__(self, nc, T, NE, debug=False, L5=256, stop=None):
        self.nc, self.T, self.NE, self.debug, self.L5, self.stop = nc, T, NE, debug, L5, stop
        self.NT = T // 128
        self.NCt = C_LEN // 128
        self.NTA = self.NT + self.NCt
        self.NTOK = T + C_LEN
        self.fw = FW(nc)
        self.din = {}
        self.scr = {}

    def inp(self, name, shape, dt=F32):
        t = self.nc.dram_tensor(name, list(shape), dt, kind="ExternalInput")
        self.din[name] = Buf(name, t)
        return self.din[name]

    def scratch(self, name, shape, dt):
        kind = "ExternalOutput" if self.debug else "Internal"
        t = self.nc.dram_tensor(name, list(shape), dt, kind=kind)
        self.scr[name] = Buf(name, t)
        return self.scr[name]

    def declare(self):
        T, NE, NT, NTOK = self.T, self.NE, self.NT, self.NTOK
        i = self.inp
        i("x", [T, D]); i("ctx", [C_LEN, D]); i("cvecT", [128, 8, 2])
        i("ada_w", [2, D, IN_W]); i("ada_b", [2, IN_W]); i("norm_mix_g", [2, D]); i("norm_ffn_g", [2, D])
        i("w_in", [2, D, IN_W]); i("q_norm_g", [2, 64]); i("k_norm_g", [2, 64]); i("diff_lambda", [2, 256])
        i("subln_g", [2, 128]); i("s5_a_re", [2, 2, 16, 64]); i("s5_a_im", [2, 2, 16, 64]); i("s5_log_dt", [2, 32])
        i("s5_b_re", [2, 16, 64, 16]); i("s5_b_im", [2, 16, 64, 16]); i("s5_c_re", [2, 256, 64]); i("s5_c_im", [2, 256, 64])
        i("s5_d", [2, 256]); i("s5_glu_w", [2, 256, 256]); i("s5_glu_b", [2, 256])
        i("hg_lb_logits", [2, 2, 256]); i("hg_norm_g", [2, 64])
        i("w_o_att", [2, 512, D]); i("w_o_s5", [2, 256, D]); i("w_o_hg", [2, 256, D]); i("w_out", [2, D, D])
        i("moe_w_group", [2, D, 4]); i("moe_b_group", [2, 4]); i("moe_w_expert", [2, D, NE]); i("moe_b_expert", [2, NE])
        i("moe_w_gate", [2, NE, D, 512]); i("moe_w_up", [2, NE, D, 512]); i("moe_w_down", [2, NE, 512, D])
        i("k_ident", [128, 128]); i("k_ropeC", [128, NT, 32]); i("k_ropeS", [128, NT, 32])
        i("k_tri", [128, 2, 64]); i("k_su", [128, 2, 64]); i("k_tribd", [128, 2, 128]); i("k_subd", [128, 2, 128]); i("k_rowmask", [128, 8]); i("k_colmask", [128, 8, 128])
        i("k_sgn", [128, 2]); i("k_p2", [128, 128]); i("k_iota", [128, 512])
        self.NBLK = (2 * NTOK) // 128 + NE
        i("k_tokid", [128, self.NTA], I32); i("k_bstart", [128, self.NBLK]); i("k_pidx", [128, 1]); i("k_slt", [128, 128])
        self.out = Buf("y", self.nc.dram_tensor("y", [T, D], F32, kind="ExternalOutput"))
        s = self.scratch
        s("modd", [2, IN_W], F32)
        s("xa", [NTOK, D], F32); s("xb", [NTOK, D], F32)
        s("QT", [4, 128, NTOK], BF16); s("KT", [4, 128, NTOK], BF16); s("V", [NTOK, 512], BF16)
        s("UT", [256, NTOK], BF16)
        s("HGtok", [NTOK, 7, 256], F32)
        s("HGfeat", [3, 256, NTOK], F32)
        s("G", [NTOK, 3 * D], BF16)
        s("YAT", [512, NTOK], BF16); s("YST", [256, NTOK], BF16); s("YHT", [256, NTOK], BF16)
        s("HXFT", [128, 8, NTOK], BF16); s("WV", [NTOK, NE], F32)
        s("S5st", [128, 64], F32)
        s("E12", [NTOK, 2 * NE], BF16); s("W12", [NTOK, 2], F32); s("HXFtok", [NTOK, D], BF16)
        s("BUFTOK", [self.NBLK * 128, 1], I32); s("YBUF", [self.NBLK * 128, D], BF16)
        if self.debug:
            s("dDST", [128, self.NTA, 2], I32); s("dIDXG", [128, 1, self.NBLK], I32); s("dBLKE", [128, self.NBLK], F32)
            s("dPEND", [128, NE], F32); s("dCNT", [128, 2 * NE], F32)

    def mm(self, out, lhsT, rhs, start, stop, R, W):
        return self.fw.op('pe', lambda e: e.matmul(out, lhsT, rhs, start=start, stop=stop), reads=R, writes=W)

    def tr(self, out, in_, ident, R, W):
        return self.fw.op('pe', lambda e: e.transpose(out, in_, ident), reads=R, writes=W)

    def act(self, out, in_, func, R, W, eng='act', **kw):
        return self.fw.op('act', lambda e: e.activation(out=out, in_=in_, func=func, **kw), reads=R, writes=W)

    def tt(self, out, in0, in1, op, R, W, eng='dve'):
        return self.fw.op(eng, lambda e: e.tensor_tensor(out=out, in0=in0, in1=in1, op=op), reads=R, writes=W)

    def ts(self, out, in0, s1, s2, op0, op1, R, W, eng='dve'):
        if op1 is None:
            return self.fw.op(eng, lambda e: e.tensor_scalar(out=out, in0=in0, scalar1=s1, scalar2=None, op0=op0), reads=R, writes=W)
        return self.fw.op(eng, lambda e: e.tensor_scalar(out=out, in0=in0, scalar1=s1, scalar2=s2, op0=op0, op1=op1), reads=R, writes=W)

    def stt(self, out, in0, sc, in1, op0, op1, R, W, eng='dve'):
        return self.fw.op(eng, lambda e: e.scalar_tensor_tensor(out=out, in0=in0, scalar=sc, in1=in1, op0=op0, op1=op1), reads=R, writes=W)

    def cp(self, out, in_, R, W, eng='dve'):
        if eng == 'act':
            return self.fw.op('act', lambda e: e.activation(out=out, in_=in_, func=AF.Copy), reads=R, writes=W)
        return self.fw.op(eng, lambda e: e.tensor_copy(out=out, in_=in_), reads=R, writes=W)

    def rstd(self, buf, n, scratch=None):
        self.ts(buf.ap(), buf.ap(), 1.0 / n, EPS, ALU.mult, ALU.add, [buf], [buf])
        self.act(buf.ap(), buf.ap(), AF.Ln, [buf], [buf])
        self.act(buf.ap(), buf.ap(), AF.Exp, [buf], [buf], scale=-0.5)

    def tok_src(self, which, l):
        raise NotImplementedError

    def load_consts(self):
        fw, din = self.fw, self.din
        self.bcreg = self.nc.gpsimd.to_reg(2 * self.NE * 128 - 1)
        self.identf = fw.sb("identf", [128, 128], F32)
        self.identb = fw.sb("identb", [128, 128], BF16)
        fw.dma('sp', self.identf.ap(), din["k_ident"].ap(), writes=[self.identf])
        fw.dma('pool', self.identb.ap(), din["k_ident"].ap(), writes=[self.identb])
        self.onesb = fw.sb("onesb", [128, 128], BF16)
        fw.op('dve', lambda e: e.memset(self.onesb.ap(), 1.0), writes=[self.onesb])
        self.cact = fw.sb("cact", [128, 8, 2], F32)
        fw.dma('sp', self.cact.ap(), din["cvecT"].ap(), writes=[self.cact])
        self.act(self.cact.ap(), self.cact.ap(), AF.Silu, [self.cact], [self.cact])

    def prologue(self, l):
        fw, din = self.fw, self.din
        fw.push()
        modd = self.scr["modd"]
        msb = fw.sb("msb", [2, IN_W], F32)
        bsb = fw.sb("bsb", [2, IN_W], F32)
        fw.dma('sp', bsb.ap(), din["ada_b"][l:l + 1, :].partition_broadcast(2) if False else din["ada_b"][l:l + 1, :].to_broadcast([2, IN_W]), writes=[bsb])
        wst = [fw.sb("adaw%d" % i, [128, 8, 512], F32) for i in range(2)]
        pp = [fw.ps("pmod%d" % i, [128, 512]) for i in range(2)]
        for nb in range(IN_W // 512):
            w = wst[nb % 2]
            p = pp[nb % 2]
            fw.dma('sp' if nb % 2 == 0 else 'act', w.ap(),
                   din["ada_w"][l, :, nb * 512:(nb + 1) * 512].rearrange("(kc p) n -> p kc n", p=128), writes=[w], part=False)
            for kc in range(8):
                self.mm(p[0:2, :], self.cact[:, kc, :], w[:, kc, :], kc == 0, kc == 7, [self.cact, w], [p])
            self.tt(msb[:, nb * 512:(nb + 1) * 512], p[0:2, :], bsb[:, nb * 512:(nb + 1) * 512], ALU.add, [p, bsb], [msb])
        fw.dma('sp', modd.ap(), msb.ap(), reads=[msb], writes=[modd], part=False)
        fw.pop()

    def load_mod(self, l, row, sect, name):
        fw = self.fw
        t = fw.sb(name, [128, D], F32)
        fw.dma('sp', t.ap(), self.scr["modd"][row:row + 1, sect * D:(sect + 1) * D].to_broadcast([128, D]),
               reads=[self.scr["modd"]], writes=[t])
        return t

    def bcast_row(self, name, ap_row, n, dt=F32, q='sp'):
        t = self.fw.sb(name, [128, n], dt)
        self.fw.dma(q, t.ap(), ap_row.to_broadcast([128, n]), writes=[t])
        return t


def _phaseA(self, l):
    fw, din, scr = self.fw, self.din, self.scr
    NCt, NTA = self.NCt, self.NTA
    fw.push()
    win = fw.sb("win", [128, 8, IN_W], BF16)
    for kc in range(8):
        fw.dma('pool', win[:, kc, :], din["w_in"][l, kc * 128:(kc + 1) * 128, :], writes=[win])
    gmix = self.bcast_row("gmix", din["norm_mix_g"][l:l + 1, :], D)
    A1, SH1 = [], []
    for row in range(2):
        sc = self.load_mod(l, row, 1, "sc1_%d" % row)
        self.stt(sc.ap(), sc.ap(), 1.0, gmix.ap(), ALU.add, ALU.mult, [sc, gmix], [sc])
        A1.append(sc)
        SH1.append(self.load_mod(l, row, 0, "sh1_%d" % row))
    gq = self.bcast_row("gq", din["q_norm_g"][l:l + 1, :], 64)
    gk = self.bcast_row("gk", din["k_norm_g"][l:l + 1, :], 64)
    ropeC = fw.sb("ropeC", [128, self.NT, 32], F32)
    ropeS = fw.sb("ropeS", [128, self.NT, 32], F32)
    fw.dma('sp', ropeC.ap(), din["k_ropeC"].ap(), writes=[ropeC])
    fw.dma('sp', ropeS.ap(), din["k_ropeS"].ap(), writes=[ropeS])
    LB = fw.sb("LB", [128, 512], F32)
    OML = fw.sb("OML", [128, 512], F32)
    LBT = fw.sb("LBT", [128, 2, 2], F32)
    OMLT = fw.sb("OMLT", [128, 2, 2], F32)
    if l == 0:
        fw.op('dve', lambda e: e.memset(LB.ap(), 0.0), writes=[LB])
        fw.op('dve', lambda e: e.memset(LBT.ap(), 0.0), writes=[LBT])
    else:
        l0 = self.bcast_row("lbl0", din["hg_lb_logits"][0:1].rearrange("o d c -> o (d c)"), 512)
        fw.dma('sp', LB.ap(), din["hg_lb_logits"][1:2].rearrange("o d c -> o (d c)").to_broadcast([128, 512]), writes=[LB])
        self.tt(LB.ap(), LB.ap(), l0.ap(), ALU.subtract, [LB, l0], [LB])
        self.act(LB.ap(), LB.ap(), AF.Sigmoid, [LB], [LB])
        l0t = fw.sb("l0t", [128, 2, 2], F32)
        fw.dma('sp', l0t.ap(), din["hg_lb_logits"][0].rearrange("d (ct p) -> p d ct", p=128), writes=[l0t], allow_slow_non_contiguous=True)
        fw.dma('sp', LBT.ap(), din["hg_lb_logits"][1].rearrange("d (ct p) -> p d ct", p=128), writes=[LBT], allow_slow_non_contiguous=True)
        self.tt(LBT.ap(), LBT.ap(), l0t.ap(), ALU.subtract, [LBT, l0t], [LBT])
        self.act(LBT.ap(), LBT.ap(), AF.Sigmoid, [LBT], [LBT])
    self.ts(OML.ap(), LB.ap(), -1.0, 1.0, ALU.mult, ALU.add, [LB], [OML])
    self.ts(OMLT.ap(), LBT.ap(), -1.0, 1.0, ALU.mult, ALU.add, [LBT], [OMLT])

    NB = 2
    xt = [fw.sb("xt%d" % i, [128, D], F32) for i in range(NB)]
    junk = fw.sb("junk", [128, D], F32)
    ssx = [fw.sb("ssx%d" % i, [128, 1], F32) for i in range(NB)]
    hx = [fw.sb("hx%d" % i, [128, D], BF16) for i in range(NB)]
    hxT = [fw.sb("hxT%d" % i, [128, 8, 128], BF16) for i in range(NB)]
    pT = fw.ps("pT", [128, 8, 128], BF16)
    pM = [fw.ps("pM%d" % i, [128, 512]) for i in range(4)]
    pF = fw.ps("pF", [128, 8, 128])
    pQ = fw.ps("pQ", [128, 4, 128], BF16)
    sq = fw.sb("sq", [128, 512], F32)
    ss8 = fw.sb("ss8", [128, 8], F32)
    qn = fw.sb("qn", [128, 512], F32)
    r1 = fw.sb("r1", [128, 8, 32], F32)
    r2 = fw.sb("r2", [128, 8, 32], F32)
    qr = [fw.sb("qr%d" % i, [128, 512], BF16) for i in range(2)]
    qTs = [fw.sb("qTs%d" % i, [128, 4, 128], BF16) for i in range(2)]
    vsb = [fw.sb("vsb%d" % i, [128, 512], BF16) for i in range(2)]
    usb = [fw.sb("usb%d" % i, [128, 2, 128], BF16) for i in range(2)]
    hgt = [fw.sb("hgt%d" % i, [128, 7, 256], F32) for i in range(2)]
    hgf = [fw.sb("hgf%d" % i, [128, 3, 2, 128], F32) for i in range(2)]
    gsb = [fw.sb("gsb%d" % i, [128, 3 * D], BF16) for i in range(2)]
    mi = 0
    for i in range(NTA):
        b = i % NB
        row = 1 if i < NCt else 0
        tok0 = i * 128
        if l == 0:
            src = din["ctx"] if i < NCt else din["x"]
            sap = src[tok0:tok0 + 128, :] if i < NCt else src[tok0 - C_LEN:tok0 - C_LEN + 128, :]
        else:
            src = scr["xb"]
            sap = src[tok0:tok0 + 128, :]
        fw.dma('sp', xt[b].ap(), sap, reads=[src], writes=[xt[b]], part=False)
        self.act(junk.ap(), xt[b].ap(), AF.Square, [xt[b]], [junk, ssx[b]], accum_out=ssx[b].ap())
        self.rstd(ssx[b], D)
        self.stt(junk.ap(), xt[b].ap(), ssx[b].ap(), A1[row].ap(), ALU.mult, ALU.mult, [xt[b], ssx[b], A1[row]], [junk])
        self.tt(hx[b].ap(), junk.ap(), SH1[row].ap(), ALU.add, [junk, SH1[row]], [hx[b]])
        for kc in range(8):
            self.tr(pT[:, kc, :], hx[b][:, kc * 128:(kc + 1) * 128], self.identb.ap(), [hx[b], self.identb], [pT])
        self.cp(hxT[b].ap(), pT.ap(), [pT], [hxT[b]], eng='act')
        H = hxT[b]

        def tokmm(c0, n):
            nonlocal mi
            p = pM[mi % 4]
            mi += 1
            for kc in range(8):
                self.mm(p[:, 0:n], H[:, kc, :], win[:, kc, c0:c0 + n], kc == 0, kc == 7, [H, win], [p])
            return p

        for sec, gbc, dst in ((0, gq, scr["QT"]), (1, gk, scr["KT"])):
            p = tokmm(sec * 512, 512)
            self.act(sq.ap(), p.ap(), AF.Square, [p], [sq])
            fw.op('dve', lambda e: e.tensor_reduce(out=ss8.ap(), in_=sq.ap().rearrange("p (a d) -> p a d", d=64),
                                                   axis=AX.X, op=ALU.add), reads=[sq], writes=[ss8])
            self.rstd(ss8, 64)
            qn3 = qn.ap().rearrange("p (a d) -> p a d", d=64)
            self.tt(qn3, p.ap().rearrange("p (a d) -> p a d", d=64),
                    ss8.ap().rearrange("p (a o) -> p a o", o=1).to_broadcast([128, 8, 64]), ALU.mult, [p, ss8], [qn])
            Q = qr[sec]
            if i < NCt:
                self.tt(Q.ap().rearrange("p (a d) -> p a d", d=64), qn3,
                        gbc.ap().rearrange("p (o d) -> p o d", o=1).to_broadcast([128, 8, 64]), ALU.mult, [qn, gbc], [Q])
            else:
                self.tt(qn3, qn3, gbc.ap().rearrange("p (o d) -> p o d", o=1).to_broadcast([128, 8, 64]), ALU.mult, [qn, gbc], [qn])
                q4 = qn.ap().rearrange("p (a m d) -> p a m d", a=8, m=2)
                Q4 = Q.ap().rearrange("p (a m d) -> p a m d", a=8, m=2)
                t1, t2 = q4[:, :, 0, :], q4[:, :, 1, :]
                ti = i - NCt
                cs = ropeC[:, ti, :].rearrange("p (o d) -> p o d", o=1).to_broadcast([128, 8, 32])
                sn = ropeS[:, ti, :].rearrange("p (o d) -> p o d", o=1).to_broadcast([128, 8, 32])
                self.tt(r1.ap(), t1, cs, ALU.mult, [qn, ropeC], [r1])
                self.tt(r2.ap(), t2, sn, ALU.mult, [qn, ropeS], [r2], eng='pool')
                self.tt(Q4[:, :, 0, :], r1.ap(), r2.ap(), ALU.subtract, [r1, r2], [Q])
                self.tt(r1.ap(), t1, sn, ALU.mult, [qn, ropeS], [r1])
                self.tt(r2.ap(), t2, cs, ALU.mult, [qn, ropeC], [r2], eng='pool')
                self.tt(Q4[:, :, 1, :], r1.ap(), r2.ap(), ALU.add, [r1, r2], [Q])
            for a in range(4):
                self.tr(pQ[:, a, :], Q[:, a * 128:(a + 1) * 128], self.identb.ap(), [Q, self.identb], [pQ])
            self.cp(qTs[sec].ap(), pQ.ap(), [pQ], [qTs[sec]], eng='act')
            fw.dma('sp', dst[:, :, tok0:tok0 + 128].rearrange("a p t -> p a t"), qTs[sec].ap(), reads=[qTs[sec]], writes=[dst])
        p = tokmm(1024, 512)
        self.cp(vsb[i % 2].ap(), p.ap(), [p], [vsb[i % 2]], eng='act')
        fw.dma('act', scr["V"][tok0:tok0 + 128, :], vsb[i % 2].ap(), reads=[vsb[i % 2]], writes=[scr["V"]])
        for j in range(8):
            c0 = 1536 + j * 128
            for kc in range(8):
                self.mm(pF[:, j, :], win[:, kc, c0:c0 + 128], H[:, kc, :], kc == 0, kc == 7, [H, win], [pF])
        U = usb[i % 2]
        self.cp(U.ap(), pF[:, 0:2, :], [pF], [U], eng='act')
        fw.dma('act', scr["UT"][:, tok0:tok0 + 128].rearrange("(ct p) t -> p ct t", p=128), U.ap(), reads=[U], writes=[scr["UT"]])
        HF = hgf[i % 2]
        self.act(HF[:, 0, :, :], pF[:, 2:4, :], AF.Silu, [pF], [HF])
        for d_ in range(2):
            for ct in range(2):
                self.act(HF[:, 1 + d_, ct, :], pF[:, 4 + 2 * d_ + ct, :], AF.Sigmoid, [pF], [HF], scale=-1.0)
                self.ts(HF[:, 1 + d_, ct, :], HF[:, 1 + d_, ct, :], OMLT[:, d_, ct:ct + 1], None, ALU.mult, None, [HF, OMLT], [HF])
        fw.dma('sp', scr["HGfeat"][:, :, tok0:tok0 + 128].rearrange("s (ct p) t -> p s ct t", p=128), HF.ap(), reads=[HF], writes=[scr["HGfeat"]])
        HT = hgt[i % 2]
        p = tokmm(1792, 256)
        self.act(HT[:, 0, :], p[:, 0:256], AF.Silu, [p], [HT])
        p = tokmm(2048, 512)
        self.act(sq.ap(), p.ap(), AF.Sigmoid, [p], [sq])
        self.tt(sq.ap(), sq.ap(), OML.ap(), ALU.mult, [sq, OML], [sq])
        self.tt(sq.ap(), sq.ap(), LB.ap(), ALU.add, [sq, LB], [sq])
        self.ts(HT[:, 1:3, :], sq.ap().rearrange("p (a d) -> p a d", d=256), -1.0, 1.0, ALU.mult, ALU.add, [sq], [HT])
        self.act(HT[:, 3:5, :], sq.ap().rearrange("p (a d) -> p a d", d=256), AF.Ln, [sq], [HT])
        p = tokmm(2560, 512)
        self.cp(HT[:, 5, :], p[:, 0:256], [p], [HT])
        self.act(HT[:, 6, :], p[:, 256:512], AF.Silu, [p], [HT])
        fw.dma('sp', scr["HGtok"][tok0:tok0 + 128, :, :], HT.ap(), reads=[HT], writes=[scr["HGtok"]])
        Gs = gsb[i % 2]
        for j in range(6):
            p = tokmm(3072 + j * 512, 512)
            self.act(Gs[:, j * 512:(j + 1) * 512], p.ap(), AF.Sigmoid, [p], [Gs])
        fw.dma('act', scr["G"][tok0:tok0 + 128, :], Gs.ap(), reads=[Gs], writes=[scr["G"]])
    fw.pop()


Prog.phaseA = _phaseA


def _phaseB(self, l):
    fw, din, scr = self.fw, self.din, self.scr
    NCt, NTA, NTOK = self.NCt, self.NTA, self.NTOK
    lam_init = 0.8 - 0.6 * math.exp(-0.3 * l)
    fw.push()
    KTr = fw.sb("KTr", [128, 4, NTOK], BF16)
    Vr = fw.sb("Vr", [128, NTA, 512], BF16)
    for a in range(4):
        fw.dma('sp' if a % 2 == 0 else 'act', KTr[:, a, :], scr["KT"][a, :, :], reads=[scr["KT"]], writes=[KTr])
    step = 8
    for k0 in range(0, NTA, step):
        k1 = min(NTA, k0 + step)
        fw.dma('sp' if (k0 // step) % 2 == 0 else 'act', Vr[:, k0:k1, :],
               scr["V"][k0 * 128:k1 * 128, :].rearrange("(kt p) c -> p kt c", p=128), reads=[scr["V"]], writes=[Vr])
    dl = self.bcast_row("dl", din["diff_lambda"][l:l + 1, :], 256)
    pr = fw.sb("pr", [128, 2, 64], F32)
    dl4 = dl.ap().rearrange("p (a b d) -> p a b d", a=2, b=2)
    self.tt(pr.ap(), dl4[:, :, 0, :], dl4[:, :, 1, :], ALU.mult, [dl], [pr])
    e2 = fw.sb("e2", [128, 2], F32)
    fw.op('dve', lambda e: e.tensor_reduce(out=e2.ap(), in_=pr.ap(), axis=AX.X, op=ALU.add), reads=[pr], writes=[e2])
    self.act(e2.ap(), e2.ap(), AF.Exp, [e2], [e2])
    nlam = fw.sb("nlam", [128, 1], F32)
    self.tt(nlam.ap(), e2[:, 1:2], e2[:, 0:1], ALU.subtract, [e2], [nlam])
    self.ts(nlam.ap(), nlam.ap(), -lam_init, None, ALU.add, None, [nlam], [nlam])
    sgs = fw.sb("sgs", [128, 1], F32)
    fw.dma('sp', sgs.ap(), din["subln_g"][l].rearrange("(p o) -> p o", o=1), writes=[sgs])
    self.ts(sgs.ap(), sgs.ap(), 1.0 - lam_init, None, ALU.mult, None, [sgs], [sgs])

    ps_s = [[fw.ps("ps_s%d%d" % (i, m), [128, 512]) for m in range(2)] for i in range(2)]
    ps_o = [fw.ps("ps_o%d" % m, [128, 512]) for m in range(2)]
    ps_z = fw.ps("ps_z0", [128, 512])
    ps_n = fw.ps("ps_n", [128, 512])
    NPB = 4
    pbuf = [[fw.sb("pb%d%d" % (i, m), [128, 512], BF16) for m in range(2)] for i in range(NPB)]
    QTb = [fw.sb("QTb%d" % i, [128, 4, 512], BF16) for i in range(2)]
    zacc = fw.sb("zacc", [128, 512], F32)
    onesf = fw.sb("onesf", [128, 128], F32)
    fw.op('pool', lambda e: e.memset(onesf.ap(), 1.0), writes=[onesf])
    rr = [fw.sb("rr%d" % m, [128, 512], F32) for m in range(2)]
    aa = [fw.sb("aa%d" % m, [128, 512], F32) for m in range(2)]
    asq = fw.sb("asq", [128, 512], BF16)
    rs = fw.sb("rs", [128, 512], F32)
    yab = [fw.sb("yab%d" % i, [128, 512], BF16) for i in range(2)]
    blocks = []
    if l == 0:
        blocks.append((0, C_LEN, 0, NCt))
    for j in range(self.T // 512):
        blocks.append((C_LEN + j * 512, 512, 0, NTA))
    cnt = 0
    ei = 0
    for bi, (q0, nq, kt0, kt1) in enumerate(blocks):
        Qb = QTb[bi % 2]
        fw.dma('sp', Qb[:, :, 0:nq], scr["QT"][:, :, q0:q0 + nq].rearrange("a p t -> p a t"), reads=[scr["QT"]], writes=[Qb], part=False)
        for h in range(4):
            def score(kt, c):
                for m in range(2):
                    base = m * 64
                    S = ps_s[c % 2][m]
                    self.mm(S[:, 0:nq], KTr[base:base + 64, h, kt * 128:(kt + 1) * 128], Qb[base:base + 64, h, 0:nq], True, True, [KTr, Qb], [S])
            score(kt0, cnt)
            for kt in range(kt0, kt1):
                if kt + 1 < kt1:
                    score(kt + 1, cnt + 1)
                for m in range(2):
                    self.act(pbuf[cnt % NPB][m][:, 0:nq], ps_s[cnt % 2][m][:, 0:nq], AF.Exp, [ps_s[cnt % 2][m]], [pbuf[cnt % NPB][m]], scale=0.125)
                for m in range(2):
                    pT = pbuf[cnt % NPB][m]
                    self.mm(ps_o[m][:, 0:nq], Vr[:, kt, h * 128:(h + 1) * 128], pT[:, 0:nq], kt == kt0, kt == kt1 - 1, [Vr, pT], [ps_o[m]])
                pT0, pT1 = pbuf[cnt % NPB]
                self.mm(ps_z[:, 0:nq], self.onesb.ap(), pT0[:, 0:nq], kt == kt0, kt == kt1 - 1, [self.onesb, pT0], [ps_z])
                if kt == kt0:
                    self.cp(zacc[:, 0:nq], pT1[:, 0:nq], [pT1], [zacc])
                else:
                    self.tt(zacc[:, 0:nq], zacc[:, 0:nq], pT1[:, 0:nq], ALU.add, [zacc, pT1], [zacc])
                cnt += 1
            self.mm(ps_n[:, 0:nq], onesf.ap(), zacc[:, 0:nq], True, True, [onesf, zacc], [ps_n])
            fw.op('dve', lambda e: e.reciprocal(out=rr[0][:, 0:nq], in_=ps_z[:, 0:nq]), reads=[ps_z], writes=[rr[0]])
            fw.op('dve', lambda e: e.reciprocal(out=rr[1][:, 0:nq], in_=ps_n[:, 0:nq]), reads=[ps_n], writes=[rr[1]])
            for m in range(2):
                self.tt(aa[m][:, 0:nq], ps_o[m][:, 0:nq], rr[m][:, 0:nq], ALU.mult, [ps_o[m], rr[m]], [aa[m]])
            self.stt(aa[0][:, 0:nq], aa[1][:, 0:nq], nlam.ap(), aa[0][:, 0:nq], ALU.mult, ALU.add, [aa[0], aa[1], nlam], [aa[0]])
            self.tt(asq[:, 0:nq], aa[0][:, 0:nq], aa[0][:, 0:nq], ALU.mult, [aa[0]], [asq], eng='pool')
            self.mm(ps_n[:, 0:nq], self.onesb.ap(), asq[:, 0:nq], True, True, [self.onesb, asq], [ps_n])
            self.ts(rs[:, 0:nq], ps_n[:, 0:nq], 1.0 / 128, EPS, ALU.mult, ALU.add, [ps_n], [rs])
            self.act(rs[:, 0:nq], rs[:, 0:nq], AF.Ln, [rs], [rs])
            self.act(rs[:, 0:nq], rs[:, 0:nq], AF.Exp, [rs], [rs], scale=-0.5)
            Y = yab[ei % 2]
            ei += 1
            self.stt(Y[:, 0:nq], aa[0][:, 0:nq], sgs.ap(), rs[:, 0:nq], ALU.mult, ALU.mult, [aa[0], sgs, rs], [Y])
            fw.dma('act', scr["YAT"][h * 128:(h + 1) * 128, q0:q0 + nq], Y[:, 0:nq], reads=[Y], writes=[scr["YAT"]])
    fw.pop()


Prog.phaseB = _phaseB


def _rev(ap):
    a = [list(x) for x in ap.ap]
    st, n = a[-1]
    a[-1] = [-st, n]
    return bass.AP(ap.tensor, ap.offset + st * (n - 1), a)


def _sincos(self, dst, ang, shape, tmpf, tmpi, want_cos):
    P = [ang, tmpf, tmpi]
    if want_cos:
        self.ts(ang.ap(), ang.ap(), math.pi / 2, None, ALU.add, None, [ang], [ang])
    self.ts(tmpf.ap(), ang.ap(), 1.0 / TWO_PI, None, ALU.mult, None, [ang], [tmpf])
    self.cp(tmpi.ap(), tmpf.ap(), [tmpf], [tmpi])
    self.cp(tmpf.ap(), tmpi.ap(), [tmpi], [tmpf])
    self.stt(ang.ap(), tmpf.ap(), -TWO_PI, ang.ap(), ALU.mult, ALU.add, [tmpf, ang], [ang])
    self.ts(tmpf.ap(), ang.ap(), math.pi, -TWO_PI, ALU.is_gt, ALU.mult, [ang], [tmpf])
    self.tt(ang.ap(), ang.ap(), tmpf.ap(), ALU.add, [ang, tmpf], [ang])
    self.ts(tmpf.ap(), ang.ap(), -math.pi, TWO_PI, ALU.is_lt, ALU.mult, [ang], [tmpf])
    self.tt(ang.ap(), ang.ap(), tmpf.ap(), ALU.add, [ang, tmpf], [ang])
    self.ts(ang.ap(), ang.ap(), math.pi, -math.pi, ALU.min, ALU.max, [ang], [ang])
    self.act(dst, ang.ap(), AF.Sin, [ang], [])


def _phaseC(self, l):
    fw, din, scr = self.fw, self.din, self.scr
    L, NTOK = self.L5, self.NTOK
    NCH = NTOK // L
    NCc = C_LEN // L
    fw.push()
    are = fw.sb("are", [128, 32], F32); aim = fw.sb("aim", [128, 32], F32); dt = fw.sb("dt", [128, 32], F32)
    for hf in range(2):
        fw.dma('sp', are[hf * 64:(hf + 1) * 64, :], din["s5_a_re"][l].rearrange("d g p -> p (d g)"), writes=[are], allow_slow_non_contiguous=True)
        fw.dma('act', aim[hf * 64:(hf + 1) * 64, :], din["s5_a_im"][l].rearrange("d g p -> p (d g)"), writes=[aim], allow_slow_non_contiguous=True)
    fw.dma('sp', dt.ap(), din["s5_log_dt"][l:l + 1, :].to_broadcast([128, 32]), writes=[dt])
    self.act(dt.ap(), dt.ap(), AF.Exp, [dt], [dt])
    th = fw.sb("th", [128, 32], F32); R = fw.sb("R", [128, 32], F32)
    self.tt(th.ap(), aim.ap(), dt.ap(), ALU.mult, [aim, dt], [th])
    self.tt(R.ap(), are.ap(), dt.ap(), ALU.mult, [are, dt], [R])
    self.act(R.ap(), R.ap(), AF.Exp, [R], [R])
    a32 = fw.sb("a32", [128, 32], F32); f32 = fw.sb("f32t", [128, 32], F32); i32 = fw.sb("i32t", [128, 32], I32)
    sn1 = fw.sb("sn1", [128, 32], F32); cs1 = fw.sb("cs1", [128, 32], F32)
    snL = fw.sb("snL", [128, 32], F32); csL = fw.sb("csL", [128, 32], F32)
    for dst, mul, wc in ((sn1, 1.0, False), (cs1, 1.0, True), (snL, float(L), False), (csL, float(L), True)):
        self.ts(a32.ap(), th.ap(), mul, None, ALU.mult, None, [th], [a32])
        _sincos(self, dst.ap(), a32, None, f32, i32, wc)
        dst.writers['act'] = fw.E['act'].count
    nre = fw.sb("nre", [128, 32], F32); nim = fw.sb("nim", [128, 32], F32); den = fw.sb("den", [128, 32], F32)
    kre = fw.sb("kre", [128, 32], F32); kim = fw.sb("kim", [128, 32], F32); tmp = fw.sb("tmp32", [128, 32], F32)
    self.tt(nre.ap(), R.ap(), cs1.ap(), ALU.mult, [R, cs1], [nre])
    self.ts(nre.ap(), nre.ap(), -1.0, None, ALU.add, None, [nre], [nre])
    self.tt(nim.ap(), R.ap(), sn1.ap(), ALU.mult, [R, sn1], [nim])
    self.tt(den.ap(), are.ap(), are.ap(), ALU.mult, [are], [den])
    self.tt(tmp.ap(), aim.ap(), aim.ap(), ALU.mult, [aim], [tmp])
    self.tt(den.ap(), den.ap(), tmp.ap(), ALU.add, [den, tmp], [den])
    fw.op('dve', lambda e: e.reciprocal(out=den.ap(), in_=den.ap()), reads=[den], writes=[den])
    self.tt(kre.ap(), nre.ap(), are.ap(), ALU.mult, [nre, are], [kre])
    self.tt(tmp.ap(), nim.ap(), aim.ap(), ALU.mult, [nim, aim], [tmp])
    self.tt(kre.ap(), kre.ap(), tmp.ap(), ALU.add, [kre, tmp], [kre])
    self.tt(kre.ap(), kre.ap(), den.ap(), ALU.mult, [kre, den], [kre])
    self.tt(kim.ap(), nim.ap(), are.ap(), ALU.mult, [nim, are], [kim])
    self.tt(tmp.ap(), nre.ap(), aim.ap(), ALU.mult, [nre, aim], [tmp])
    self.tt(kim.ap(), kim.ap(), tmp.ap(), ALU.subtract, [kim, tmp], [kim])
    self.tt(kim.ap(), kim.ap(), den.ap(), ALU.mult, [kim, den], [kim])
    Bexp = fw.sb("Bexp", [128, 2, 16, 2, 128], BF16)
    W1e = fw.sb("W1e", [128, 16, 128], BF16); W2e = fw.sb("W2e", [128, 16, 128], BF16)
    rowm = fw.sb("rowm", [128, 8], F32); colm = fw.sb("colm", [128, 8, 128], F32); sgn = fw.sb("sgn", [128, 2], F32)
    P2 = fw.sb("P2", [128, 128], F32)
    fw.dma('sp', rowm.ap(), din["k_rowmask"].ap(), writes=[rowm]); fw.dma('sp', colm.ap(), din["k_colmask"].ap(), writes=[colm])
    fw.dma('sp', sgn.ap(), din["k_sgn"].ap(), writes=[sgn]); fw.dma('sp', P2.ap(), din["k_p2"].ap(), writes=[P2])
    fw.push()
    Bre = fw.sb("Bre", [64, 16, 16], F32); Bim = fw.sb("Bim", [64, 16, 16], F32)
    fw.dma('sp', Bre.ap(), din["s5_b_re"][l].rearrange("g p h -> p g h"), writes=[Bre])
    fw.dma('act', Bim.ap(), din["s5_b_im"][l].rearrange("g p h -> p g h"), writes=[Bim])
    bb = fw.sb("bb", [64, 2, 2, 256], F32)
    t16 = fw.sb("t16", [64, 256], F32)
    pX = fw.ps("pX", [128, 8, 64])
    TT = fw.sb("TT", [128, 2, 2, 128], F32); TS = fw.sb("TS", [128, 2, 2, 128], F32)
    for d_ in range(2):
        kr = kre[0:64, d_ * 16:(d_ + 1) * 16].rearrange("p (g o) -> p g o", o=1).to_broadcast([64, 16, 16])
        ki = kim[0:64, d_ * 16:(d_ + 1) * 16].rearrange("p (g o) -> p g o", o=1).to_broadcast([64, 16, 16])
        v3 = lambda ap: ap.rearrange("p (g h) -> p g h", h=16)
        self.tt(v3(bb[:, d_, 0, :]), Bre.ap(), kr, ALU.mult, [Bre, kre], [bb])
        self.tt(v3(t16.ap()), Bim.ap(), ki, ALU.mult, [Bim, kim], [t16])
        self.tt(bb[:, d_, 0, :], bb[:, d_, 0, :], t16.ap(), ALU.subtract, [bb, t16], [bb])
        self.tt(v3(bb[:, d_, 1, :]), Bim.ap(), kr, ALU.mult, [Bim, kre], [bb])
        self.tt(v3(t16.ap()), Bre.ap(), ki, ALU.mult, [Bre, kim], [t16])
        self.tt(bb[:, d_, 1, :], bb[:, d_, 1, :], t16.ap(), ALU.add, [bb, t16], [bb])
        for ct in range(2):
            for c in range(2):
                self.tr(pX[:, ct * 2 + c, :], bb[:, d_, c, ct * 128:(ct + 1) * 128], self.identf[0:64, 0:64], [bb, self.identf], [pX])
            self.cp(TT[:, d_, ct, 0:64], pX[:, ct * 2 + 0, :], [pX], [TT])
            self.cp(TT[:, d_, ct, 64:128], pX[:, ct * 2 + 1, :], [pX], [TT])
            self.cp(TS[:, d_, ct, 0:64], pX[:, ct * 2 + 1, :], [pX], [TS])
            self.ts(TS[:, d_, ct, 64:128], pX[:, ct * 2 + 0, :], -1.0, None, ALU.mult, None, [pX], [TS])
        for g in range(16):
            self.ts(Bexp[:, d_, g, 0, :], TT[:, d_, g // 8, :], rowm[:, g % 8:g % 8 + 1], None, ALU.mult, None, [TT, rowm], [Bexp])
            self.ts(Bexp[:, d_, g, 1, :], TS[:, d_, g // 8, :], rowm[:, g % 8:g % 8 + 1], None, ALU.mult, None, [TS, rowm], [Bexp])
    CC = fw.sb("CC", [128, 2, 2, 128], F32)
    for ct in range(2):
        fw.dma('sp', CC[:, ct, 0, 0:64], din["s5_c_re"][l, ct * 128:(ct + 1) * 128, :], writes=[CC])
        fw.dma('act', CC[:, ct, 0, 64:128], din["s5_c_im"][l, ct * 128:(ct + 1) * 128, :], writes=[CC])
        fw.dma('sp', CC[:, ct, 1, 0:64], din["s5_c_im"][l, ct * 128:(ct + 1) * 128, :], writes=[CC])
        fw.dma('act', CC[:, ct, 1, 64:128], din["s5_c_re"][l, ct * 128:(ct + 1) * 128, :], writes=[CC])
    pW = fw.ps("pW", [128, 4, 128])
    WA = fw.sb("WA", [128, 2, 2, 128], F32)
    for ct in range(2):
        for vv in range(2):
            self.tr(pW[:, ct * 2 + vv, :], CC[:, ct, vv, :], self.identf.ap(), [CC, self.identf], [pW])
            self.ts(WA[:, ct, vv, :], pW[:, ct * 2 + vv, :], sgn[:, vv:vv + 1], None, ALU.mult, None, [pW, sgn], [WA])
    for g in range(16):
        self.tt(W1e[:, g, :], WA[:, g // 8, 0, :], colm[:, g % 8, :], ALU.mult, [WA, colm], [W1e])
        self.tt(W2e[:, g, :], WA[:, g // 8, 1, :], colm[:, g % 8, :], ALU.mult, [WA, colm], [W2e])
    fw.pop()
    Ctab = fw.sb("Ctab", [128, 32, L], F32); Stab = fw.sb("Stab", [128, 32, L], F32)
    iot = fw.sb("iot", [128, L], F32)
    fw.dma('sp', iot.ap(), din["k_iota"][:, 0:L], writes=[iot])
    angL = fw.sb("angL", [128, L], F32); fL = fw.sb("fL", [128, L], F32); iL = fw.sb("iL", [128, L], I32)
    for dg in range(32):
        for dst, wc in ((Stab, False), (Ctab, True)):
            self.ts(angL.ap(), iot.ap(), th[:, dg:dg + 1], None, ALU.mult, None, [iot, th], [angL])
            _sincos(self, dst[:, dg, :], angL, None, fL, iL, wc)
            dst.writers['act'] = fw.E['act'].count
    dvec = fw.sb("dvec", [128, 2], F32); glub = fw.sb("glub", [128, 2], F32)
    fw.dma('sp', dvec.ap(), din["s5_d"][l].rearrange("(ct p) -> p ct", p=128), writes=[dvec], allow_slow_non_contiguous=True)
    fw.dma('sp', glub.ap(), din["s5_glu_b"][l].rearrange("(ct p) -> p ct", p=128), writes=[glub], allow_slow_non_contiguous=True)
    gluw = fw.sb("gluw", [128, 2, 256], BF16)
    fw.dma('pool', gluw.ap(), din["s5_glu_w"][l].rearrange("(kt p) n -> p kt n", p=128), writes=[gluw])
    ginit = fw.sb("ginit", [128, 32], F32)
    fw.op('dve', lambda e: e.memset(ginit.ap(), 0.0), writes=[ginit])
    yacc = fw.sb("yacc", [128, 2, NTOK], F32)
    pBU = [fw.ps("pBU%d" % i, [128, 512]) for i in range(2)]
    pBS = [fw.ps("pBS%d" % i, [128, 512]) for i in range(2)]
    pY = [fw.ps("pY%d" % i, [128, 512]) for i in range(2)]
    pG = fw.ps("pG", [128, 512]); pZ = fw.ps("pZ", [128, 512])
    Ub = [fw.sb("Ub%d" % i, [128, 2, L], BF16) for i in range(2)]
    t1 = [fw.sb("t1_%d" % i, [128, L], F32) for i in range(2)]
    t2 = [fw.sb("t2_%d" % i, [128, L], F32) for i in range(2)]
    bp = [fw.sb("bp%d" % i, [128, L], F32) for i in range(2)]
    Gs = [fw.sb("Gs%d" % i, [128, L], F32) for i in range(2)]
    G1 = [fw.sb("G1_%d" % i, [128, L], BF16) for i in range(2)]
    G2 = [fw.sb("G2_%d" % i, [128, L], BF16) for i in range(2)]
    gtb = [fw.sb("gt%d" % i, [128, 1], F32) for i in range(2)]
    ginit_b = [Buf("ginit%d" % i, ginit.t) for i in range(32)]
    for gb_ in ginit_b:
        gb_.writers = dict(ginit.writers)
    pGb = [Buf("pGv%d" % i, pG.t) for i in range(2)]
    yy = fw.sb("yy", [128, 2, L], F32); y2 = fw.sb("y2", [128, 2, L], F32); zz = fw.sb("zz", [128, 2, L], F32)
    zb = fw.sb("zb", [128, 2, L], BF16); gg = fw.sb("gg", [128, L], F32)
    yo = [fw.sb("yo%d" % i, [128, L], BF16) for i in range(2)]
    it = 0
    for d_ in range(2):
        order = list(range(NCH)) if d_ == 0 else (list(range(NCc - 1, -1, -1)) + list(range(NCH - 1, NCc - 1, -1)))
        for ci, c in enumerate(order):
            U = Ub[ci % 2]
            fw.dma('sp', U.ap(), scr["UT"][:, c * L:(c + 1) * L].rearrange("(ct p) t -> p ct t", p=128), reads=[scr["UT"]], writes=[U], part=False)
            for ct in range(2):
                for g8 in range(8):
                    g = ct * 8 + g8
                    dg = d_ * 16 + g
                    k = it % 2
                    it += 1
                    self.mm(pBU[k][:, 0:L], Bexp[:, d_, g, 0, :], U[:, ct, :], True, True, [Bexp, U], [pBU[k]])
                    self.mm(pBS[k][:, 0:L], Bexp[:, d_, g, 1, :], U[:, ct, :], True, True, [Bexp, U], [pBS[k]])
                    i0 = pBU[k][:, 0:L] if d_ == 0 else _rev(pBU[k][:, 0:L])
                    i1 = pBS[k][:, 0:L] if d_ == 0 else _rev(pBS[k][:, 0:L])
                    self.tt(t1[k].ap(), i0, Ctab[:, dg, :], ALU.mult, [pBU[k], Ctab], [t1[k]])
                    self.tt(t2[k].ap(), i1, Stab[:, dg, :], ALU.mult, [pBS[k], Stab], [t2[k]])
                    self.tt(bp[k].ap(), t1[k].ap(), t2[k].ap(), ALU.add, [t1[k], t2[k]], [bp[k]])
                    gi = ginit_b[dg]
                    fw.op('dve', lambda e: e.tensor_tensor_scan(out=Gs[k].ap(), data0=R[:, dg:dg + 1].to_broadcast([128, L]), data1=bp[k].ap(),
                                                                 initial=ginit[:, dg:dg + 1], op0=ALU.mult, op1=ALU.add),
                          reads=[R, bp[k], gi], writes=[Gs[k]])
                    self.mm(pG[:, 2 * k:2 * k + 2], P2.ap(), Gs[k][:, L - 2:L], True, True, [P2, Gs[k]], [pGb[k]])
                    self.tt(G1[k].ap(), Gs[k].ap(), Ctab[:, dg, :], ALU.mult, [Gs[k], Ctab], [G1[k]])
                    self.tt(G2[k].ap(), Gs[k].ap(), Stab[:, dg, :], ALU.mult, [Gs[k], Stab], [G2[k]], eng='pool')
                    self.ts(gtb[k].ap(), Gs[k][:, L - 1:L], csL[:, dg:dg + 1], None, ALU.mult, None, [Gs[k], csL], [gtb[k]])
                    self.stt(ginit[:, dg:dg + 1], pG[:, 2 * k + 1:2 * k + 2], snL[:, dg:dg + 1], gtb[k].ap(), ALU.mult, ALU.add, [pGb[k], snL, gtb[k]], [gi])
                    self.mm(pY[ct][:, 0:L], W1e[:, g, :], G1[k].ap(), g8 == 0, False, [W1e, G1[k]], [pY[ct]])
                    self.mm(pY[ct][:, 0:L], W2e[:, g, :], G2[k].ap(), False, g8 == 7, [W2e, G2[k]], [pY[ct]])
                if d_ == 0:
                    self.cp(yacc[:, ct, c * L:(c + 1) * L], pY[ct][:, 0:L], [pY[ct]], [yacc], eng='act')
                else:
                    self.tt(yacc[:, ct, c * L:(c + 1) * L], yacc[:, ct, c * L:(c + 1) * L], _rev(pY[ct][:, 0:L]), ALU.add, [yacc, pY[ct]], [yacc])
            if d_ == 1:
                for ct in range(2):
                    self.stt(yy[:, ct, :], U[:, ct, :], dvec[:, ct:ct + 1], yacc[:, ct, c * L:(c + 1) * L], ALU.mult, ALU.add, [U, dvec, yacc], [yy])
                self.tt(y2.ap(), yy.ap(), yy.ap(), ALU.mult, [yy], [y2], eng='pool')
                self.ts(y2.ap(), y2.ap(), 0.044715, 1.0, ALU.mult, ALU.add, [y2], [y2])
                self.tt(y2.ap(), y2.ap(), yy.ap(), ALU.mult, [y2, yy], [y2], eng='pool')
                self.act(y2.ap(), y2.ap(), AF.Sigmoid, [y2], [y2], scale=1.5957691216057308)
                self.tt(zz.ap(), yy.ap(), y2.ap(), ALU.mult, [yy, y2], [zz])
                self.cp(zb.ap(), zz.ap(), [zz], [zb], eng='pool')
                for c2 in range(2):
                    for kt in range(2):
                        self.mm(pZ[:, 0:L], gluw[:, kt, c2 * 128:(c2 + 1) * 128], zb[:, kt, :], kt == 0, kt == 1, [gluw, zb], [pZ])
                    self.act(gg.ap(), pZ[:, 0:L], AF.Sigmoid, [pZ, glub], [gg], bias=glub[:, c2:c2 + 1])
                    Y = yo[c2]
                    self.tt(Y.ap(), zz[:, c2, :], gg.ap(), ALU.mult, [zz, gg], [Y])
                    fw.dma('act', scr["YST"][c2 * 128:(c2 + 1) * 128, c * L:(c + 1) * L], Y.ap(), reads=[Y], writes=[scr["YST"]])
    fw.pop()


Prog.phaseC = _phaseC


def _phaseD(self, l):
    fw, din, scr = self.fw, self.din, self.scr
    NCH = self.NTOK // 64
    NCc = C_LEN // 64
    fw.push()
    TRI = fw.sb("TRI", [64, 2, 64], F32); SU = fw.sb("SU", [64, 2, 64], F32)
    fw.dma('sp', TRI.ap(), din["k_tri"][0:64], writes=[TRI]); fw.dma('sp', SU.ap(), din["k_su"][0:64], writes=[SU])
    ghg = self.bcast_row("ghg", din["hg_norm_g"][l:l + 1, :], 64)
    oacc = fw.sb("oacc", [64, NCH, 256], F32)
    S = fw.sb("S", [64, 4, 64], F32); Sb = fw.sb("Sb", [64, 4, 64], BF16)
    HTb = [fw.sb("HTb%d" % i, [64, 7, 256], F32) for i in range(2)]
    HFb = [fw.sb("HFb%d" % i, [64, 3, 4, 64], F32) for i in range(2)]
    Vb = [fw.sb("Vb%d" % i, [64, 256], BF16) for i in range(2)]
    pB = fw.ps("pB", [64, 4, 64]); pE = fw.ps("pE", [64, 256]); pA = fw.ps("pA", [64, 256])
    pO1 = fw.ps("pO1", [64, 256]); pO2 = fw.ps("pO2", [64, 256]); pS = fw.ps("pS", [64, 4, 64])
    bT = fw.sb("bT", [64, 4, 64], F32); nbm = fw.sb("nbm", [64, 4], F32)
    eq = fw.sb("eq", [64, 4, 64], F32); ek = fw.sb("ek", [64, 4, 64], F32); eb = fw.sb("eb", [64, 4, 64], F32)
    qtl = fw.sb("qtl", [64, 4, 64], BF16); ktl = fw.sb("ktl", [64, 4, 64], BF16); qe = fw.sb("qe", [64, 4, 64], BF16)
    ex = fw.sb("ex", [64, 256], F32); khat = fw.sb("khat", [64, 256], BF16); attm = fw.sb("attm", [64, 256], BF16)
    o1 = fw.sb("o1", [64, 256], F32)
    for d_ in range(2):
        fw.op('dve', lambda e: e.memset(S.ap(), 0.0), writes=[S])
        fw.op('dve', lambda e: e.memset(Sb.ap(), 0.0), writes=[Sb])
        if d_ == 0:
            chunks = list(range(NCH))
        else:
            chunks = list(range(NCc - 1, -1, -1)) + list(range(NCH - 1, NCc - 1, -1))
        mid = 31 if d_ == 0 else 32
        last = 63 if d_ == 0 else 0
        for ti, c in enumerate(chunks):
            tok0 = c * 64
            HT = HTb[ti % 2]; HF = HFb[ti % 2]; V = Vb[ti % 2]
            fw.dma('sp', HT.ap(), scr["HGtok"][tok0:tok0 + 64, :, :], reads=[scr["HGtok"]], writes=[HT], part=False)
            fw.dma('act', HF.ap(), scr["HGfeat"][:, :, tok0:tok0 + 64].rearrange("s (h p) t -> p s h t", p=64), reads=[scr["HGfeat"]], writes=[HF], part=False)
            self.cp(V.ap(), HT[:, 5, :], [HT], [V], eng='pool')
            for h in range(4):
                self.mm(pB[:, h, :], HT[:, 3 + d_, h * 64:(h + 1) * 64], TRI[:, d_, :], True, True, [HT, TRI], [pB])
            self.mm(pE.ap(), SU[:, d_, :], HT[:, 3 + d_, :], True, True, [SU, HT], [pE])
            self.cp(bT.ap(), pB.ap(), [pB], [bT], eng='act')
            self.ts(nbm.ap(), bT[:, :, mid], -1.0, None, ALU.mult, None, [bT], [nbm])
            self.act(eb.ap(), bT.ap(), AF.Exp, [bT], [eb])
            for h in range(4):
                self.ts(eq[:, h, :], bT[:, h, :], nbm[:, h:h + 1], 40.0, ALU.add, ALU.min, [bT, nbm], [eq])
            self.ts(eq.ap(), eq.ap(), -40.0, None, ALU.max, None, [eq], [eq])
            self.act(ek.ap(), eq.ap(), AF.Exp, [eq], [ek], scale=-1.0)
            self.act(eq.ap(), eq.ap(), AF.Exp, [eq], [eq])
            self.tt(qtl.ap(), HF[:, 0, :, :], eq.ap(), ALU.mult, [HF, eq], [qtl])
            self.tt(ktl.ap(), HF[:, 1 + d_, :, :], ek.ap(), ALU.mult, [HF, ek], [ktl], eng='pool')
            self.tt(qe.ap(), HF[:, 0, :, :], eb.ap(), ALU.mult, [HF, eb], [qe], eng='pool')
            self.act(ex.ap(), pE.ap(), AF.Exp, [pE], [ex])
            self.tt(khat.ap(), HT[:, 1 + d_, :], ex.ap(), ALU.mult, [HT, ex], [khat])
            for h in range(4):
                self.mm(pA[:, h * 64:(h + 1) * 64], ktl[:, h, :], qtl[:, h, :], True, True, [ktl, qtl], [pA])
            self.tt(attm.ap().rearrange("p (h t) -> p h t", h=4), pA.ap().rearrange("p (h t) -> p h t", h=4),
                    TRI[:, d_, :].rearrange("p (o t) -> p o t", o=1).to_broadcast([64, 4, 64]), ALU.mult, [pA, TRI], [attm])
            for h in range(4):
                self.mm(pO1[:, h * 64:(h + 1) * 64], qe[:, h, :], Sb[:, h, :], True, True, [qe, Sb], [pO1])
            for h in range(4):
                self.mm(pO2[:, h * 64:(h + 1) * 64], attm[:, h * 64:(h + 1) * 64], V[:, h * 64:(h + 1) * 64], True, True, [attm, V], [pO2])
            self.cp(o1.ap(), pO1.ap(), [pO1], [o1], eng='act')
            if d_ == 0:
                self.tt(oacc[:, c, :], o1.ap(), pO2.ap(), ALU.add, [o1, pO2], [oacc])
            else:
                self.tt(o1.ap(), o1.ap(), pO2.ap(), ALU.add, [o1, pO2], [o1])
                self.tt(oacc[:, c, :], oacc[:, c, :], o1.ap(), ALU.add, [oacc, o1], [oacc], eng='pool')
            for h in range(4):
                self.mm(pS[:, h, :], khat[:, h * 64:(h + 1) * 64], V[:, h * 64:(h + 1) * 64], True, True, [khat, V], [pS])
            for h in range(4):
                self.stt(S[:, h, :], S[:, h, :], eb[:, h, last:last + 1], pS[:, h, :], ALU.mult, ALU.add, [S, eb, pS], [S])
            self.cp(Sb.ap(), S.ap(), [S], [Sb], eng='act')
    sqo = fw.sb("sqo", [64, 256], F32); ss4 = fw.sb("ss4", [64, 4], F32); yn = fw.sb("yn", [64, 256], F32)
    gob = [fw.sb("gob%d" % i, [64, 256], F32) for i in range(2)]
    yhb = [fw.sb("yhb%d" % i, [64, 256], BF16) for i in range(2)]
    yhT = [fw.sb("yhT%d" % i, [128, 2, 64], BF16) for i in range(2)]
    pTr = fw.ps("pTr", [128, 2, 64], BF16)
    for c in range(NCH):
        tok0 = c * 64
        go = gob[c % 2]
        fw.dma('sp', go.ap(), scr["HGtok"][tok0:tok0 + 64, 6, :], reads=[scr["HGtok"]], writes=[go], part=False)
        self.tt(sqo.ap(), oacc[:, c, :], oacc[:, c, :], ALU.mult, [oacc], [sqo], eng='pool')
        fw.op('dve', lambda e: e.tensor_reduce(out=ss4.ap(), in_=sqo.ap().rearrange("p (a d) -> p a d", d=64), axis=AX.X, op=ALU.add),
              reads=[sqo], writes=[ss4])
        self.rstd(ss4, 64)
        yn3 = yn.ap().rearrange("p (a d) -> p a d", d=64)
        self.tt(yn3, oacc[:, c, :].rearrange("p (a d) -> p a d", d=64),
                ss4.ap().rearrange("p (a o) -> p a o", o=1).to_broadcast([64, 4, 64]), ALU.mult, [oacc, ss4], [yn])
        self.tt(yn3, yn3, ghg[0:64, :].rearrange("p (o d) -> p o d", o=1).to_broadcast([64, 4, 64]), ALU.mult, [yn, ghg], [yn])
        Y = yhb[c % 2]
        self.tt(Y.ap(), yn.ap(), go.ap(), ALU.mult, [yn, go], [Y])
        for ct in range(2):
            self.tr(pTr[:, ct, :], Y[:, ct * 128:(ct + 1) * 128], self.identb[0:64, 0:64], [Y, self.identb], [pTr])
        YT = yhT[c % 2]
        self.cp(YT.ap(), pTr.ap(), [pTr], [YT], eng='act')
        fw.dma('act', scr["YHT"][:, tok0:tok0 + 64].rearrange("(ct p) t -> p ct t", p=128), YT.ap(), reads=[YT], writes=[scr["YHT"]])
    fw.pop()


Prog.phaseD = _phaseD


def _phaseE(self, l):
    fw, din, scr = self.fw, self.din, self.scr
    NCt, NTA, NE = self.NCt, self.NTA, self.NE
    NR = 4 + NE
    EPG = NE // 4
    fw.push()
    Wa = fw.sb("Wa", [128, 4, D], BF16); Ws = fw.sb("Ws", [128, 2, D], BF16); Wh = fw.sb("Wh", [128, 2, D], BF16)
    Wo = fw.sb("Wo", [128, 8, D], BF16)
    fw.dma('pool', Wa.ap(), din["w_o_att"][l].rearrange("(j p) n -> p j n", p=128), writes=[Wa])
    fw.dma('pool', Ws.ap(), din["w_o_s5"][l].rearrange("(j p) n -> p j n", p=128), writes=[Ws])
    fw.dma('pool', Wh.ap(), din["w_o_hg"][l].rearrange("(j p) n -> p j n", p=128), writes=[Wh])
    fw.dma('pool', Wo.ap(), din["w_out"][l].rearrange("(j p) n -> p j n", p=128), writes=[Wo])
    Wr = fw.sb("Wr", [128, 8, NR], F32)
    fw.dma('sp', Wr[:, :, 0:4], din["moe_w_group"][l].rearrange("(kc p) n -> p kc n", p=128), writes=[Wr])
    fw.dma('sp', Wr[:, :, 4:NR], din["moe_w_expert"][l].rearrange("(kc p) n -> p kc n", p=128), writes=[Wr])
    br = fw.sb("br", [128, NR], F32)
    fw.dma('sp', br[:, 0:4], din["moe_b_group"][l:l + 1, :].to_broadcast([128, 4]), writes=[br])
    fw.dma('sp', br[:, 4:NR], din["moe_b_expert"][l:l + 1, :].to_broadcast([128, NE]), writes=[br])
    gffn = self.bcast_row("gffn", din["norm_ffn_g"][l:l + 1, :], D)
    G1m, A2, SH2 = [], [], []
    for row in range(2 if l == 0 else 1):
        G1m.append(self.load_mod(l, row, 2, "g1m_%d" % row))
        sc = self.load_mod(l, row, 4, "sc2_%d" % row)
        self.stt(sc.ap(), sc.ap(), 1.0, gffn.ap(), ALU.add, ALU.mult, [sc, gffn], [sc])
        A2.append(sc)
        SH2.append(self.load_mod(l, row, 3, "sh2_%d" % row))
    ya = [fw.sb("ya%d" % i, [128, 4, 128], BF16) for i in range(2)]
    ys = [fw.sb("ys%d" % i, [128, 2, 128], BF16) for i in range(2)]
    yh = [fw.sb("yh%d" % i, [128, 2, 128], BF16) for i in range(2)]
    gb = [fw.sb("gb%d" % i, [128, 3 * D], BF16) for i in range(2)]
    xt = [fw.sb("xe%d" % i, [128, D], F32) for i in range(2)]
    x1 = [fw.sb("x1_%d" % i, [128, D], F32) for i in range(2)]
    m1 = fw.sb("m1", [128, 512], F32); m2 = fw.sb("m2", [128, 512], F32); m3 = fw.sb("m3", [128, 512], F32)
    mb = fw.sb("mb", [128, D], BF16); mT = fw.sb("mT", [128, 8, 128], BF16)
    junk = fw.sb("junkE", [128, D], F32); ssx = fw.sb("ssxE", [128, 1], F32)
    hxf = fw.sb("hxf", [128, D], F32); hT = fw.sb("hT", [128, 8, 128], F32)
    hTb = [fw.sb("hTb%d" % i, [128, 8, 128], BF16) for i in range(2)]
    lg = fw.sb("lg", [128, NR], F32); gmx = fw.sb("gmx", [128, 4], F32); ge = fw.sb("ge", [128, 4], F32)
    gmask = fw.sb("gmask", [128, 4], F32); pe = fw.sb("pe", [128, NE], F32); pe2 = fw.sb("pe2", [128, NE], F32)
    e1 = fw.sb("e1m", [128, NE], F32); e2 = fw.sb("e2m", [128, NE], F32)
    sc4 = fw.sb("sc4", [128, 8], F32)
    wvb = [fw.sb("wvb%d" % i, [128, NE], F32) for i in range(2)]
    e12b = [fw.sb("e12b%d" % i, [128, 2 * NE], BF16) for i in range(2)]
    w12b = [fw.sb("w12b%d" % i, [128, 2], F32) for i in range(2)]
    hxfb = [fw.sb("hxfb%d" % i, [128, D], BF16) for i in range(2)]
    pA_ = fw.ps("pEa", [128, 512]); pS_ = fw.ps("pEs", [128, 512]); pH_ = fw.ps("pEh", [128, 512])
    pT = fw.ps("pEt", [128, 8, 128], BF16); pO = [fw.ps("pEo%d" % i, [128, 512]) for i in range(2)]
    pTf = fw.ps("pEtf", [128, 8, 128]);
    tiles = list(range(NTA)) if l == 0 else list(range(NCt, NTA))
    for ti, i in enumerate(tiles):
        b = ti % 2
        row = 1 if i < NCt else 0
        tok0 = i * 128
        fw.dma('sp', ya[b].ap(), scr["YAT"][:, tok0:tok0 + 128].rearrange("(j p) t -> p j t", p=128), reads=[scr["YAT"]], writes=[ya[b]], part=False)
        fw.dma('act', ys[b].ap(), scr["YST"][:, tok0:tok0 + 128].rearrange("(j p) t -> p j t", p=128), reads=[scr["YST"]], writes=[ys[b]], part=False)
        fw.dma('act', yh[b].ap(), scr["YHT"][:, tok0:tok0 + 128].rearrange("(j p) t -> p j t", p=128), reads=[scr["YHT"]], writes=[yh[b]], part=False)
        fw.dma('sp', gb[b].ap(), scr["G"][tok0:tok0 + 128, :], reads=[scr["G"]], writes=[gb[b]], part=False)
        if l == 0:
            src = din["ctx"] if i < NCt else din["x"]
            sap = src[tok0:tok0 + 128, :] if i < NCt else src[tok0 - C_LEN:tok0 - C_LEN + 128, :]
        else:
            src = scr["xb"]; sap = src[tok0:tok0 + 128, :]
        fw.dma('sp', xt[b].ap(), sap, reads=[src], writes=[xt[b]], part=False)
        for half in range(2):
            c0 = half * 512
            for j in range(4):
                self.mm(pA_.ap(), ya[b][:, j, :], Wa[:, j, c0:c0 + 512], j == 0, j == 3, [ya[b], Wa], [pA_])
            for j in range(2):
                self.mm(pS_.ap(), ys[b][:, j, :], Ws[:, j, c0:c0 + 512], j == 0, j == 1, [ys[b], Ws], [pS_])
            for j in range(2):
                self.mm(pH_.ap(), yh[b][:, j, :], Wh[:, j, c0:c0 + 512], j == 0, j == 1, [yh[b], Wh], [pH_])
            self.tt(m1.ap(), pA_.ap(), gb[b][:, c0:c0 + 512], ALU.mult, [pA_, gb[b]], [m1])
            self.tt(m2.ap(), pS_.ap(), gb[b][:, D + c0:D + c0 + 512], ALU.mult, [pS_, gb[b]], [m2])
            self.tt(m3.ap(), pH_.ap(), gb[b][:, 2 * D + c0:2 * D + c0 + 512], ALU.mult, [pH_, gb[b]], [m3])
            self.tt(m1.ap(), m1.ap(), m2.ap(), ALU.add, [m1, m2], [m1])
            self.tt(mb[:, c0:c0 + 512], m1.ap(), m3.ap(), ALU.add, [m1, m3], [mb])
        for kc in range(8):
            self.tr(pT[:, kc, :], mb[:, kc * 128:(kc + 1) * 128], self.identb.ap(), [mb, self.identb], [pT])
        self.cp(mT.ap(), pT.ap(), [pT], [mT], eng='act')
        X1 = x1[b]
        for half in range(2):
            c0 = half * 512
            for kc in range(8):
                self.mm(pO[half].ap(), mT[:, kc, :], Wo[:, kc, c0:c0 + 512], kc == 0, kc == 7, [mT, Wo], [pO[half]])
            self.tt(m1.ap(), pO[half].ap(), G1m[row][:, c0:c0 + 512], ALU.mult, [pO[half], G1m[row]], [m1])
            self.tt(X1[:, c0:c0 + 512], m1.ap(), xt[b][:, c0:c0 + 512], ALU.add, [m1, xt[b]], [X1])
        fw.dma('sp', scr["xa"][tok0:tok0 + 128, :], X1.ap(), reads=[X1], writes=[scr["xa"]])
        self.act(junk.ap(), X1.ap(), AF.Square, [X1], [junk, ssx], accum_out=ssx.ap())
        self.rstd(ssx, D)
        self.stt(junk.ap(), X1.ap(), ssx.ap(), A2[row].ap(), ALU.mult, ALU.mult, [X1, ssx, A2[row]], [junk])
        self.tt(hxf.ap(), junk.ap(), SH2[row].ap(), ALU.add, [junk, SH2[row]], [hxf])
        for kc in range(8):
            self.tr(pTf[:, kc, :], hxf[:, kc * 128:(kc + 1) * 128], self.identf.ap(), [hxf, self.identf], [pTf])
        self.cp(hT.ap(), pTf.ap(), [pTf], [hT], eng='act')
        self.cp(hTb[b].ap(), hT.ap(), [hT], [hTb[b]], eng='act')
        fw.dma('act', scr["HXFT"][:, :, tok0:tok0 + 128], hTb[b].ap(), reads=[hTb[b]], writes=[scr["HXFT"]])
        pR = pA_
        for kc in range(8):
            self.mm(pR[:, 0:NR], hT[:, kc, :], Wr[:, kc, :], kc == 0, kc == 7, [hT, Wr], [pR])
        self.tt(lg.ap(), pR[:, 0:NR], br.ap(), ALU.add, [pR, br], [lg])
        fw.op('dve', lambda e: e.tensor_reduce(out=sc4[:, 0:1], in_=lg[:, 0:4], axis=AX.X, op=ALU.max), reads=[lg], writes=[sc4])
        self.ts(sc4[:, 1:2], sc4[:, 0:1], -1.0, None, ALU.mult, None, [sc4], [sc4])
        self.act(ge.ap(), lg[:, 0:4], AF.Exp, [lg, sc4], [ge], bias=sc4[:, 1:2])
        fw.op('dve', lambda e: e.tensor_reduce(out=sc4[:, 2:3], in_=ge.ap(), axis=AX.X, op=ALU.add), reads=[ge], writes=[sc4])
        fw.op('dve', lambda e: e.reciprocal(out=sc4[:, 2:3], in_=sc4[:, 2:3]), reads=[sc4], writes=[sc4])
        self.ts(gmask.ap(), lg[:, 0:4], sc4[:, 0:1], None, ALU.is_ge, None, [lg, sc4], [gmask])
        fw.op('dve', lambda e: e.tensor_reduce(out=sc4[:, 3:4], in_=lg[:, 4:NR], axis=AX.X, op=ALU.max), reads=[lg], writes=[sc4])
        self.ts(sc4[:, 3:4], sc4[:, 3:4], -1.0, None, ALU.mult, None, [sc4], [sc4])
        self.act(pe.ap(), lg[:, 4:NR], AF.Exp, [lg, sc4], [pe], bias=sc4[:, 3:4])
        self.tt(pe.ap().rearrange("p (g e) -> p g e", g=4), pe.ap().rearrange("p (g e) -> p g e", g=4),
                gmask.ap().rearrange("p (g o) -> p g o", o=1).to_broadcast([128, 4, EPG]), ALU.mult, [pe, gmask], [pe])
        fw.op('dve', lambda e: e.tensor_reduce(out=sc4[:, 4:5], in_=pe.ap(), axis=AX.X, op=ALU.max), reads=[pe], writes=[sc4])
        self.ts(e1.ap(), pe.ap(), sc4[:, 4:5], None, ALU.is_ge, None, [pe, sc4], [e1])
        self.tt(pe2.ap(), e1.ap(), pe.ap(), ALU.mult, [e1, pe], [pe2])
        self.tt(pe2.ap(), pe.ap(), pe2.ap(), ALU.subtract, [pe, pe2], [pe2])
        fw.op('dve', lambda e: e.tensor_reduce(out=sc4[:, 5:6], in_=pe2.ap(), axis=AX.X, op=ALU.max), reads=[pe2], writes=[sc4])
        self.ts(e2.ap(), pe2.ap(), sc4[:, 5:6], None, ALU.is_ge, None, [pe2, sc4], [e2])
        self.tt(sc4[:, 6:7], sc4[:, 4:5], sc4[:, 5:6], ALU.add, [sc4], [sc4])
        fw.op('dve', lambda e: e.reciprocal(out=sc4[:, 6:7], in_=sc4[:, 6:7]), reads=[sc4], writes=[sc4])
        self.tt(sc4[:, 6:7], sc4[:, 6:7], sc4[:, 2:3], ALU.mult, [sc4], [sc4])
        self.tt(sc4[:, 7:8], sc4[:, 5:6], sc4[:, 6:7], ALU.mult, [sc4], [sc4])
        self.tt(sc4[:, 6:7], sc4[:, 4:5], sc4[:, 6:7], ALU.mult, [sc4], [sc4])
        WVt = wvb[b]
        self.ts(WVt.ap(), e1.ap(), sc4[:, 6:7], None, ALU.mult, None, [e1, sc4], [WVt])
        self.stt(WVt.ap(), e2.ap(), sc4[:, 7:8], WVt.ap(), ALU.mult, ALU.add, [e2, sc4, WVt], [WVt])
        fw.dma('sp', scr["WV"][tok0:tok0 + 128, :], WVt.ap(), reads=[WVt], writes=[scr["WV"]])
        E12t = e12b[b]
        self.cp(E12t[:, 0:NE], e1.ap(), [e1], [E12t], eng='pool')
        self.cp(E12t[:, NE:2 * NE], e2.ap(), [e2], [E12t], eng='pool')
        fw.dma('sp', scr["E12"][tok0:tok0 + 128, :], E12t.ap(), reads=[E12t], writes=[scr["E12"]])
        self.cp(w12b[b].ap(), sc4[:, 6:8], [sc4], [w12b[b]], eng='pool')
        fw.dma('sp', scr["W12"][tok0:tok0 + 128, :], w12b[b].ap(), reads=[w12b[b]], writes=[scr["W12"]])
        self.cp(hxfb[b].ap(), hxf.ap(), [hxf], [hxfb[b]], eng='act')
        fw.dma('act', scr["HXFtok"][tok0:tok0 + 128, :], hxfb[b].ap(), reads=[hxfb[b]], writes=[scr["HXFtok"]])
    fw.pop()


def _phaseF(self, l):
    fw, din, scr = self.fw, self.din, self.scr
    NCt, NTA, NE = self.NCt, self.NTA, self.NE
    fw.push()
    G2m = [self.load_mod(l, row, 5, "g2m_%d" % row) for row in range(2 if l == 0 else 1)]
    GT = 8
    groups = []
    if l == 0:
        groups.append(list(range(NCt)))
    for t0 in range(NCt, NTA, GT):
        groups.append(list(range(t0, min(NTA, t0 + GT))))
    hx = fw.sb("hxF", [128, 8, GT * 128], BF16)
    wv = fw.sb("wvF", [128, GT, NE], F32)
    acc = fw.sb("accF", [128, GT, D], F32)
    Wg = [fw.sb("Wg%d" % i, [128, 8, 512], BF16) for i in range(2)]
    Wu = [fw.sb("Wu%d" % i, [128, 8, 512], BF16) for i in range(2)]
    Wd = [fw.sb("Wd%d" % i, [128, 4, D], BF16) for i in range(2)]
    actb = [fw.sb("actb%d" % i, [128, 4, 512], BF16) for i in range(2)]
    sg = [fw.sb("sgF%d" % i, [128, 512], F32) for i in range(2)]
    xr = [fw.sb("xrF%d" % i, [128, D], F32) for i in range(2)]
    pG = [fw.ps("pFg%d" % i, [128, 512]) for i in range(2)]
    pU = [fw.ps("pFu%d" % i, [128, 512]) for i in range(2)]
    pY = [fw.ps("pFy%d" % i, [128, 512]) for i in range(2)]
    wi = 0; ci = 0; yi = 0; ai = 0
    for grp in groups:
        ntl = len(grp); ntok = ntl * 128; tok0 = grp[0] * 128
        fw.dma('sp', hx[:, :, 0:ntok], scr["HXFT"][:, :, tok0:tok0 + ntok], reads=[scr["HXFT"]], writes=[hx], part=False)
        fw.dma('act', wv[:, 0:ntl, :], scr["WV"][tok0:tok0 + ntok, :].rearrange("(t p) e -> p t e", p=128), reads=[scr["WV"]], writes=[wv], part=False)
        fw.op('pool', lambda e: e.memset(acc.ap(), 0.0), writes=[acc])
        for e_ in range(NE):
            k = wi % 2; wi += 1
            fw.dma('pool', Wg[k].ap(), din["moe_w_gate"][l, e_].rearrange("(kc p) n -> p kc n", p=128), writes=[Wg[k]], part=False)
            fw.dma('pool', Wu[k].ap(), din["moe_w_up"][l, e_].rearrange("(kc p) n -> p kc n", p=128), writes=[Wu[k]], part=False)
            fw.dma('pool', Wd[k].ap(), din["moe_w_down"][l, e_].rearrange("(dc p) n -> p dc n", p=128), writes=[Wd[k]], part=False)
            for tb in range((ntok + 511) // 512):
                nt = min(512, ntok - tb * 512)
                AB = actb[ai % 2]; ai += 1
                for dc in range(4):
                    g_ = pG[ci % 2]; u_ = pU[ci % 2]; s_ = sg[ci % 2]; ci += 1
                    for kc in range(8):
                        self.mm(g_[:, 0:nt], Wg[k][:, kc, dc * 128:(dc + 1) * 128], hx[:, kc, tb * 512:tb * 512 + nt], kc == 0, kc == 7, [Wg[k], hx], [g_])
                    for kc in range(8):
                        self.mm(u_[:, 0:nt], Wu[k][:, kc, dc * 128:(dc + 1) * 128], hx[:, kc, tb * 512:tb * 512 + nt], kc == 0, kc == 7, [Wu[k], hx], [u_])
                    self.act(s_[:, 0:nt], g_[:, 0:nt], AF.Silu, [g_], [s_])
                    self.tt(AB[:, dc, 0:nt], s_[:, 0:nt], u_[:, 0:nt], ALU.mult, [s_, u_], [AB])
                for ts_ in range(nt // 128):
                    tl = tb * 4 + ts_
                    for ch in range(2):
                        y_ = pY[yi % 2]; yi += 1
                        for dc in range(4):
                            self.mm(y_.ap(), AB[:, dc, ts_ * 128:(ts_ + 1) * 128], Wd[k][:, dc, ch * 512:(ch + 1) * 512], dc == 0, dc == 3, [AB, Wd[k]], [y_])
                        self.stt(acc[:, tl, ch * 512:(ch + 1) * 512], y_.ap(), wv[:, tl, e_:e_ + 1], acc[:, tl, ch * 512:(ch + 1) * 512],
                                 ALU.mult, ALU.add, [y_, wv, acc], [acc])
        for j, i in enumerate(grp):
            row = 1 if i < NCt else 0
            X = xr[j % 2]
            fw.dma('sp', X.ap(), scr["xa"][i * 128:(i + 1) * 128, :], reads=[scr["xa"]], writes=[X], part=False)
            self.tt(acc[:, j, :], acc[:, j, :], G2m[row].ap(), ALU.mult, [acc, G2m[row]], [acc], eng='pool')
            self.tt(X.ap(), X.ap(), acc[:, j, :], ALU.add, [X, acc], [X])
            if l == 0:
                fw.dma('act', scr["xb"][i * 128:(i + 1) * 128, :], X.ap(), reads=[X], writes=[scr["xb"]])
            else:
                fw.dma('act', self.out[i * 128 - C_LEN:(i + 1) * 128 - C_LEN, :], X.ap(), reads=[X], writes=[self.out], final=True)
    fw.pop()


def _phaseFs(self, l):
    fw, din, scr = self.fw, self.din, self.scr
    NCt, NTA, NE = self.NCt, self.NTA, self.NE
    IO = bass.IndirectOffsetOnAxis
    tiles = list(range(NTA)) if l == 0 else list(range(NCt, NTA))
    nt_ = len(tiles)
    t_lo = tiles[0]
    NBLK = (2 * nt_ * 128) // 128 + NE
    fw.push()
    G2m = [self.load_mod(l, row, 5, "g2m_%d" % row) for row in range(2 if l == 0 else 1)]
    onesb = self.onesb
    slt = fw.sb("slt", [128, 128], BF16)
    fw.dma('pool', slt.ap(), din["k_slt"].ap(), writes=[slt])
    tokid = fw.sb("tokid", [128, NTA], I32); bstart = fw.sb("bstart", [128, NBLK], F32); pidx = fw.sb("pidx", [128, 1], F32)
    fw.dma('sp', tokid.ap(), din["k_tokid"].ap(), writes=[tokid]); fw.dma('sp', bstart.ap(), din["k_bstart"][:, 0:NBLK], writes=[bstart])
    fw.dma('sp', pidx.ap(), din["k_pidx"].ap(), writes=[pidx])
    E12a = fw.sb("E12a", [128, nt_, 2 * NE], BF16); W12a = fw.sb("W12a", [128, nt_, 2], F32)
    fw.dma('sp', E12a.ap(), scr["E12"][t_lo * 128:(t_lo + nt_) * 128, :].rearrange("(t p) c -> p t c", p=128), reads=[scr["E12"]], writes=[E12a])
    fw.dma('sp', W12a.ap(), scr["W12"][t_lo * 128:(t_lo + nt_) * 128, :].rearrange("(t p) c -> p t c", p=128), reads=[scr["W12"]], writes=[W12a])
    zi = fw.sb("zi", [128, NBLK], I32)
    fw.op('dve', lambda e: e.memset(zi.ap(), 0), writes=[zi])
    fw.dma('sp', scr["BUFTOK"][0:NBLK * 128, :].rearrange("(p b) o -> p (b o)", p=128), zi.ap(), reads=[zi], writes=[scr["BUFTOK"]], part=False)
    IDXG = fw.sb("IDXG", [128, 1, NBLK], I32)
    DST = fw.sb("DST", [128, nt_, 2], I32)
    fw.push()
    pC = fw.ps("pC", [128, 2 * NE]); pR = [fw.ps("pR%d" % i, [128, 2 * NE]) for i in range(2)]; pT2 = [fw.ps("pT2%d" % i, [128, 2 * NE]) for i in range(2)]
    for j in range(nt_):
        self.mm(pC.ap(), onesb.ap(), E12a[:, j, :], j == 0, j == nt_ - 1, [onesb, E12a], [pC])
    cnt = fw.sb("cnt", [128, 2 * NE], F32); tot = fw.sb("tot", [128, NE], F32); toti = fw.sb("toti", [128, NE], I32)
    padd = fw.sb("padd", [128, NE], F32); pend = fw.sb("pend", [128, NE], F32); ones32 = fw.sb("ones32", [128, NE], F32)
    carry = fw.sb("carry", [128, 2 * NE], F32)
    self.cp(cnt.ap(), pC.ap(), [pC], [cnt])
    self.tt(tot.ap(), cnt[:, 0:NE], cnt[:, NE:2 * NE], ALU.add, [cnt], [tot])
    qf = fw.sb("qf", [128, NE], F32); qt = fw.sb("qt", [128, NE], F32)
    self.ts(qf.ap(), tot.ap(), 1.0 / 128, None, ALU.mult, None, [tot], [qf])
    self.cp(toti.ap(), qf.ap(), [qf], [toti])
    self.cp(qf.ap(), toti.ap(), [toti], [qf])
    self.stt(qt.ap(), qf.ap(), 128.0, tot.ap(), ALU.mult, ALU.is_lt, [qf, tot], [qt])
    self.tt(qf.ap(), qf.ap(), qt.ap(), ALU.add, [qf, qt], [qf])
    self.ts(qt.ap(), qf.ap(), -1.0, 128.0, ALU.add, ALU.mult, [qf], [qt])
    self.tt(qt.ap(), qt.ap(), tot.ap(), ALU.is_ge, [qt, tot], [qt])
    self.tt(qf.ap(), qf.ap(), qt.ap(), ALU.subtract, [qf, qt], [qf])
    self.ts(padd.ap(), qf.ap(), 128.0, None, ALU.mult, None, [qf], [padd])
    fw.op('dve', lambda e: e.memset(ones32.ap(), 1.0), writes=[ones32])
    fw.op('dve', lambda e: e.tensor_tensor_scan(out=pend.ap(), data0=ones32.ap(), data1=padd.ap(), initial=0.0, op0=ALU.mult, op1=ALU.add),
          reads=[ones32, padd], writes=[pend])
    self.tt(carry[:, 0:NE], pend.ap(), padd.ap(), ALU.subtract, [pend, padd], [carry])
    self.tt(carry[:, NE:2 * NE], carry[:, 0:NE], cnt[:, 0:NE], ALU.add, [carry, cnt], [carry])
    blke = fw.sb("blke", [128, NBLK], F32); tmpb = fw.sb("tmpb", [128, NBLK], F32)
    fw.op('dve', lambda e: e.memset(blke.ap(), 0.0), writes=[blke])
    for e_ in range(NE):
        self.ts(tmpb.ap(), bstart.ap(), pend[:, e_:e_ + 1], None, ALU.is_ge, None, [bstart, pend], [tmpb])
        self.tt(blke.ap(), blke.ap(), tmpb.ap(), ALU.add, [blke, tmpb], [blke])
    self.ts(blke.ap(), blke.ap(), float(NE - 1), None, ALU.min, None, [blke], [blke])
    same2 = fw.sb("same2", [128, NBLK], F32)
    fw.op('dve', lambda e: e.memset(same2.ap(), 0.0), writes=[same2])
    self.tt(same2[:, 1:NBLK], blke[:, 1:NBLK], blke[:, 0:NBLK - 1], ALU.is_equal, [blke], [same2])
    self.ts(tmpb.ap(), blke.ap(), float(l * NE), 128.0, ALU.add, ALU.mult, [blke], [tmpb])
    self.ts(tmpb.ap(), tmpb.ap(), pidx.ap(), None, ALU.add, None, [tmpb, pidx], [tmpb])
    self.stt(tmpb.ap(), same2.ap(), 1.0e6, tmpb.ap(), ALU.mult, ALU.add, [same2, tmpb], [tmpb])
    self.cp(IDXG[:, 0, :], tmpb.ap(), [tmpb], [IDXG])
    rank = fw.sb("rank", [128, 2 * NE], F32); dstf = fw.sb("dstf", [128, 2], F32)
    for j in range(nt_):
        k = j % 2
        self.mm(pR[k].ap(), slt.ap(), E12a[:, j, :], True, True, [slt, E12a], [pR[k]])
        self.mm(pT2[k].ap(), onesb.ap(), E12a[:, j, :], True, True, [onesb, E12a], [pT2[k]])
        self.tt(rank.ap(), pR[k].ap(), carry.ap(), ALU.add, [pR[k], carry], [rank])
        self.tt(rank.ap(), rank.ap(), E12a[:, j, :], ALU.mult, [rank, E12a], [rank])
        fw.op('dve', lambda e: e.tensor_reduce(out=dstf.ap(), in_=rank.ap().rearrange("p (a e) -> p a e", a=2), axis=AX.X, op=ALU.add),
              reads=[rank], writes=[dstf])
        self.cp(DST[:, j, :], dstf.ap(), [dstf], [DST])
        self.tt(carry.ap(), carry.ap(), pT2[k].ap(), ALU.add, [carry, pT2[k]], [carry])
        for a in range(2):
            if self.stop == "F0":
                continue
            fw.idma(lambda e: e.indirect_dma_start(out=scr["BUFTOK"][:, :], out_offset=IO(ap=DST[:, j, a:a + 1], axis=0),
                                                   in_=tokid[:, tiles[j]:tiles[j] + 1], in_offset=None),
                    reads=[DST, tokid], writes=[scr["BUFTOK"]], part=(j > 0 or a > 0))
    if self.stop == "F0":
        fw.dma('sp', scr["dDST"][:, 0:nt_, :], DST.ap(), reads=[DST], writes=[scr["dDST"]])
        fw.dma('sp', scr["dIDXG"][:, :, 0:NBLK], IDXG.ap(), reads=[IDXG], writes=[scr["dIDXG"]])
        fw.dma('sp', scr["dBLKE"][:, 0:NBLK], blke.ap(), reads=[blke], writes=[scr["dBLKE"]])
        fw.dma('sp', scr["dPEND"].ap(), pend.ap(), reads=[pend], writes=[scr["dPEND"]])
        fw.dma('sp', scr["dCNT"].ap(), cnt.ap(), reads=[cnt], writes=[scr["dCNT"]])
        fw.pop(); fw.pop()
        return
    fw.pop()
    if self.stop == "F1":
        fw.pop()
        return
    tki = [fw.sb("tki%d" % i, [128, 1], I32) for i in range(2)]
    xg = [fw.sb("xg%d" % i, [128, D], BF16) for i in range(2)]
    xgT = [fw.sb("xgT%d" % i, [128, 8, 128], BF16) for i in range(2)]
    Wg = [fw.sb("Wg%d" % i, [128, 8, 512], BF16) for i in range(2)]
    Wu = [fw.sb("Wu%d" % i, [128, 8, 512], BF16) for i in range(2)]
    Wd = [fw.sb("Wd%d" % i, [128, 4, D], BF16) for i in range(2)]
    sgt = [fw.sb("sgt%d" % i, [128, 512], F32) for i in range(2)]
    ab = [fw.sb("ab%d" % i, [128, 512], BF16) for i in range(2)]
    aT = [fw.sb("aT%d" % i, [128, 4, 128], BF16) for i in range(2)]
    yb = [fw.sb("ybF%d" % i, [128, D], BF16) for i in range(2)]
    pX = fw.ps("pFx", [128, 8, 128], BF16); pGt = fw.ps("pFg", [128, 512]); pUt = fw.ps("pFu", [128, 512])
    pAT = fw.ps("pFa", [128, 4, 128], BF16); pY = [fw.ps("pFy%d" % i, [128, 512]) for i in range(2)]
    wgate = din["moe_w_gate"].ap().rearrange("l e (p kc) n -> (l e p) (kc n)", kc=8)
    wup = din["moe_w_up"].ap().rearrange("l e (p kc) n -> (l e p) (kc n)", kc=8)
    wdown = din["moe_w_down"].ap().rearrange("l e (p dc) n -> (l e p) (dc n)", dc=4)
    for b in range(NBLK):
        k = b % 2
        fw.dma('sp', tki[k].ap(), scr["BUFTOK"][b * 128:(b + 1) * 128, :], reads=[scr["BUFTOK"]], writes=[tki[k]], part=False)
        fw.idma(lambda e: e.indirect_dma_start(out=xg[k].ap(), out_offset=None, in_=scr["HXFtok"][:, :], in_offset=IO(ap=tki[k].ap(), axis=0)),
                reads=[tki[k], scr["HXFtok"]], writes=[xg[k]], part=False)
        BC = self.bcreg
        ix = IDXG[:, 0, b:b + 1]
        fw.idma(lambda e: e.indirect_dma_start(out=Wg[0].ap().rearrange("p k n -> p (k n)"), out_offset=None, in_=wgate, in_offset=IO(ap=ix, axis=0),
                                               bounds_check=BC, oob_is_err=False), reads=[IDXG], writes=[Wg[0]], part=False)
        fw.idma(lambda e: e.indirect_dma_start(out=Wu[0].ap().rearrange("p k n -> p (k n)"), out_offset=None, in_=wup, in_offset=IO(ap=ix, axis=0),
                                               bounds_check=BC, oob_is_err=False), reads=[IDXG], writes=[Wu[0]], part=False)
        fw.idma(lambda e: e.indirect_dma_start(out=Wd[0].ap().rearrange("p k n -> p (k n)"), out_offset=None, in_=wdown, in_offset=IO(ap=ix, axis=0),
                                               bounds_check=BC, oob_is_err=False), reads=[IDXG], writes=[Wd[0]], part=False)
        for kc in range(8):
            self.tr(pX[:, kc, :], xg[k].ap().rearrange("t (p kc) -> t kc p", kc=8)[:, kc, :], self.identb.ap(), [xg[k], self.identb], [pX])
        self.cp(xgT[k].ap(), pX.ap(), [pX], [xgT[k]], eng='act')
        for kc in range(8):
            self.mm(pGt.ap(), xgT[k][:, kc, :], Wg[0][:, kc, :], kc == 0, kc == 7, [xgT[k], Wg[0]], [pGt])
        for kc in range(8):
            self.mm(pUt.ap(), xgT[k][:, kc, :], Wu[0][:, kc, :], kc == 0, kc == 7, [xgT[k], Wu[0]], [pUt])
        self.act(sgt[k].ap(), pGt.ap(), AF.Silu, [pGt], [sgt[k]])
        self.tt(ab[k].ap(), sgt[k].ap(), pUt.ap(), ALU.mult, [sgt[k], pUt], [ab[k]])
        for dc in range(4):
            self.tr(pAT[:, dc, :], ab[k].ap().rearrange("t (p dc) -> t dc p", dc=4)[:, dc, :], self.identb.ap(), [ab[k], self.identb], [pAT])
        self.cp(aT[k].ap(), pAT.ap(), [pAT], [aT[k]])
        for hh in range(2):
            for dc in range(4):
                self.mm(pY[hh].ap(), aT[k][:, dc, :], Wd[0][:, dc, hh * 512:(hh + 1) * 512], dc == 0, dc == 3, [aT[k], Wd[0]], [pY[hh]])
            self.cp(yb[k][:, hh * 512:(hh + 1) * 512], pY[hh].ap(), [pY[hh]], [yb[k]], eng='act')
        fw.dma('sp', scr["YBUF"][b * 128:(b + 1) * 128, :], yb[k].ap(), reads=[yb[k]], writes=[scr["YBUF"]])
    y1 = [fw.sb("y1c%d" % i, [128, D], BF16) for i in range(2)]
    y2 = [fw.sb("y2c%d" % i, [128, D], BF16) for i in range(2)]
    ycf = [fw.sb("ycf%d" % i, [128, D], F32) for i in range(2)]
    xr = [fw.sb("xrF%d" % i, [128, D], F32) for i in range(2)]
    for j, i in enumerate(tiles):
        k = j % 2
        row = 1 if i < NCt else 0
        fw.idma(lambda e: e.indirect_dma_start(out=y1[k].ap(), out_offset=None, in_=scr["YBUF"][:, :], in_offset=IO(ap=DST[:, j, 0:1], axis=0)),
                reads=[DST, scr["YBUF"]], writes=[y1[k]], part=False)
        fw.idma(lambda e: e.indirect_dma_start(out=y2[k].ap(), out_offset=None, in_=scr["YBUF"][:, :], in_offset=IO(ap=DST[:, j, 1:2], axis=0)),
                reads=[DST, scr["YBUF"]], writes=[y2[k]], part=False)
        X = xr[k]
        fw.dma('sp', X.ap(), scr["xa"][i * 128:(i + 1) * 128, :], reads=[scr["xa"]], writes=[X], part=False)
        Yc = ycf[k]
        self.ts(Yc.ap(), y1[k].ap(), W12a[:, j, 0:1], None, ALU.mult, None, [y1[k], W12a], [Yc])
        self.stt(Yc.ap(), y2[k].ap(), W12a[:, j, 1:2], Yc.ap(), ALU.mult, ALU.add, [y2[k], W12a, Yc], [Yc])
        self.tt(Yc.ap(), Yc.ap(), G2m[row].ap(), ALU.mult, [Yc, G2m[row]], [Yc])
        self.tt(X.ap(), X.ap(), Yc.ap(), ALU.add, [X, Yc], [X])
        if l == 0:
            fw.dma('act', scr["xb"][i * 128:(i + 1) * 128, :], X.ap(), reads=[X], writes=[scr["xb"]])
        else:
            fw.dma('act', self.out[i * 128 - C_LEN:(i + 1) * 128 - C_LEN, :], X.ap(), reads=[X], writes=[self.out], final=True)
    fw.pop()


Prog.phaseFs = _phaseFs
Prog.phaseE = _phaseE
Prog.phaseF = _phaseF

def host_consts(T, NE=32):
    NT = T // 128
    k = {}
    nblk = (2 * (T + C_LEN)) // 128 + NE
    k["k_bstart"] = np.broadcast_to((np.arange(nblk, dtype=np.float32) * 128.0)[None, :], (128, nblk)).copy()
    k["k_ident"] = np.eye(128, dtype=np.float32)
    t = np.arange(T)
    n_freq = 16
    inv = (10000.0 ** (-np.arange(n_freq, dtype=np.float32) / n_freq)).astype(np.float32)
    ang = np.concatenate([(t // 64).astype(np.float32)[:, None] * inv, (t % 64).astype(np.float32)[:, None] * inv], -1)
    k["k_ropeC"] = np.ascontiguousarray(np.cos(ang).astype(np.float32).reshape(NT, 128, 32).transpose(1, 0, 2))
    k["k_ropeS"] = np.ascontiguousarray(np.sin(ang).astype(np.float32).reshape(NT, 128, 32).transpose(1, 0, 2))
    s_ = np.arange(128)[:, None] % 64
    t_ = np.arange(64)[None, :]
    tri = np.stack([(s_ <= t_), (s_ >= t_)], 1).astype(np.float32)
    su = np.stack([(s_ > t_), (s_ < t_)], 1).astype(np.float32)
    k["k_tri"], k["k_su"] = tri, su
    tb = np.zeros((128, 2, 128), np.float32); sb_ = np.zeros((128, 2, 128), np.float32)
    for hf in range(2):
        tb[hf * 64:(hf + 1) * 64, :, hf * 64:(hf + 1) * 64] = tri[hf * 64:(hf + 1) * 64]
        sb_[hf * 64:(hf + 1) * 64, :, hf * 64:(hf + 1) * 64] = su[hf * 64:(hf + 1) * 64]
    k["k_tribd"], k["k_subd"] = tb, sb_
    p = np.arange(128)
    k["k_rowmask"] = (p[:, None] // 16 == np.arange(8)[None, :]).astype(np.float32)
    cm = np.zeros((128, 8, 128), np.float32)
    for g in range(8):
        cm[:, g, g * 16:(g + 1) * 16] = 1.0
    k["k_colmask"] = cm
    sg = np.ones((128, 2), np.float32)
    sg[64:, 0] = -1.0
    sg[:, 1] = -1.0
    k["k_sgn"] = sg
    p2 = np.zeros((128, 128), np.float32)
    for m in range(64):
        p2[m + 64, m] = -1.0
        p2[m, m + 64] = 1.0
    k["k_p2"] = p2
    NTA_ = NT + C_LEN // 128
    k["k_tokid"] = (np.arange(NTA_, dtype=np.int32)[None, :] * 128 + np.arange(128, dtype=np.int32)[:, None]).astype(np.int32)
    k["k_pidx"] = np.arange(128, dtype=np.float32).reshape(128, 1)
    k["k_slt"] = (np.arange(128)[:, None] < np.arange(128)[None, :]).astype(np.float32)
    k["k_iota"] = np.broadcast_to(np.arange(512, dtype=np.float32)[None, :], (128, 512)).copy()
    return k


def core_inputs(inputs, b, T, NE):
    f = lambda a: np.ascontiguousarray(np.asarray(a, dtype=np.float32))
    m = {}
    m["x"] = f(inputs["x"][b]); m["ctx"] = f(inputs["ctx"][b])
    cv = np.stack([np.asarray(inputs["c"][b]), np.asarray(inputs["c_ctx"])], -1)
    m["cvecT"] = f(cv.reshape(8, 128, 2).transpose(1, 0, 2))
    for n in ("ada_w", "ada_b", "norm_mix_g", "norm_ffn_g", "w_in", "q_norm_g", "k_norm_g", "subln_g", "s5_a_re", "s5_a_im",
              "s5_b_re", "s5_b_im", "s5_d", "s5_glu_w", "s5_glu_b", "hg_lb_logits", "hg_norm_g", "w_o_att", "w_o_s5",
              "w_o_hg", "w_out", "moe_w_group", "moe_b_group", "moe_w_expert", "moe_b_expert", "moe_w_gate", "moe_w_up", "moe_w_down"):
        m[n] = f(inputs[n])
    m["diff_lambda"] = f(np.asarray(inputs["diff_lambda"]).reshape(2, 256))
    m["s5_log_dt"] = f(np.asarray(inputs["s5_log_dt"]).reshape(2, 32))
    m["s5_c_re"] = f(np.asarray(inputs["s5_c_re"]).reshape(2, 256, 64))
    m["s5_c_im"] = f(np.asarray(inputs["s5_c_im"]).reshape(2, 256, 64))
    return m


def build_program(T, NE, debug=False, stop=None):
    nc = bass.Bass("TRN2", target_bir_lowering=False)
    P = Prog(nc, T, NE, debug=debug, stop=stop)
    P.declare()
    P.load_consts()
    P.run()
    P.fw.finish()
    return nc, P


def _run(self):
    for l in range(2):
        self.prologue(l)
        self.phaseA(l)
        if self.stop == "A":
            return
        self.phaseB(l)
        if self.stop == "B":
            return
        self.phaseC(l)
        if self.stop == "C":
            return
        self.phaseD(l)
        if self.stop == "D":
            return
        self.phaseE(l)
        if self.stop == "E":
            return
        self.phaseFs(l) if SPARSE else self.phaseF(l)
        if self.stop in ("F", "F0", "F1"):
            return


Prog.run = _run


def run_all(inputs, T, NE, debug=False, stop=None):
    nc, P = build_program(T, NE, debug, stop)
    consts = host_consts(T, NE)
    maps = []
    for b in range(8):
        m = core_inputs(inputs, b, T, NE)
        m.update(consts)
        maps.append({k_: v for k_, v in m.items() if k_ in P.din})
    res = run_bass_kernel_spmd(nc, maps, core_ids=list(range(8)))
    return res.results


def kernel(**inputs):
    T = inputs["x"].shape[1]
    NE = inputs["moe_w_gate"].shape[1]
    res = run_all(inputs, T, NE)
    return np.stack([r["y"] for r in res], 0).astype(np.float32)
```
